# Optimizing a Trainium2 kernel written in Bass

```python
import jax
import jax.numpy as jnp
from jax import lax
import numpy as np


D_MODEL = 1024
BATCH = 8
SEQ = 8192
DEPTH = 4

MEM_LEN = 256
EPS = 1e-6
N_EVEN = (DEPTH + 1) // 2
N_ODD = DEPTH // 2

MLSTM_HEADS = 4
MLSTM_DQK = 64
MLSTM_DV = 128
MLSTM_CHUNK = 64
GATE_SOFTCAP = 15.0

SB_HEADS = 4
SB_DH = 128
SB_BLOCK = 128

GDN_HEADS = 8
GDN_DK = 128
GDN_DV = 128
GDN_CONV = 4
GDN_CHUNK = 64

XA_HEADS = 4
XA_DH = D_MODEL // XA_HEADS
D_FF = 4 * D_MODEL

ML_QK = MLSTM_HEADS * MLSTM_DQK
ML_V = MLSTM_HEADS * MLSTM_DV
SB_W = SB_HEADS * SB_DH
AB_SIZES = (ML_QK, ML_QK, ML_V, ML_V, MLSTM_HEADS, MLSTM_HEADS, SB_W, SB_W, SB_W)
AB_IN = sum(AB_SIZES)
AB_OUT = ML_V + SB_W

GDN_QK = GDN_HEADS * GDN_DK
GDN_VW = GDN_HEADS * GDN_DV
GDN_CONV_CH = 2 * GDN_QK + GDN_VW
C_SIZES = (GDN_CONV_CH, GDN_VW, GDN_HEADS, GDN_HEADS)
C_IN = sum(C_SIZES)

kernel_name = 'hybrid_mlstm_stickbreak_gdn_memxattn'


def _split(y, sizes):
    return jnp.split(y, np.cumsum(sizes)[:-1].tolist(), axis=-1)


def rmsnorm(x, g):
    xf = x.astype(jnp.float32)
    y = xf * lax.rsqrt(jnp.mean(xf * xf, axis=-1, keepdims=True) + EPS)
    return (y * g.astype(jnp.float32)).astype(x.dtype)


def l2norm(x):
    xf = x.astype(jnp.float32)
    return xf * lax.rsqrt(jnp.sum(xf * xf, axis=-1, keepdims=True) + EPS)


def _to_chunks(a, L):
    B, T = a.shape[0], a.shape[1]
    a = a.reshape((B, T // L, L) + a.shape[2:])
    return jnp.moveaxis(a, (1, 3), (0, 2))


def _from_chunks(a):
    a = jnp.moveaxis(a, (0, 2), (1, 3))
    nc, L = a.shape[1], a.shape[2]
    return a.reshape((a.shape[0], nc * L) + a.shape[3:])


def mlstm(q, k, v, i_pre, f_pre):
    f32 = jnp.float32
    B, T, H, dk = q.shape
    dv = v.shape[-1]
    L = MLSTM_CHUNK
    log_i = GATE_SOFTCAP * jnp.tanh(i_pre.astype(f32) / GATE_SOFTCAP)
    log_f = jax.nn.log_sigmoid(f_pre.astype(f32))
    xs = (_to_chunks(q.astype(f32), L),
          _to_chunks(k.astype(f32) * (dk ** -0.5), L),
          _to_chunks(v.astype(f32), L),
          _to_chunks(log_i, L),
          _to_chunks(log_f, L))
    causal = jnp.tril(jnp.ones((L, L), dtype=bool))

    def step(carry, inp):
        C, n, m = carry
        qc, kc, vc, li, lf = inp
        b = jnp.cumsum(lf, axis=-1)
        d = jnp.where(causal, b[..., :, None] - b[..., None, :] + li[..., None, :], -jnp.inf)
        inter = b + m[..., None]
        m_t = jnp.maximum(inter, jnp.max(d, axis=-1))
        w_inter = jnp.exp(inter - m_t)
        s = jnp.einsum('bhtd,bhsd->bhts', qc, kc) * jnp.exp(d - m_t[..., None])
        num = jnp.einsum('bhts,bhsv->bhtv', s, vc) + w_inter[..., None] * jnp.einsum('bhvd,bhtd->bhtv', C, qc)
        den = jnp.sum(s, axis=-1) + w_inter * jnp.einsum('bhd,bhtd->bht', n, qc)
        h = num / jnp.maximum(jnp.abs(den), jnp.exp(-m_t))[..., None]
        m_new = m_t[..., -1]
        w_old = jnp.exp(b[..., -1] + m - m_new)
        w_s = jnp.exp(b[..., -1:] - b + li - m_new[..., None])
        C = w_old[..., None, None] * C + jnp.einsum('bhsv,bhsd->bhvd', vc * w_s[..., None], kc)
        n = w_old[..., None] * n + jnp.einsum('bhs,bhsd->bhd', w_s, kc)
        return (C, n, m_new), h

    init = (jnp.zeros((B, H, dv, dk), f32), jnp.zeros((B, H, dk), f32), jnp.zeros((B, H), f32))
    _, h = lax.scan(step, init, xs)
    return _from_chunks(h)


def stick_breaking(q, k, v):
    f32 = jnp.float32
    B, T, H, dh = q.shape
    scale = dh ** -0.5
    qf, kf, vf = q.astype(f32), k.astype(f32), v.astype(f32)
    outs = []
    for t0 in range(0, T, SB_BLOCK):
        t1 = t0 + SB_BLOCK
        z = jnp.einsum('bqhd,bkhd->bhqk', qf[:, t0:t1], kf[:, :t1]) * scale
        qpos = t0 + jnp.arange(SB_BLOCK)
        kpos = jnp.arange(t1)
        strict = kpos[None, :] < qpos[:, None]
        log_beta = jnp.where(strict, jax.nn.log_sigmoid(z), -jnp.inf)
        log_1m = jnp.where(strict, jax.nn.log_sigmoid(-z), 0.0)
        acc = lax.cumsum(log_1m, axis=3, reverse=True) - log_1m
        w = jnp.exp(log_beta + acc)
        outs.append(jnp.einsum('bhqk,bkhd->bqhd', w, vf[:, :t1]))
    return jnp.concatenate(outs, axis=1)


def short_conv(x, w):
    K = w.shape[0]
    T = x.shape[1]
    xp = jnp.pad(x, ((0, 0), (K - 1, 0), (0, 0)))
    return sum(xp[:, j:j + T] * w[j] for j in range(K))


def gated_deltanet(q, k, v, beta, log_alpha):
    f32 = jnp.float32
    B, T, H, dk = q.shape
    dv = v.shape[-1]
    L = GDN_CHUNK
    qc = _to_chunks(q.astype(f32) * (dk ** -0.5), L)
    kc = _to_chunks(k.astype(f32), L)
    vc = _to_chunks(v.astype(f32), L)
    bc = _to_chunks(beta.astype(f32), L)
    g = jnp.cumsum(_to_chunks(log_alpha.astype(f32), L), axis=-1)
    incl = jnp.tril(jnp.ones((L, L), dtype=bool))
    strict = jnp.tril(jnp.ones((L, L), dtype=bool), k=-1)
    decay = jnp.where(incl, jnp.exp(jnp.where(incl, g[..., :, None] - g[..., None, :], 0.0)), 0.0)
    kk = jnp.einsum('nbhtd,nbhsd->nbhts', kc, kc)
    lower = jnp.where(strict, bc[..., :, None] * kk * decay, 0.0)
    eye = jnp.eye(L, dtype=f32)
    rhs = jnp.concatenate([vc * bc[..., None], kc * (bc * jnp.exp(g))[..., None]], axis=-1)
    sol = lax.linalg.triangular_solve(lower + eye, rhs, left_side=True, lower=True, unit_diagonal=True)
    u_c, w_c = sol[..., :dv], sol[..., dv:]
    attn = jnp.einsum('nbhtd,nbhsd->nbhts', qc, kc) * decay
    q_dec = qc * jnp.exp(g)[..., None]
    g_last = g[..., -1]
    k_dec = kc * jnp.exp(g_last[..., None] - g)[..., None]

    def step(S, inp):
        u, wk, att, qd, kd, gl = inp
        v_new = u - jnp.einsum('bhtk,bhkv->bhtv', wk, S)
        o = jnp.einsum('bhtk,bhkv->bhtv', qd, S) + jnp.einsum('bhts,bhsv->bhtv', att, v_new)
        S = S * jnp.exp(gl)[..., None, None] + jnp.einsum('bhsk,bhsv->bhkv', kd, v_new)
        return S, o

    S0 = jnp.zeros((B, H, dk, dv), f32)
    _, o = lax.scan(step, S0, (u_c, w_c, attn, q_dec, k_dec, g_last))
    return _from_chunks(o)


def ab_mixer(h, w_in, b_i, b_f, head_gain, w_out):
    B, T, _ = h.shape
    mq, mk, mv, mo, mi, mf, sq, sk, sv = _split(h @ w_in, AB_SIZES)
    hm = mlstm(mq.reshape(B, T, MLSTM_HEADS, MLSTM_DQK),
               mk.reshape(B, T, MLSTM_HEADS, MLSTM_DQK),
               mv.reshape(B, T, MLSTM_HEADS, MLSTM_DV),
               mi + b_i, mf + b_f)
    hm = rmsnorm(hm, head_gain) * jax.nn.sigmoid(mo.reshape(B, T, MLSTM_HEADS, MLSTM_DV).astype(jnp.float32))
    hs = stick_breaking(sq.reshape(B, T, SB_HEADS, SB_DH),
                        sk.reshape(B, T, SB_HEADS, SB_DH),
                        sv.reshape(B, T, SB_HEADS, SB_DH))
    out = jnp.concatenate([hm.reshape(B, T, ML_V), hs.reshape(B, T, SB_W)], axis=-1).astype(h.dtype)
    return out @ w_out


def c_mixer(h, w_in, conv_w, a_log, dt_bias, head_gain, w_out):
    B, T, _ = h.shape
    qkv, gate, b, a = _split(h @ w_in, C_SIZES)
    qkv = jax.nn.silu(short_conv(qkv, conv_w))
    q, k, v = _split(qkv, (GDN_QK, GDN_QK, GDN_VW))
    q = l2norm(q.reshape(B, T, GDN_HEADS, GDN_DK))
    k = l2norm(k.reshape(B, T, GDN_HEADS, GDN_DK))
    v = v.reshape(B, T, GDN_HEADS, GDN_DV)
    beta = jax.nn.sigmoid(b.astype(jnp.float32))
    log_alpha = -jnp.exp(a_log.astype(jnp.float32)) * jax.nn.softplus(a.astype(jnp.float32) + dt_bias.astype(jnp.float32))
    o = gated_deltanet(q, k, v, beta, log_alpha)
    o = rmsnorm(o, head_gain) * jax.nn.silu(gate.reshape(B, T, GDN_HEADS, GDN_DV).astype(jnp.float32))
    return o.reshape(B, T, GDN_VW).astype(h.dtype) @ w_out


def mem_cross_attn(h, memn, wq, wk, wv, wo):
    B, T, _ = h.shape
    M = memn.shape[1]
    q = (h @ wq).reshape(B, T, XA_HEADS, XA_DH)
    k = (memn @ wk).reshape(B, M, XA_HEADS, XA_DH)
    v = (memn @ wv).reshape(B, M, XA_HEADS, XA_DH)
    s = jnp.einsum('bthd,bmhd->bhtm', q, k).astype(jnp.float32) * (XA_DH ** -0.5)
    p = jax.nn.softmax(s, axis=-1).astype(v.dtype)
    o = jnp.einsum('bhtm,bmhd->bthd', p, v).reshape(B, T, XA_HEADS * XA_DH)
    return o @ wo


def sq_relu_mlp(h, w1, w2):
    return jnp.square(jax.nn.relu(h @ w1)) @ w2


def setup_inputs(seed: int = 0) -> dict:
    key = jax.random.key(seed)
    ks = jax.random.split(key, 32)
    f32 = jnp.float32

    def w(k, shape, fan_in):
        return jax.random.normal(k, shape, f32) * (fan_in ** -0.5)

    def gain(k, shape):
        return 1.0 + 0.02 * jax.random.normal(k, shape, f32)

    x = jax.random.normal(ks[0], (BATCH, SEQ, D_MODEL), f32)
    mem = jax.random.normal(ks[1], (BATCH, MEM_LEN, D_MODEL), f32)
    mix_norm = gain(ks[2], (DEPTH, D_MODEL))
    ab_w_in = w(ks[3], (N_EVEN, D_MODEL, AB_IN), D_MODEL)
    ab_b_i = 0.1 * jax.random.normal(ks[4], (N_EVEN, MLSTM_HEADS), f32)
    ab_b_f = jnp.linspace(3.0, 6.0, MLSTM_HEADS, dtype=f32)[None, :] + 0.1 * jax.random.normal(ks[5], (N_EVEN, MLSTM_HEADS), f32)
    ab_head_gain = gain(ks[6], (N_EVEN, MLSTM_HEADS, MLSTM_DV))
    ab_w_out = w(ks[7], (N_EVEN, AB_OUT, D_MODEL), AB_OUT)
    c_w_in = w(ks[8], (N_ODD, D_MODEL, C_IN), D_MODEL)
    c_conv_w = w(ks[9], (N_ODD, GDN_CONV, GDN_CONV_CH), GDN_CONV)
    c_a_log = jnp.log(jax.random.uniform(ks[10], (N_ODD, GDN_HEADS), f32, 1.0, 16.0))
    dt = jnp.exp(jax.random.uniform(ks[11], (N_ODD, GDN_HEADS), f32, float(np.log(1e-3)), float(np.log(1e-1))))
    c_dt_bias = dt + jnp.log(-jnp.expm1(-dt))
    c_head_gain = gain(ks[12], (N_ODD, GDN_DV))
    c_w_out = w(ks[13], (N_ODD, GDN_VW, D_MODEL), GDN_VW)
    xa_norm = gain(ks[14], (DEPTH, D_MODEL))
    mem_norm = gain(ks[15], (D_MODEL,))
    xa_wq = w(ks[16], (DEPTH, D_MODEL, D_MODEL), D_MODEL)
    xa_wk = w(ks[17], (DEPTH, D_MODEL, D_MODEL), D_MODEL)
    xa_wv = w(ks[18], (DEPTH, D_MODEL, D_MODEL), D_MODEL)
    xa_wo = w(ks[19], (DEPTH, D_MODEL, D_MODEL), D_MODEL)
    mlp_norm = gain(ks[20], (DEPTH, D_MODEL))
    mlp_w1 = w(ks[21], (DEPTH, D_MODEL, D_FF), D_MODEL)
    mlp_w2 = w(ks[22], (DEPTH, D_FF, D_MODEL), D_FF)
    final_norm = gain(ks[23], (D_MODEL,))
    return {'x': x, 'mem': mem, 'mix_norm': mix_norm,
            'ab_w_in': ab_w_in, 'ab_b_i': ab_b_i, 'ab_b_f': ab_b_f,
            'ab_head_gain': ab_head_gain, 'ab_w_out': ab_w_out,
            'c_w_in': c_w_in, 'c_conv_w': c_conv_w, 'c_a_log': c_a_log,
            'c_dt_bias': c_dt_bias, 'c_head_gain': c_head_gain, 'c_w_out': c_w_out,
            'xa_norm': xa_norm, 'mem_norm': mem_norm, 'xa_wq': xa_wq, 'xa_wk': xa_wk,
            'xa_wv': xa_wv, 'xa_wo': xa_wo, 'mlp_norm': mlp_norm, 'mlp_w1': mlp_w1,
            'mlp_w2': mlp_w2, 'final_norm': final_norm}


def reference(x, mem, mix_norm, ab_w_in, ab_b_i, ab_b_f, ab_head_gain, ab_w_out,
              c_w_in, c_conv_w, c_a_log, c_dt_bias, c_head_gain, c_w_out,
              xa_norm, mem_norm, xa_wq, xa_wk, xa_wv, xa_wo,
              mlp_norm, mlp_w1, mlp_w2, final_norm):
    memn = rmsnorm(mem, mem_norm)
    for l in range(DEPTH):
        j = l // 2
        h = rmsnorm(x, mix_norm[l])
        if l % 2 == 0:
            x = x + ab_mixer(h, ab_w_in[j], ab_b_i[j], ab_b_f[j], ab_head_gain[j], ab_w_out[j])
        else:
            x = x + c_mixer(h, c_w_in[j], c_conv_w[j], c_a_log[j], c_dt_bias[j], c_head_gain[j], c_w_out[j])
        x = x + mem_cross_attn(rmsnorm(x, xa_norm[l]), memn, xa_wq[l], xa_wk[l], xa_wv[l], xa_wo[l])
        x = x + sq_relu_mlp(rmsnorm(x, mlp_norm[l]), mlp_w1[l], mlp_w2[l])
    return rmsnorm(x, final_norm)
```

```python
import numpy as np
from contextlib import ExitStack
import concourse.bass as bass
import concourse.mybir as mybir
from concourse.bass_utils import run_bass_kernel_spmd

F32 = mybir.dt.float32
BF16 = mybir.dt.bfloat16
AF = mybir.ActivationFunctionType
ALU = mybir.AluOpType

D = 1024
DEPTH = 4
MEM = 256
EPS = 1e-6
AB_IN = 3080
C_IN = 4112
DFF = 4096
LIM = 30000
DLIM = 1800
SAME_ENGINE_SYNC = True


class Res:
    __slots__ = ("w", "r", "x")

    def __init__(self, x=False):
        self.w = {}
        self.r = {}
        self.x = x


class V:
    __slots__ = ("ap", "res")

    def __init__(self, ap, res):
        self.ap = ap
        self.res = res


class Tl:
    def __init__(self, t, nres=1, x=False):
        self.t = t
        self.res = [Res(x) for _ in range(nres)]

    def __getitem__(self, idx):
        return V(self.t[idx], self.res[0])

    def v(self, idx, k=0):
        return V(self.t[idx], self.res[k])


class Prog:
    ENGS = ("pe", "act", "dve", "pool", "sp")

    def __init__(self, nc):
        self.nc = nc
        self.lists = {e: [] for e in self.ENGS}
        self.cnt = {}
        self.seen = {e: {} for e in self.ENGS}

    DMA_SLOTS = {"sp": 24, "pool": 12, "act": 8}

    def op(self, eng, fn, reads=(), writes=(), dma=False):
        if dma:
            tot = self.cnt.get(("dq", eng), 0)
            self.cnt[("dq", eng)] = tot + 1
            key = ("d", eng, tot % self.DMA_SLOTS[eng])
        else:
            key = ("c", eng)
        n = self.cnt.get(key, 0) + 1
        self.cnt[key] = n
        waits = {}
        seen = self.seen[eng]
        if dma and n > 1 and n - 1 > seen.get(key, 0):
            waits[key] = n - 1

        def need(k, v):
            if k[0] == "c" and k[1] == eng and (eng == "pe" or not SAME_ENGINE_SYNC):
                return
            if v > seen.get(k, 0) and v > waits.get(k, 0):
                waits[k] = v

        xr = [r for r in reads if r.res.x]
        if xr:
            writes = tuple(writes) + tuple(xr)
        for r in reads:
            for k, v in r.res.w.items():
                need(k, v)
        for w in writes:
            for k, v in w.res.w.items():
                need(k, v)
            for k, v in w.res.r.items():
                need(k, v)
        for k, v in waits.items():
            seen[k] = v
        self.lists[eng].append((list(waits.items()), fn, key, n))
        for r in reads:
            if r.res.r.get(key, 0) < n:
                r.res.r[key] = n
        for w in writes:
            w.res.w = {key: n}
            w.res.r = {}

    def barrier(self):
        snap = {k: v for k, v in self.cnt.items() if k[0] != "dq"}
        for e in self.ENGS:
            waits = []
            for k, v in snap.items():
                if k[0] == "c" and k[1] == e:
                    continue
                if v > self.seen[e].get(k, 0):
                    waits.append((k, v))
                    self.seen[e][k] = v
            if waits:
                self.lists[e].append((waits, None, None, 0))

    def finalize(self):
        nc = self.nc
        self.barrier()
        with ExitStack() as es:
            sems = {}
            for key, n in self.cnt.items():
                if key[0] == "dq":
                    continue
                lim = DLIM if key[0] == "d" else LIM
                for g in range((n - 1) // lim + 1):
                    sems[(key, g)] = es.enter_context(nc.semaphore("s" + "_".join(str(z) for z in key) + f"_{g}"))
            block = es.enter_context(nc.Block())

            def run(name, eng):
                for waits, fn, key, n in self.lists[name]:
                    for (k, v) in waits:
                        lim = DLIM if k[0] == "d" else LIM
                        g = (v - 1) // lim
                        val = v - g * lim
                        if k[0] == "d":
                            if g > 0:
                                eng.wait_ge(sems[(k, g - 1)], lim * 16)
                            eng.wait_ge(sems[(k, g)], val * 16)
                        else:
                            eng.wait_ge(sems[(k, g)], val)
                    if fn is not None:
                        inst = fn(eng)
                        lim = DLIM if key[0] == "d" else LIM
                        g = (n - 1) // lim
                        inst.then_inc(sems[(key, g)], 16 if key[0] == "d" else 1)

            @block.tensor
            def _(e):
                run("pe", e)

            @block.scalar
            def _(e):
                run("act", e)

            @block.vector
            def _(e):
                run("dve", e)

            @block.gpsimd
            def _(e):
                run("pool", e)

            @block.sync
            def _(e):
                run("sp", e)

    def mm(self, out, lhsT, rhs, start=True, stop=True):
        self.op("pe", lambda e: e.matmul(out.ap, lhsT.ap, rhs.ap, start=start, stop=stop),
                reads=(lhsT, rhs) if start else (lhsT, rhs, out), writes=(out,))

    def tr(self, out, in_, ident):
        self.op("pe", lambda e: e.transpose(out.ap, in_.ap, ident.ap), reads=(in_, ident), writes=(out,))

    def act(self, out, in_, func, scale=1.0, bias=0.0, accum=None, eng="act"):
        rd = [in_]
        if isinstance(scale, V):
            rd.append(scale)
        if isinstance(bias, V):
            rd.append(bias)
        sc = scale.ap if isinstance(scale, V) else scale
        bi = bias.ap if isinstance(bias, V) else bias
        wr = [out]
        if accum is not None:
            wr.append(accum)
        acc = accum.ap if accum is not None else None
        self.op("act", lambda e: e.activation(out.ap, in_.ap, func, bias=bi, scale=sc, accum_out=acc),
                reads=rd, writes=wr)

    def tt(self, out, a, b, op, eng="dve"):
        self.op(eng, lambda e: e.tensor_tensor(out.ap, a.ap, b.ap, op), reads=(a, b), writes=(out,))

    def ts(self, out, a, s1, s2, op0, op1=None, eng="dve", accum=None):
        rd = [a]
        if isinstance(s1, V):
            rd.append(s1)
        if isinstance(s2, V):
            rd.append(s2)
        v1 = s1.ap if isinstance(s1, V) else s1
        v2 = s2.ap if isinstance(s2, V) else s2
        wr = [out]
        if accum is not None:
            wr.append(accum)
        acc = accum.ap if accum is not None else None
        if op1 is None:
            self.op(eng, lambda e: e.tensor_scalar(out.ap, a.ap, v1, v2, op0, accum_out=acc) if acc is not None
                    else e.tensor_scalar(out.ap, a.ap, v1, v2, op0), reads=rd, writes=wr)
        else:
            self.op(eng, lambda e: e.tensor_scalar(out.ap, a.ap, v1, v2, op0, op1, accum_out=acc) if acc is not None
                    else e.tensor_scalar(out.ap, a.ap, v1, v2, op0, op1), reads=rd, writes=wr)

    def stt(self, out, a, s, b, op0, op1):
        rd = [a, b]
        if isinstance(s, V):
            rd.append(s)
        sv = s.ap if isinstance(s, V) else s
        self.op("dve", lambda e: e.scalar_tensor_tensor(out.ap, a.ap, sv, b.ap, op0, op1), reads=rd, writes=(out,))

    def scan(self, out, d0, d1, initial, op0, op1):
        rd = [d0, d1]
        if isinstance(initial, V):
            rd.append(initial)
        iv = initial.ap if isinstance(initial, V) else initial
        self.op("dve", lambda e: e.tensor_tensor_scan(out.ap, d0.ap, d1.ap, iv, op0, op1), reads=rd, writes=(out,))

    def copy(self, out, in_, eng="dve"):
        self.op(eng, lambda e: e.tensor_copy(out.ap, in_.ap), reads=(in_,), writes=(out,))

    def recip(self, out, in_):
        self.op("dve", lambda e: e.reciprocal(out.ap, in_.ap), reads=(in_,), writes=(out,))

    def memset(self, out, val, eng="dve"):
        self.op(eng, lambda e: e.memset(out.ap, val), writes=(out,))

    def dma(self, out, in_, q="sp", **kw):
        self.op(q, lambda e: e.dma_start(out=out.ap, in_=in_.ap, **kw), reads=(in_,), writes=(out,), dma=True)


class Ctx:
    _uid = [0]

    def __init__(self, nc, P, es):
        self.nc, self.P, self.es = nc, P, es
        self.n = 0
        Ctx._uid[0] += 1
        self.uid = Ctx._uid[0]

    def sb(self, shape, dt, nres=1, name=None):
        self.n += 1
        t = self.es.enter_context(self.nc.sbuf_tensor(f"{name or 't'}_{self.uid}_{self.n}", list(shape), dt))
        return Tl(t, nres)

    def ps(self, shape, dt=F32, name=None):
        self.n += 1
        assert shape[0] == 128 and shape[1] * (4 if dt == F32 else 2) == 2048, "PSUM tiles are whole banks"
        t = self.es.enter_context(self.nc.psum_tensor(f"{name or 'p'}_{self.uid}_{self.n}", list(shape), dt))
        return Tl(t, x=True)


class Rot:
    def __init__(self, items):
        self.items = items
        self.i = 0

    def next(self):
        t = self.items[self.i % len(self.items)]
        self.i += 1
        return t


def dram(nc, name, shape, dt, kind=None):
    if kind is None:
        t = nc.dram_tensor(name, list(shape), dt)
    else:
        t = nc.dram_tensor(name, list(shape), dt, kind=kind)
    return Tl(t.ap())


def load_w(P, C, w_dram_ap, K, N, name, q="pool", res=None):
    kc = K // 128
    w = C.sb([128, kc, N], BF16, name=name)
    src = w_dram_ap.rearrange("(c p) n -> p c n", p=128)
    step = 2048
    for c in range(kc):
        for n0 in range(0, N, step):
            n1 = min(N, n0 + step)
            P.dma(V(w.t[:, c, n0:n1], w.res[0]), V(src[:, c, n0:n1], res or Res()), q=q)
    return w


def rmsnorm_tile(P, C, x, g, h, N, ones, ps_rot, sq, rstd, lnt):
    if isinstance(sq, Tl):
        P.act(sq[:, :, :], x[:, :, :], AF.Square)
        sq = [sq[:, c, :] for c in range(8)]
    else:
        for c in range(8):
            P.act(sq[c], x[:, c, :], AF.Square)
    ps = ps_rot.next()
    for c in range(8):
        P.mm(ps[:, 0:N], ones[:, :], sq[c], start=(c == 0), stop=(c == 7))
    P.act(lnt[:, :], ps[:, 0:N], AF.Ln, scale=1.0 / D, bias=C.eps[:, 0:1])
    P.act(rstd[:, :], lnt[:, :], AF.Exp, scale=-0.5)
    for c in range(8):
        P.stt(h[:, c, :], x[:, c, :], g[:, c:c + 1], rstd[:, :], ALU.mult, ALU.mult)


def setup_consts(P, C):
    C.ones = C.sb([128, 128], BF16, name="ones")
    P.memset(C.ones[:, :], 1.0)
    C.eps = C.sb([128, 1], F32, name="eps")
    P.memset(C.eps[:, :], EPS)


def load_gain(P, C, g_dram_row_ap, name):
    g = C.sb([128, 8], F32, name=name)
    P.op("sp", lambda e: e.dma_start(out=g.t[:, :], in_=g_dram_row_ap.rearrange("(c p) -> p c", p=128),
                                     allow_slow_non_contiguous=True), writes=(g[:, :],), dma=True)
    return g


def sec_memn(nc, P, memT, mem_norm, memnT_out):
    with ExitStack() as es:
        C = Ctx(nc, P, es)
        setup_consts(P, C)
        g = load_gain(P, C, mem_norm, "gmem")
        x = C.sb([128, 8, MEM], F32)
        P.dma(x[:, :, :], V(memT.t.rearrange("(c p) t -> p c t", p=128), memT.res[0]))
        sq = C.sb([128, 8, MEM], BF16)
        rstd = C.sb([128, MEM], F32)
        lnt = C.sb([128, MEM], F32)
        h = C.sb([128, 8, MEM], BF16)
        ps = C.ps([128, 512])
        rmsnorm_tile(P, C, x, g, h, MEM, C.ones, Rot([ps]), sq, rstd, lnt)
        P.dma(V(memnT_out.t.rearrange("(c p) t -> p c t", p=128), memnT_out.res[0]), h[:, :, :])
        P.barrier()


def sec_post_a(nc, P, T, xin, hc, xout, w_out, xa_norm, wq, wk, wv, wo, memnT, N=512):
    with ExitStack() as es:
        C = Ctx(nc, P, es)
        setup_consts(P, C)
        g = load_gain(P, C, xa_norm, "gxa")
        wout_sb = load_w(P, C, w_out, D, D, "wout")
        wq_sb = load_w(P, C, wq, D, D, "wq")
        wo_sb = load_w(P, C, wo, D, D, "wo")
        kT = C.sb([128, 8, MEM], BF16, name="kT")
        v_sb = C.sb([128, 2, D], BF16, name="vsb")
        pss = Rot([C.ps([128, 512]) for _ in range(8)])
        with ExitStack() as es2:
            C2 = Ctx(nc, P, es2)
            wk_sb = load_w(P, C2, wk, D, D, "wk")
            wv_sb = load_w(P, C2, wv, D, D, "wv")
            mn = C2.sb([128, 8, MEM], BF16, name="mn")
            P.dma(mn[:, :, :], V(memnT.t.rearrange("(c p) t -> p c t", p=128), memnT.res[0]))
            for o in range(8):
                ps = pss.next()
                for c in range(8):
                    P.mm(ps[:, 0:MEM], wk_sb[:, c, o * 128:(o + 1) * 128], mn[:, c, :], start=(c == 0), stop=(c == 7))
                P.act(kT[:, o, :], ps[:, 0:MEM], AF.Copy)
            for mb in range(2):
                for half in range(2):
                    ps = pss.next()
                    for c in range(8):
                        P.mm(ps[:, :], mn[:, c, mb * 128:(mb + 1) * 128], wv_sb[:, c, half * 512:(half + 1) * 512],
                             start=(c == 0), stop=(c == 7))
                    P.copy(v_sb[:, mb, half * 512:(half + 1) * 512], ps[:, :])
            P.barrier()
        xs = [C.sb([128, 8, N], F32, name="x") for _ in range(2)]
        hcs = [C.sb([128, 8, N], BF16, name="hc") for _ in range(2)]
        sq = C.sb([128, 8, N], BF16, name="sq")
        h = C.sb([128, 8, N], BF16, name="h")
        qs = C.sb([128, 8, N], BF16, name="q")
        os_ = C.sb([128, 8, N], BF16, name="o")
        rstd = C.sb([128, N], F32, name="rstd")
        lnt = C.sb([128, N], F32, name="lnt")
        pT = [C.sb([128, N], BF16, name="pT") for _ in range(4)]
        rden = [C.sb([128, N], F32, name="rden") for _ in range(2)]
        xin_v = xin.t.rearrange("(c p) t -> p c t", p=128)
        hc_v = hc.t.rearrange("(c p) t -> p c t", p=128)
        xout_v = xout.t.rearrange("(c p) t -> p c t", p=128)
        for it in range(T // N):
            t0 = it * N
            x = xs[it % 2]
            hcx = hcs[it % 2]
            P.dma(x[:, :, :], V(xin_v[:, :, t0:t0 + N], xin.res[0]))
            P.dma(hcx[:, :, :], V(hc_v[:, :, t0:t0 + N], hc.res[0]))
            for o in range(8):
                ps = pss.next()
                for c in range(8):
                    P.mm(ps[:, 0:N], wout_sb[:, c, o * 128:(o + 1) * 128], hcx[:, c, :], start=(c == 0), stop=(c == 7))
                P.tt(x[:, o, :], x[:, o, :], ps[:, 0:N], ALU.add)
            rmsnorm_tile(P, C, x, g, h, N, C.ones, pss, sq, rstd, lnt)
            for o in range(8):
                ps = pss.next()
                for c in range(8):
                    P.mm(ps[:, 0:N], wq_sb[:, c, o * 128:(o + 1) * 128], h[:, c, :], start=(c == 0), stop=(c == 7))
                P.act(qs[:, o, :], ps[:, 0:N], AF.Copy, scale=1.0 / 16.0)
            for hd in range(4):
                pts = []
                for mb in range(2):
                    ps = pss.next()
                    for j in range(2):
                        P.mm(ps[:, 0:N], kT[:, 2 * hd + j, mb * 128:(mb + 1) * 128], qs[:, 2 * hd + j, :],
                             start=(j == 0), stop=(j == 1))
                    pt = pT[(2 * hd + mb) % 4]
                    P.act(pt[:, :], ps[:, 0:N], AF.Exp)
                    pts.append(pt)
                ps = pss.next()
                for mb in range(2):
                    P.mm(ps[:, 0:N], C.ones[:, :], pts[mb][:, :], start=(mb == 0), stop=(mb == 1))
                rd = rden[hd % 2]
                P.recip(rd[:, :], ps[:, 0:N])
                for j in range(2):
                    ps = pss.next()
                    for mb in range(2):
                        P.mm(ps[:, 0:N], v_sb[:, mb, (2 * hd + j) * 128:(2 * hd + j + 1) * 128], pts[mb][:, :],
                             start=(mb == 0), stop=(mb == 1))
                    P.tt(os_[:, 2 * hd + j, :], ps[:, 0:N], rd[:, :], ALU.mult)
            for o in range(8):
                ps = pss.next()
                for c in range(8):
                    P.mm(ps[:, 0:N], wo_sb[:, c, o * 128:(o + 1) * 128], os_[:, c, :], start=(c == 0), stop=(c == 7))
                P.tt(x[:, o, :], x[:, o, :], ps[:, 0:N], ALU.add)
            P.dma(V(xout_v[:, :, t0:t0 + N], xout.res[0]), x[:, :, :], q="sp")
        P.barrier()


def sec_post_b(nc, P, T, xin, xout, mlp_norm, w1, w2, final_norm=None, N=512):
    with ExitStack() as es:
        C = Ctx(nc, P, es)
        setup_consts(P, C)
        g = load_gain(P, C, mlp_norm, "gmlp")
        gf = load_gain(P, C, final_norm, "gfin") if final_norm is not None else None
        w1_sb = load_w(P, C, w1, D, DFF, "w1")
        w2_sb = load_w(P, C, w2, DFF, D, "w2")
        pss = Rot([C.ps([128, 512]) for _ in range(8)])
        xs = [C.sb([128, 8, N], F32, name="x") for _ in range(1)]
        h = C.sb([128, 8, N], BF16, name="h")
        gg = C.sb([128, 32, N], BF16, name="gg", nres=32)
        sq = [gg.v((slice(None), 24 + c, slice(None)), 24 + c) for c in range(8)]
        rr = [C.sb([128, N], F32, name="rr") for _ in range(3)]
        rstd = C.sb([128, N], F32, name="rstd")
        lnt = C.sb([128, N], F32, name="lnt")
        xin_v = xin.t.rearrange("(c p) t -> p c t", p=128)
        xout_v = xout.t.rearrange("(c p) t -> p c t", p=128)
        for it in range(T // N):
            t0 = it * N
            x = xs[0]
            P.dma(x[:, :, :], V(xin_v[:, :, t0:t0 + N], xin.res[0]))
            rmsnorm_tile(P, C, x, g, h, N, C.ones, pss, sq, rstd, lnt)
            for f in range(32):
                ps = pss.next()
                for c in range(8):
                    P.mm(ps[:, 0:N], w1_sb[:, c, f * 128:(f + 1) * 128], h[:, c, :], start=(c == 0), stop=(c == 7))
                r = rr[f % 3]
                P.act(r[:, :], ps[:, 0:N], AF.Relu)
                P.tt(gg.v((slice(None), f, slice(None)), f), r[:, :], r[:, :], ALU.mult, eng="pool")
            for o in range(8):
                ps = pss.next()
                for f in range(32):
                    P.mm(ps[:, 0:N], w2_sb[:, f, o * 128:(o + 1) * 128], gg.v((slice(None), f, slice(None)), f),
                         start=(f == 0), stop=(f == 31))
                P.tt(x[:, o, :], x[:, o, :], ps[:, 0:N], ALU.add)
            if gf is not None:
                for c in range(8):
                    P.act(sq[c], x[:, c, :], AF.Square)
                ps = pss.next()
                for c in range(8):
                    P.mm(ps[:, 0:N], C.ones[:, :], sq[c], start=(c == 0), stop=(c == 7))
                P.act(lnt[:, :], ps[:, 0:N], AF.Ln, scale=1.0 / D, bias=C.eps[:, 0:1])
                P.act(rstd[:, :], lnt[:, :], AF.Exp, scale=-0.5)
                for c in range(8):
                    P.stt(x[:, c, :], x[:, c, :], gf[:, c:c + 1], rstd[:, :], ALU.mult, ALU.mult)
            P.dma(V(xout_v[:, :, t0:t0 + N], xout.res[0]), x[:, :, :], q="sp")
        P.barrier()


def sec_pre_ab(nc, P, T, xin, mix_norm, w_in, S, N=512):
    with ExitStack() as es:
        C = Ctx(nc, P, es)
        setup_consts(P, C)
        g = load_gain(P, C, mix_norm, "gmix")
        w = load_w(P, C, w_in, D, AB_IN, "win")
        pss = Rot([C.ps([128, 512]) for _ in range(8)])
        xs = [C.sb([128, 8, N], F32, name="x") for _ in range(2)]
        sq = C.sb([128, 8, N], BF16, name="sq")
        h = C.sb([128, 8, N], BF16, name="h")
        rstd = C.sb([128, N], F32, name="rstd")
        lnt = C.sb([128, N], F32, name="lnt")
        mq_sb = [C.sb([64, 4, N], BF16, name="mq") for _ in range(2)]
        mk_sb = [C.sb([64, 4, N], BF16, name="mk") for _ in range(2)]
        g_sb = [C.sb([8, N], F32, name="gs") for _ in range(2)]
        sq_sb = [C.sb([128, 4, N], BF16, name="sqs") for _ in range(2)]
        sk_sb = [C.sb([128, 4, N], BF16, name="sks") for _ in range(2)]
        tmk = [C.sb([128, 256], BF16, name="tmk") for _ in range(2)]
        tmv = [C.sb([128, 512], BF16, name="tmv") for _ in range(2)]
        tsv = [C.sb([128, 512], BF16, name="tsv") for _ in range(2)]
        tsg = [C.sb([128, 512], BF16, name="tsg") for _ in range(2)]
        ex = [C.sb([128, 512], F32, name="ex") for _ in range(2)]
        xin_v = xin.t.rearrange("(c p) t -> p c t", p=128)
        for it in range(T // N):
            t0 = it * N
            b = it % 2
            x = xs[b]
            P.dma(x[:, :, :], V(xin_v[:, :, t0:t0 + N], xin.res[0]))
            rmsnorm_tile(P, C, x, g, h, N, C.ones, pss, sq, rstd, lnt)

            def fm(col0, M, dst, scale=1.0):
                ps = pss.next()
                for c in range(8):
                    P.mm(ps[0:M, 0:N], w[:, c, col0:col0 + M], h[:, c, :], start=(c == 0), stop=(c == 7))
                P.act(dst, ps[0:M, 0:N], AF.Copy, scale=scale)

            for hd in range(4):
                fm(64 * hd, 64, mq_sb[b][:, hd, :])
                fm(256 + 64 * hd, 64, mk_sb[b][:, hd, :], 0.125)
                fm(1544 + 128 * hd, 128, sq_sb[b][:, hd, :], 128 ** -0.5)
                fm(2056 + 128 * hd, 128, sk_sb[b][:, hd, :])
            fm(1536, 8, g_sb[b][:, :])
            P.dma(V(S["mqT"].t.rearrange("h d t -> d h t")[:, :, t0:t0 + N], S["mqT"].res[0]), mq_sb[b][:, :, :])
            P.dma(V(S["mkT"].t.rearrange("h d t -> d h t")[:, :, t0:t0 + N], S["mkT"].res[0]), mk_sb[b][:, :, :])
            P.dma(V(S["sqT"].t.rearrange("h d t -> d h t")[:, :, t0:t0 + N], S["sqT"].res[0]), sq_sb[b][:, :, :])
            P.dma(V(S["skT"].t.rearrange("h d t -> d h t")[:, :, t0:t0 + N], S["skT"].res[0]), sk_sb[b][:, :, :])
            P.dma(V(S["gT"].t[:, t0:t0 + N], S["gT"].res[0]), g_sb[b][:, :])
            for tb in range(N // 128):
                r0 = t0 + tb * 128
                bb = tb % 2

                def tm(col0, W):
                    ps = pss.next()
                    for c in range(8):
                        P.mm(ps[:, 0:W], h[:, c, tb * 128:(tb + 1) * 128], w[:, c, col0:col0 + W],
                             start=(c == 0), stop=(c == 7))
                    return ps

                ps = tm(256, 256)
                P.act(tmk[bb][:, :], ps[:, 0:256], AF.Copy, scale=0.125)
                P.dma(V(S["mk_tm"].t[r0:r0 + 128, :], S["mk_tm"].res[0]), tmk[bb][:, :])
                ps = tm(512, 512)
                P.copy(tmv[bb][:, :], ps[:, :])
                P.dma(V(S["mv_tm"].t[r0:r0 + 128, :], S["mv_tm"].res[0]), tmv[bb][:, :])
                ps = tm(2568, 512)
                P.copy(tsv[bb][:, :], ps[:, :])
                P.dma(V(S["sv_tm"].t[r0:r0 + 128, :], S["sv_tm"].res[0]), tsv[bb][:, :])
                ps = tm(1024, 512)
                P.act(ex[bb][:, :], ps[:, :], AF.Exp, scale=-1.0)
                P.ts(ex[bb][:, :], ex[bb][:, :], 1.0, None, ALU.add)
                P.recip(ex[bb][:, :], ex[bb][:, :])
                P.copy(tsg[bb][:, :], ex[bb][:, :], eng="pool")
                P.dma(V(S["sg_tm"].t[r0:r0 + 128, :], S["sg_tm"].res[0]), tsg[bb][:, :])
        P.barrier()


def make_mask(P, C, shape, pattern, base, cm, op, val=1.0, dt=BF16, name="mask"):
    src = C.sb(shape, F32, name=name + "s")
    P.memset(src[:, :], val, eng="pool")
    m = C.sb(shape, dt, name=name)
    P.op("pool", lambda e: e.affine_select(m.t[:, :], src.t[:, :], pattern, op, 0.0, base=base, channel_multiplier=cm),
         reads=(src[:, :],), writes=(m[:, :],))
    return m


def sec_sb(nc, P, T, S, hcT, N=512):
    NB = T // 128
    QB = N // 128
    with ExitStack() as es:
        C = Ctx(nc, P, es)
        setup_consts(P, C)
        one_col = C.sb([128, 1], F32, name="onec")
        P.memset(one_col[:, :], 1.0)
        negtri = make_mask(P, C, [128, 128], [[-1, 128]], 0, 1, ALU.is_ge, val=-1.0, name="ntri")
        negones = C.sb([128, 128], BF16, name="nones")
        P.memset(negones[:, :], -1.0)
        masks = [make_mask(P, C, [128, N], [[1, N]], -r * 128, -1, ALU.is_gt, name=f"m{r}") for r in range(QB)]
        KT = [C.sb([128, T], BF16, name="KT") for _ in range(2)]
        QT = [C.sb([128, T], BF16, name="QT") for _ in range(2)]
        Vv = [C.sb([128, NB, 128], BF16, name="Vv") for _ in range(2)]
        NSLOT = 4
        bankAB = [C.ps([128, 512]) for _ in range(NSLOT)]
        bankO = [C.ps([128, 512]) for _ in range(NSLOT)]
        ezs = [C.sb([128, N], F32, name="ez") for _ in range(NSLOT)]
        sps = [Rot([C.sb([128, N], BF16, name="sp") for _ in range(2)]) for _ in range(NSLOT)]
        wts = [Rot([C.sb([128, N], BF16, name="wt") for _ in range(2)]) for _ in range(NSLOT)]
        Sfs = [C.sb([128, N], F32, name="Sf") for _ in range(NSLOT)]
        Sbs = [Rot([C.sb([128, N], BF16, name="Sb") for _ in range(2)]) for _ in range(NSLOT)]
        obs = [C.sb([128, N], BF16, name="ob") for _ in range(NSLOT)]
        loaded = set()

        def load_head(hd):
            b = hd % 2
            P.dma(KT[b][:, :], V(S["skT"].t[hd, :, :], S["skT"].res[0]))
            P.dma(QT[b][:, :], V(S["sqT"].t[hd, :, :], S["sqT"].res[0]))
            P.dma(Vv[b][:, :, :], V(S["sv_tm"].t.rearrange("(j p) c -> p j c", p=128)[:, :, hd * 128:(hd + 1) * 128],
                                   S["sv_tm"].res[0]))

        def chain(slot, hd, gq):
            b = hd % 2
            q = QT[b][:, gq * N:(gq + 1) * N]
            jmax = gq * QB + QB - 1
            pab, po = bankAB[slot], bankO[slot]
            e, Sf, o = ezs[slot], Sfs[slot], obs[slot]
            sb_prev = None
            for j in range(jmax, -1, -1):
                k = KT[b][:, j * 128:(j + 1) * 128]
                P.mm(pab[:, 0:N], k, q)
                yield
                P.act(e[:, :], pab[:, 0:N], AF.Exp)
                yield
                s_ = sps[slot].next()
                P.act(s_[:, :], e[:, :], AF.Ln, bias=one_col[:, 0:1])
                yield
                r = j - gq * QB
                if r >= 0:
                    P.tt(s_[:, :], s_[:, :], masks[r][:, :], ALU.mult, eng="pool")
                    yield
                P.mm(pab[:, 0:N], negtri[:, :], s_[:, :], start=True, stop=False)
                if sb_prev is not None:
                    P.mm(pab[:, 0:N], negones[:, :], sb_prev[:, :], start=False, stop=False)
                P.mm(pab[:, 0:N], k, q, start=False, stop=True)
                yield
                w = wts[slot].next()
                P.act(w[:, :], pab[:, 0:N], AF.Exp)
                yield
                if r >= 0:
                    P.tt(w[:, :], w[:, :], masks[r][:, :], ALU.mult, eng="pool")
                    yield
                P.mm(po[:, 0:N], Vv[b][:, j, :], w[:, :], start=(j == jmax), stop=(j == 0))
                if j > 0:
                    if j == jmax:
                        P.copy(Sf[:, :], s_[:, :])
                    else:
                        P.tt(Sf[:, :], Sf[:, :], s_[:, :], ALU.add)
                    yield
                    sb_prev = Sbs[slot].next()
                    P.copy(sb_prev[:, :], Sf[:, :])
                yield
            P.copy(o[:, :], po[:, 0:N])
            yield
            P.dma(V(hcT.t[512 + hd * 128:512 + (hd + 1) * 128, gq * N:(gq + 1) * N], hcT.res[0]), o[:, :])

        work = [(hd, gq) for hd in range(4) for gq in range(T // N - 1, -1, -1)]
        slots = [None] * NSLOT
        slot_head = [None] * NSLOT
        wi = 0
        while True:
            busy = False
            for sl_ in range(NSLOT):
                if slots[sl_] is None and wi < len(work):
                    hd, gq = work[wi]
                    if not any(slots[z] is not None and slot_head[z] == hd - 2 for z in range(NSLOT)):
                        wi += 1
                        if hd not in loaded:
                            load_head(hd)
                            loaded.add(hd)
                        slots[sl_] = chain(sl_, hd, gq)
                        slot_head[sl_] = hd
                if slots[sl_] is not None:
                    busy = True
                    try:
                        next(slots[sl_])
                    except StopIteration:
                        slots[sl_] = None
            if not busy and wi >= len(work):
                break
        P.barrier()


def sec_mlstm(nc, P, T, S, b_i, b_f, head_gain, hcT):
    NCH = T // 128
    with ExitStack() as es:
        C = Ctx(nc, P, es)
        setup_consts(P, C)
        one4 = C.sb([4, 1], F32, name="one4")
        P.memset(one4[:, :], 1.0)
        ident = make_mask(P, C, [128, 128], [[-1, 128]], 0, 1, ALU.is_equal, dt=F32, name="identf")
        identb = C.sb([128, 128], BF16, name="identb")
        P.copy(identb[:, :], ident[:, :])
        maskLT = make_mask(P, C, [128, 128], [[1, 128]], 0, -1, ALU.is_ge, name="mlt")
        gain_bc = C.sb([128, 512], F32, name="gainbc")
        P.op("sp", lambda e: e.dma_start(out=gain_bc.t[:, :],
                                         in_=head_gain.rearrange("h v -> (h v)").partition_broadcast(128)),
             writes=(gain_bc[:, :],), dma=True)
        tok = C.sb([128, NCH, 12], F32, name="tok")
        with ExitStack() as es2:
            C2 = Ctx(nc, P, es2)
            mi = C2.sb([4, T], F32, name="mi")
            mf = C2.sb([4, T], F32, name="mf")
            P.dma(mi[:, :], V(S["gT"].t[0:4, :], S["gT"].res[0]))
            P.dma(mf[:, :], V(S["gT"].t[4:8, :], S["gT"].res[0]))
            bi = C2.sb([4, 1], F32, name="bi")
            bf = C2.sb([4, 1], F32, name="bf")
            P.op("sp", lambda e: e.dma_start(out=bi.t[:, :], in_=b_i.rearrange("(h o) -> h o", o=1)),
                 writes=(bi[:, :],), dma=True)
            P.op("sp", lambda e: e.dma_start(out=bf.t[:, :], in_=b_f.rearrange("(h o) -> h o", o=1)),
                 writes=(bf[:, :],), dma=True)
            P.ts(bi[:, :], bi[:, :], 1.0 / 15.0, None, ALU.mult)
            P.ts(bf[:, :], bf[:, :], -1.0, None, ALU.mult)
            t1 = C2.sb([4, T], F32, name="t1")
            P.act(t1[:, :], mi[:, :], AF.Tanh, scale=1.0 / 15.0, bias=bi[:, 0:1])
            e1 = C2.sb([4, T], F32, name="e1")
            P.act(e1[:, :], mf[:, :], AF.Exp, scale=-1.0, bias=bf[:, 0:1])
            P.act(e1[:, :], e1[:, :], AF.Ln, bias=one4[:, 0:1])
            P.ts(e1[:, :], e1[:, :], -1.0, None, ALU.mult)
            ones_r = mf
            P.memset(ones_r[:, :], 1.0)
            Fc = C2.sb([4, T], F32, name="Fc")
            P.scan(Fc[:, :], ones_r[:, :], e1[:, :], 0.0, ALU.mult, ALU.add)
            a = mi
            P.stt(a[:, :], t1[:, :], 15.0, Fc[:, :], ALU.mult, ALU.subtract)
            M = t1
            P.scan(M[:, :], a[:, :], a[:, :], 0.0, ALU.max, ALU.max)
            Er = C2.sb([4, T], F32, name="Er")
            Dr = e1
            Gr = ones_r
            gd = C2.sb([4, NCH], F32, name="gd")
            for c in range(NCH):
                sl = slice(c * 128, (c + 1) * 128)
                me = M[:, c * 128 + 127:c * 128 + 128]
                P.ts(Er[:, sl], a[:, sl], me, None, ALU.subtract)
                P.ts(Dr[:, sl], Fc[:, sl], -1.0, me, ALU.mult, ALU.subtract)
                if c == 0:
                    P.ts(gd[:, 0:1], me, -1.0, None, ALU.mult)
                else:
                    P.tt(gd[:, c:c + 1], M[:, c * 128 - 1:c * 128], me, ALU.subtract)
                P.ts(Gr[:, sl], Fc[:, sl], 0.0, gd[:, c:c + 1], ALU.mult, ALU.add)
            P.act(Er[:, :], Er[:, :], AF.Exp)
            P.act(Dr[:, :], Dr[:, :], AF.Exp, scale=2.0)
            P.act(Gr[:, :], Gr[:, :], AF.Exp)
            pst = Rot([C2.ps([128, 512]) for _ in range(2)])
            for c in range(NCH):
                sl = slice(c * 128, (c + 1) * 128)
                ps = pst.next()
                for i, rw in enumerate((Er, Dr, Gr)):
                    P.tr(ps[:, 4 * i:4 * i + 4], rw[:, sl], ident[0:4, 0:4])
                P.copy(tok[:, c, :], ps[:, 0:12])
            P.barrier()
        qT = [C.sb([64, 4, 128], BF16, name="qT") for _ in range(2)]
        kT = [C.sb([64, 4, 128], BF16, name="kT") for _ in range(2)]
        ktm = [C.sb([128, 256], BF16, name="ktm") for _ in range(2)]
        vtm = [C.sb([128, 512], BF16, name="vtm") for _ in range(2)]
        sgt = [C.sb([128, 512], BF16, name="sgt") for _ in range(2)]
        hm = [C.sb([128, 4, 128], BF16, name="hm", nres=4) for _ in range(2)]
        bankA = [C.ps([128, 512]) for _ in range(4)]
        bankB = [C.ps([128, 512]) for _ in range(4)]

        def load_chunk(c):
            b = c % 2
            sl = slice(c * 128, (c + 1) * 128)
            P.dma(qT[b][:, :, :], V(S["mqT"].t.rearrange("h d t -> d h t")[:, :, sl], S["mqT"].res[0]))
            P.dma(kT[b][:, :, :], V(S["mkT"].t.rearrange("h d t -> d h t")[:, :, sl], S["mkT"].res[0]))
            P.dma(ktm[b][:, :], V(S["mk_tm"].t[sl, :], S["mk_tm"].res[0]))
            P.dma(vtm[b][:, :], V(S["mv_tm"].t[sl, :], S["mv_tm"].res[0]))
            P.dma(sgt[b][:, :], V(S["sg_tm"].t[sl, :], S["sg_tm"].res[0]))

        def head_chain(hd):
            pa, pb = bankA[hd], bankB[hd]
            Cst = C.sb([64, 129], F32, name=f"Cst{hd}")
            Cbf = C.sb([64, 129], BF16, name=f"Cbf{hd}")
            s_m = C.sb([128, 128], BF16, name=f"sm{hd}")
            v_e = C.sb([128, 129], BF16, name=f"vt{hd}")
            jk = C.sb([128, 128], BF16, name=f"jk{hd}")
            s_ = C.sb([128, 8], F32, name=f"sc{hd}")
            o_1 = C.sb([128, 128], F32, name=f"o1{hd}")
            o_2 = C.sb([128, 128], F32, name=f"o2{hd}")
            P.memset(Cst[:, :], 0.0)
            yield
            for c in range(NCH):
                b = c % 2
                ecol = tok[:, c, hd:hd + 1]
                dcol = tok[:, c, 4 + hd:5 + hd]
                gcol = tok[0:64, c, 8 + hd:9 + hd]
                P.mm(pa[:, 0:128], kT[b][:, hd, :], qT[b][:, hd, :])
                P.ts(v_e[:, 0:128], vtm[b][:, hd * 128:(hd + 1) * 128], ecol, None, ALU.mult, eng="pool")
                yield
                P.tt(s_m[:, :], pa[:, 0:128], maskLT[:, :], ALU.mult)
                P.copy(v_e[:, 128:129], ecol, eng="pool")
                yield
                P.ts(Cbf[:, :], Cst[:, :], gcol, None, ALU.mult)
                yield
                P.mm(pb[:, 0:129], s_m[:, :], v_e[:, :], start=True, stop=False)
                P.mm(pb[:, 0:129], qT[b][:, hd, :], Cbf[:, :], start=False, stop=True)
                P.mm(pa[0:64, 128:257], ktm[b][:, hd * 64:(hd + 1) * 64], v_e[:, :])
                yield
                P.stt(Cst[:, :], Cst[:, :], gcol, pa[0:64, 128:257], ALU.mult, ALU.add)
                P.act(jk[:, :], pb[:, 0:128], AF.Square, accum=s_[:, 0:1])
                yield
                P.copy(s_[:, 1:2], pb[:, 128:129])
                yield
                P.ts(s_[:, 2:3], s_[:, 1:2], s_[:, 1:2], None, ALU.mult)
                yield
                P.ts(s_[:, 2:3], s_[:, 2:3], dcol, EPS * 128.0, ALU.max, ALU.mult)
                yield
                P.tt(s_[:, 3:4], s_[:, 2:3], s_[:, 0:1], ALU.add)
                yield
                P.act(s_[:, 4:5], s_[:, 3:4], AF.Ln, scale=1.0 / 128.0)
                yield
                P.act(s_[:, 5:6], s_[:, 4:5], AF.Exp, scale=-0.5)
                yield
                P.stt(o_1[:, :], pb[:, 0:128], s_[:, 5:6], gain_bc[:, hd * 128:(hd + 1) * 128], ALU.mult, ALU.mult)
                yield
                P.tt(o_2[:, :], o_1[:, :], sgt[b][:, hd * 128:(hd + 1) * 128], ALU.mult, eng="pool")
                yield
                P.tr(pa[:, 384:512], o_2[:, :], ident[:, :])
                yield
                P.copy(hm[b].v((slice(None), hd, slice(None)), hd), pa[:, 384:512])
                yield "chunk_done"

        chains = [head_chain(hd) for hd in range(4)]
        for ch_ in chains:
            next(ch_)
        load_chunk(0)
        for c in range(NCH):
            if c + 1 < NCH:
                load_chunk(c + 1)
            active = list(chains)
            while active:
                for ch_ in list(active):
                    if next(ch_) == "chunk_done":
                        active.remove(ch_)
            b = c % 2
            sl = slice(c * 128, (c + 1) * 128)
            P.op("sp", lambda e, b=b, sl=sl: e.dma_start(
                out=hcT.t[0:512, :].rearrange("(h p) t -> p h t", p=128)[:, :, sl], in_=hm[b].t[:, :, :]),
                reads=tuple(V(hm[b].t[:, :, :], hm[b].res[k]) for k in range(4)), writes=(hcT[:, :],), dma=True)
        P.barrier()


def sec_pre_c(nc, P, T, xin, mix_norm, w_in, conv_w, S, N=512):
    with ExitStack() as es:
        C = Ctx(nc, P, es)
        setup_consts(P, C)
        g = load_gain(P, C, mix_norm, "gmix")
        w = load_w(P, C, w_in, D, C_IN, "win")
        cw = C.sb([128, 24, 4], F32, name="cw")
        for j in range(4):
            P.op("sp", lambda e, j=j: e.dma_start(out=cw.t[:, :, j], in_=conv_w[j, :].rearrange("(c p) -> p c", p=128),
                                                 allow_slow_non_contiguous=True), writes=(cw[:, :, :],), dma=True)
        ident = make_mask(P, C, [128, 128], [[-1, 128]], 0, 1, ALU.is_equal, dt=BF16, name="identb")
        halo = C.sb([128, 24, 3], F32, name="halo")
        P.memset(halo[:, :, :], 0.0)
        pss = Rot([C.ps([128, 512]) for _ in range(6)])
        psT = Rot([C.ps([128, 1024], BF16) for _ in range(2)])
        xs = [C.sb([128, 8, N], F32, name="x") for _ in range(1)]
        sq = C.sb([128, 8, N], BF16, name="sq")
        h = C.sb([128, 8, N], BF16, name="h")
        rstd = C.sb([128, N], F32, name="rstd")
        lnt = C.sb([128, N], F32, name="lnt")
        cv = Rot([C.sb([128, N + 3], F32, name="cv") for _ in range(2)])
        acc = Rot([C.sb([128, N], F32, name="acc") for _ in range(2)])
        y = C.sb([128, 16, N], F32, name="y", nres=16)
        vb = C.sb([128, 8, N], BF16, name="vb", nres=8)
        kb = C.sb([128, 8, N], BF16, name="kb", nres=8)
        qb = C.sb([128, 8, N], BF16, name="qb")
        s2 = Rot([C.sb([128, N], BF16, name="s2") for _ in range(2)])
        l2 = Rot([C.sb([128, N], F32, name="l2") for _ in range(2)])
        ba = [C.sb([8, N], F32, name="ba") for _ in range(2)]
        tsg = Rot([C.sb([128, 512], BF16, name="tsg") for _ in range(2)])
        ttm = Rot([C.sb([128, 1024], BF16, name="ttm") for _ in range(2)])
        xin_v = xin.t.rearrange("(c p) t -> p c t", p=128)
        for it in range(T // N):
            t0 = it * N
            x = xs[0]
            P.dma(x[:, :, :], V(xin_v[:, :, t0:t0 + N], xin.res[0]))
            rmsnorm_tile(P, C, x, g, h, N, C.ones, pss, sq, rstd, lnt)
            for ch in range(24):
                ps = pss.next()
                for c in range(8):
                    P.mm(ps[:, 0:N], w[:, c, ch * 128:(ch + 1) * 128], h[:, c, :], start=(c == 0), stop=(c == 7))
                cvt = cv.next()
                P.act(cvt[:, 3:3 + N], ps[:, 0:N], AF.Copy)
                P.copy(cvt[:, 0:3], halo[:, ch, :], eng="pool")
                P.copy(halo[:, ch, :], cvt[:, N:N + 3], eng="pool")
                a_ = acc.next()
                P.ts(a_[:, :], cvt[:, 0:N], cw[:, ch, 0:1], None, ALU.mult)
                for j in range(1, 4):
                    P.stt(a_[:, :], cvt[:, j:j + N], cw[:, ch, j:j + 1], a_[:, :], ALU.mult, ALU.add)
                if ch < 16:
                    P.act(y.v((slice(None), ch, slice(None)), ch), a_[:, :], AF.Silu)
                else:
                    P.act(vb.v((slice(None), ch - 16, slice(None)), ch - 16), a_[:, :], AF.Silu)
            for tb in range(N // 128):
                r0 = t0 + tb * 128
                for half in range(2):
                    ps = pss.next()
                    for c in range(8):
                        P.mm(ps[:, :], h[:, c, tb * 128:(tb + 1) * 128], w[:, c, 3072 + half * 512:3072 + (half + 1) * 512],
                             start=(c == 0), stop=(c == 7))
                    tg = tsg.next()
                    P.act(tg[:, :], ps[:, :], AF.Silu)
                    P.dma(V(S["sg_tm"].t[r0:r0 + 128, half * 512:(half + 1) * 512], S["sg_tm"].res[0]), tg[:, :])
            for i, nm in enumerate(("bT", "aT")):
                ps = pss.next()
                for c in range(8):
                    P.mm(ps[0:8, 0:N], w[:, c, 4096 + 8 * i:4104 + 8 * i], h[:, c, :], start=(c == 0), stop=(c == 7))
                P.act(ba[i][:, :], ps[0:8, 0:N], AF.Copy)
                P.dma(V(S[nm].t[:, t0:t0 + N], S[nm].res[0]), ba[i][:, :])
            for ch in range(16):
                yv = y.v((slice(None), ch, slice(None)), ch)
                s_ = s2.next()
                P.act(s_[:, :], yv, AF.Square)
                ps = pss.next()
                P.mm(ps[:, 0:N], C.ones[:, :], s_[:, :])
                l_ = l2.next()
                P.act(l_[:, :], ps[:, 0:N], AF.Ln, bias=C.eps[:, 0:1])
                P.act(l_[:, :], l_[:, :], AF.Exp, scale=-0.5)
                if ch < 8:
                    P.stt(qb[:, ch, :], yv, 128 ** -0.5, l_[:, :], ALU.mult, ALU.mult)
                else:
                    P.tt(kb.v((slice(None), ch - 8, slice(None)), ch - 8), yv, l_[:, :], ALU.mult)
            P.dma(V(S["qT"].t.rearrange("h d t -> d h t")[:, :, t0:t0 + N], S["qT"].res[0]), qb[:, :, :])
            P.dma(V(S["kT"].t.rearrange("h d t -> d h t")[:, :, t0:t0 + N], S["kT"].res[0]),
                  V(kb.t[:, :, :], kb.res[7]))
            for src, nm in ((kb, "k_tm"), (vb, "v_tm")):
                for tb in range(N // 128):
                    r0 = t0 + tb * 128
                    pt = psT.next()
                    for ch in range(8):
                        P.tr(pt[:, ch * 128:(ch + 1) * 128], src.v((slice(None), ch, slice(tb * 128, (tb + 1) * 128)), ch),
                             ident[:, :])
                    tt_ = ttm.next()
                    P.copy(tt_[:, :], pt[:, :])
                    P.dma(V(S[nm].t[r0:r0 + 128, :], S[nm].res[0]), tt_[:, :])
        P.barrier()


def sec_gdn(nc, P, T, S, a_log, dt_bias, head_gain, hcT, SCR):
    NSC = T // 128
    NCH = T // 64
    SEG = min(T, 2048)
    with ExitStack() as es:
        C = Ctx(nc, P, es)
        setup_consts(P, C)
        one8 = C.sb([8, 1], F32, name="one8")
        P.memset(one8[:, :], 1.0)
        identf = make_mask(P, C, [128, 128], [[-1, 128]], 0, 1, ALU.is_equal, dt=F32, name="identf")
        identb = C.sb([128, 128], BF16, name="identb")
        P.copy(identb[:, :], identf[:, :])
        mS = make_mask(P, C, [128, 128], [[-1, 128]], 0, 1, ALU.is_gt, dt=F32, name="mS")
        mI = make_mask(P, C, [128, 128], [[1, 128]], 0, -1, ALU.is_ge, dt=F32, name="mI")
        P.memset(mS[64:128, 0:64], 0.0, eng="pool")
        P.memset(mI[0:64, 64:128], 0.0, eng="pool")
        gain_bc = C.sb([128, 128], F32, name="gainbc")
        P.op("sp", lambda e: e.dma_start(out=gain_bc.t[:, :], in_=head_gain.partition_broadcast(128)),
             writes=(gain_bc[:, :],), dma=True)
        tokc = C.sb([128, NSC, 5, 8], F32, name="tokc")
        egl = C.sb([128, 8, NCH], F32, name="egl")
        with ExitStack() as es2:
            C2 = Ctx(nc, P, es2)
            bt = C2.sb([8, T], F32, name="bt")
            at = C2.sb([8, T], F32, name="at")
            P.dma(bt[:, :], V(S["bT"].t[:, :], S["bT"].res[0]))
            P.dma(at[:, :], V(S["aT"].t[:, :], S["aT"].res[0]))
            al = C2.sb([8, 1], F32, name="al")
            db = C2.sb([8, 1], F32, name="db")
            P.op("sp", lambda e: e.dma_start(out=al.t[:, :], in_=a_log.rearrange("(h o) -> h o", o=1)),
                 writes=(al[:, :],), dma=True)
            P.op("sp", lambda e: e.dma_start(out=db.t[:, :], in_=dt_bias.rearrange("(h o) -> h o", o=1)),
                 writes=(db[:, :],), dma=True)
            P.act(al[:, :], al[:, :], AF.Exp)
            P.ts(al[:, :], al[:, :], -1.0, None, ALU.mult)
            P.act(bt[:, :], bt[:, :], AF.Exp, scale=-1.0)
            P.ts(bt[:, :], bt[:, :], 1.0, None, ALU.add)
            P.recip(bt[:, :], bt[:, :])
            P.act(at[:, :], at[:, :], AF.Exp, bias=db[:, 0:1])
            P.act(at[:, :], at[:, :], AF.Ln, bias=one8[:, 0:1])
            P.ts(at[:, :], at[:, :], al[:, 0:1], None, ALU.mult)
            nf = C2.sb([8, T], F32, name="nf")
            P.memset(nf[:, :], 1.0)
            P.memset(V(nf.t[:, :].rearrange("p (c l) -> p c l", l=64)[:, :, 0:1], nf.res[0]), 0.0)
            gr = C2.sb([8, T], F32, name="gr")
            P.scan(gr[:, :], nf[:, :], at[:, :], 0.0, ALU.mult, ALU.add)
            P.dma(SCR["gD"][:, :], gr[:, :])
            eg = C2.sb([8, T], F32, name="eg")
            P.act(eg[:, :], gr[:, :], AF.Exp)
            beg = nf
            P.tt(beg[:, :], bt[:, :], eg[:, :], ALU.mult)
            egg = at
            for c in range(NCH):
                sl = slice(c * 64, (c + 1) * 64)
                P.ts(egg[:, sl], gr[:, sl], -1.0, gr[:, c * 64 + 63:c * 64 + 64], ALU.mult, ALU.add)
            P.act(egg[:, :], egg[:, :], AF.Exp)
            eG = C2.sb([8, NCH], F32, name="eG")
            P.copy(eG[:, :], V(eg.t[:, :].rearrange("p (c l) -> p c l", l=64)[:, :, 63], eg.res[0]))
            P.dma(SCR["eGD"][:, :], eG[:, :])
            P.op("sp", lambda e: e.dma_start(out=egl.t[:, :, :].rearrange("p h c -> p (h c)"),
                                             in_=SCR["eGD"].t.rearrange("h c -> (h c)").partition_broadcast(128)),
                 reads=(SCR["eGD"][:, :],), writes=(egl[:, :, :],), dma=True)
            pst = Rot([C2.ps([128, 512]) for _ in range(2)])
            for sc in range(NSC):
                sl = slice(sc * 128, (sc + 1) * 128)
                ps = pst.next()
                for i, rw in enumerate((gr, bt, beg, egg, eg)):
                    P.tr(ps[:, 8 * i:8 * i + 8], rw[:, sl], identf[0:8, 0:8])
                P.copy(V(tokc.t[:, sc, :, :].rearrange("p a b -> p (a b)"), tokc.res[0]), ps[:, 0:40])
            P.barrier()
        GT = 256
        NG = T // GT
        SPG = GT // 128
        qTg = [C.sb([128, 8, GT], BF16, name="qTg") for _ in range(2)]
        kTg = [C.sb([128, 8, GT], BF16, name="kTg") for _ in range(2)]
        ktg = [C.sb([128, SPG, D], BF16, name="ktg") for _ in range(2)]
        vtg = [C.sb([128, SPG, D], BF16, name="vtg") for _ in range(2)]
        sgg = [C.sb([128, SPG, D], BF16, name="sgg") for _ in range(2)]
        Gbg = [C.sb([128, 8, GT], F32, name="Gbg") for _ in range(2)]
        banks = [C.ps([128, 512]) for _ in range(8)]

        def load_group(gi):
            b = gi % 2
            t0 = gi * GT
            P.dma(qTg[b][:, :, :], V(S["qT"].t.rearrange("h d t -> d h t")[:, :, t0:t0 + GT], S["qT"].res[0]))
            P.dma(kTg[b][:, :, :], V(S["kT"].t.rearrange("h d t -> d h t")[:, :, t0:t0 + GT], S["kT"].res[0]))
            for tl, nm in ((ktg, "k_tm"), (vtg, "v_tm"), (sgg, "sg_tm")):
                P.dma(tl[b][:, :, :], V(S[nm].t[t0:t0 + GT, :].rearrange("(s p) c -> p s c", p=128), S[nm].res[0]))
            for hd in range(8):
                P.op("sp", lambda e, b=b, t0=t0, hd=hd: e.dma_start(
                    out=Gbg[b].t[:, hd, :], in_=SCR["gD"].t[hd, t0:t0 + GT].partition_broadcast(128)),
                    reads=(SCR["gD"][:, :],), writes=(Gbg[b][:, :, :],), dma=True)

        def head_chain(hd):
            bank = banks[hd]

            def t32(name):
                return C.sb([128, 128], F32, name=f"{name}{hd}")

            def t16(name):
                return C.sb([128, 128], BF16, name=f"{name}{hd}")

            dtmp, Da, Dt = t32("dtmp"), t32("Da"), t32("Dt")
            Xs, Ys, Qs = Rot([t32("X"), t32("X")]), Rot([t32("Y"), t32("Y")]), Rot([t32("Q"), t32("Q")])
            af, u, t3, o_, o_1, o_2 = t32("af"), t32("u"), t32("t3"), t32("o"), t32("o1"), t32("o2")
            ab_, tb_, v_b, k_b, k_d, wT_, vn, jk, hb = (t16("ab"), t16("tb"), t16("vb"), t16("kb"), t16("kd"),
                                                       t16("wT"), t16("vn"), t16("jk"), t16("hb"))
            Sst = t32("Sst")
            Sbf = Rot([t16("Sbf"), t16("Sbf")])
            s_ = C.sb([128, 4], F32, name=f"scl{hd}")
            P.memset(Sst[:, :], 0.0)
            sbf = Sbf.next()
            P.memset(sbf[:, :], 0.0)
            s0, s1, s2, s3 = (slice(0, 128), slice(128, 256), slice(256, 384), slice(384, 512))
            yield
            for sc in range(NSC):
                gi = (sc * 128) // GT
                b = gi % 2
                si = sc % SPG
                lsl = slice(si * 128, (si + 1) * 128)
                hsl = slice(hd * 128, (hd + 1) * 128)
                kTs = kTg[b][:, hd, lsl]
                qTs = qTg[b][:, hd, lsl]
                gsl = Gbg[b][:, hd, lsl]
                gcol = tokc[:, sc, 0, hd:hd + 1]
                bcol = tokc[:, sc, 1, hd:hd + 1]
                begcol = tokc[:, sc, 2, hd:hd + 1]
                eggcol = tokc[:, sc, 3, hd:hd + 1]
                P.mm(bank[:, s0], kTs, kTs)
                P.mm(bank[:, s1], kTs, qTs)
                P.ts(dtmp[:, :], gsl, gcol, 0.0, ALU.subtract, ALU.max)
                yield
                P.act(Da[:, :], dtmp[:, :], AF.Exp, scale=-1.0)
                P.ts(v_b[:, :], vtg[b][:, si, hsl], bcol, None, ALU.mult, eng="pool")
                yield
                P.stt(Da[:, :], Da[:, :], bcol, mS[:, :], ALU.mult, ALU.mult)
                P.ts(k_b[:, :], ktg[b][:, si, hsl], begcol, None, ALU.mult, eng="pool")
                yield
                X = Xs.next()
                P.tt(X[:, :], bank[:, s0], Da[:, :], ALU.mult)
                P.ts(k_d[:, :], ktg[b][:, si, hsl], eggcol, None, ALU.mult, eng="pool")
                yield
                P.tr(bank[:, s2], X[:, :], identf[:, :])
                P.ts(dtmp[:, :], gsl, gcol, 0.0, ALU.subtract, ALU.min)
                yield
                Y = Ys.next()
                P.act(Y[:, :], bank[:, s2], AF.Copy)
                yield
                P.act(Dt[:, :], dtmp[:, :], AF.Exp)
                Q = Qs.next()
                P.tt(Q[:, :], identf[:, :], Y[:, :], ALU.subtract)
                yield
                P.tt(af[:, :], bank[:, s1], Dt[:, :], ALU.mult)
                yield
                P.tt(ab_[:, :], af[:, :], mI[:, :], ALU.mult, eng="pool")
                for lvl in range(1, 6):
                    P.mm(bank[:, s0], Y[:, :], X[:, :])
                    if lvl < 5:
                        P.mm(bank[:, s1], X[:, :], Y[:, :])
                    yield
                    Xn = Xs.next()
                    P.act(Xn[:, :], bank[:, s0], AF.Copy)
                    yield
                    if lvl < 5:
                        Yn = Ys.next()
                        P.copy(Yn[:, :], bank[:, s1])
                    P.mm(bank[:, s2], Xn[:, :], Q[:, :])
                    yield
                    Qn = Qs.next()
                    P.tt(Qn[:, :], bank[:, s2], Q[:, :], ALU.add)
                    yield
                    X, Q = Xn, Qn
                    if lvl < 5:
                        Y = Yn
                P.copy(tb_[:, :], Q[:, :], eng="pool")
                yield
                P.mm(bank[:, s0], tb_[:, :], v_b[:, :])
                P.mm(bank[:, s1], k_b[:, :], tb_[:, :])
                yield
                P.act(u[:, :], bank[:, s0], AF.Copy)
                yield
                P.copy(wT_[:, :], bank[:, s1])
                yield
                for cc in range(2):
                    pr = slice(cc * 64, (cc + 1) * 64)
                    isl = slice(si * 128 + cc * 64, si * 128 + (cc + 1) * 64)
                    ch = sc * 2 + cc
                    P.mm(bank[pr, s2], wT_[:, pr], sbf[:, :])
                    P.mm(bank[pr, s3], qTg[b][:, hd, isl], sbf[:, :])
                    yield
                    P.tt(vn[pr, :], u[pr, :], bank[pr, s2], ALU.subtract)
                    yield
                    P.mm(bank[pr, s0], ab_[pr, pr], vn[pr, :])
                    P.mm(bank[:, s1], k_d[pr, :], vn[pr, :])
                    yield
                    P.stt(Sst[:, :], Sst[:, :], egl[:, hd, ch:ch + 1], bank[:, s1], ALU.mult, ALU.add)
                    yield
                    sbf = Sbf.next()
                    P.copy(sbf[:, :], Sst[:, :], eng="pool")
                    P.act(t3[pr, :], bank[pr, s0], AF.Copy)
                    yield
                    P.stt(o_[pr, :], bank[pr, s3], V(tokc.t[pr, sc, 4, hd:hd + 1], tokc.res[0]), t3[pr, :],
                          ALU.mult, ALU.add)
                    yield
                P.act(jk[:, :], o_[:, :], AF.Square, accum=s_[:, 0:1])
                yield
                P.act(s_[:, 1:2], s_[:, 0:1], AF.Ln, scale=1.0 / 128.0, bias=C.eps[:, 0:1])
                yield
                P.act(s_[:, 2:3], s_[:, 1:2], AF.Exp, scale=-0.5)
                yield
                P.stt(o_1[:, :], o_[:, :], s_[:, 2:3], gain_bc[:, :], ALU.mult, ALU.mult)
                yield
                P.tt(o_2[:, :], o_1[:, :], sgg[b][:, si, hsl], ALU.mult, eng="pool")
                yield
                P.tr(bank[:, s2], o_2[:, :], identf[:, :])
                yield
                P.copy(hb[:, :], bank[:, s2])
                yield
                P.dma(V(hcT.t[hd * 128:(hd + 1) * 128, sc * 128:(sc + 1) * 128], hcT.res[0]), hb[:, :])
                yield "sc_done"

        chains = [head_chain(hd) for hd in range(8)]
        for ch_ in chains:
            next(ch_)
        load_group(0)
        for sc in range(NSC):
            if (sc * 128) % GT == 0:
                gi = (sc * 128) // GT
                if gi + 1 < NG:
                    load_group(gi + 1)
            active = list(chains)
            while active:
                for ch_ in list(active):
                    if next(ch_) == "sc_done":
                        active.remove(ch_)
        P.barrier()


W_SHAPES = dict(
    mix_norm=[4, D], ab_w_in=[2, D, AB_IN], ab_b_i=[2, 4], ab_b_f=[2, 4], ab_head_gain=[2, 4, 128],
    ab_w_out=[2, D, D], c_w_in=[2, D, C_IN], c_conv_w=[2, 4, 3072], c_a_log=[2, 8], c_dt_bias=[2, 8],
    c_head_gain=[2, 128], c_w_out=[2, D, D], xa_norm=[4, D], mem_norm=[D], xa_wq=[4, D, D], xa_wk=[4, D, D],
    xa_wv=[4, D, D], xa_wo=[4, D, D], mlp_norm=[4, D], mlp_w1=[4, D, DFF], mlp_w2=[4, DFF, D], final_norm=[D])


def build_program(T, depth=DEPTH):
    nc = bass.Bass("TRN2", target_bir_lowering=False)
    P = Prog(nc)
    xT = dram(nc, "xT", [D, T], F32, kind="ExternalInput")
    memT = dram(nc, "memT", [D, MEM], F32, kind="ExternalInput")
    W = {n: nc.dram_tensor(n, shp, F32, kind="ExternalInput").ap() for n, shp in W_SHAPES.items()}
    outT = dram(nc, "outT", [D, T], F32, kind="ExternalOutput")
    xA = dram(nc, "xA", [D, T], F32)
    xB = dram(nc, "xB", [D, T], F32)
    hcT = dram(nc, "hcT", [D, T], BF16)
    memnT = dram(nc, "memnT", [D, MEM], BF16)
    SA = dict(mqT=dram(nc, "mqT", [4, 64, T], BF16), mkT=dram(nc, "mkT", [4, 64, T], BF16), gT=dram(nc, "gT", [8, T], F32),
              sqT=dram(nc, "sqT", [4, 128, T], BF16), skT=dram(nc, "skT", [4, 128, T], BF16),
              mk_tm=dram(nc, "mk_tm", [T, 256], BF16), mv_tm=dram(nc, "mv_tm", [T, 512], BF16),
              sg_tm=dram(nc, "sg_tm", [T, 512], BF16), sv_tm=dram(nc, "sv_tm", [T, 512], BF16))
    SC = dict(qT=dram(nc, "cqT", [8, 128, T], BF16), kT=dram(nc, "ckT", [8, 128, T], BF16),
              k_tm=dram(nc, "ck_tm", [T, D], BF16), v_tm=dram(nc, "cv_tm", [T, D], BF16),
              sg_tm=dram(nc, "csg_tm", [T, D], BF16), bT=dram(nc, "cbT", [8, T], F32), aT=dram(nc, "caT", [8, T], F32))
    SCR = dict(gD=dram(nc, "gD", [8, T], F32), eGD=dram(nc, "eGD", [8, T // 64], F32))
    sec_memn(nc, P, memT, W["mem_norm"], memnT)
    cur = xT
    for l in range(depth):
        j = l // 2
        if l % 2 == 0:
            sec_pre_ab(nc, P, T, cur, W["mix_norm"][l], W["ab_w_in"][j], SA)
            sec_sb(nc, P, T, SA, hcT)
            sec_mlstm(nc, P, T, SA, W["ab_b_i"][j], W["ab_b_f"][j], W["ab_head_gain"][j], hcT)
            w_out = W["ab_w_out"][j]
        else:
            sec_pre_c(nc, P, T, cur, W["mix_norm"][l], W["c_w_in"][j], W["c_conv_w"][j], SC)
            sec_gdn(nc, P, T, SC, W["c_a_log"][j], W["c_dt_bias"][j], W["c_head_gain"][j], hcT, SCR)
            w_out = W["c_w_out"][j]
        sec_post_a(nc, P, T, cur, hcT, xB, w_out, W["xa_norm"][l], W["xa_wq"][l], W["xa_wk"][l], W["xa_wv"][l],
                   W["xa_wo"][l], memnT)
        last = (l == depth - 1)
        sec_post_b(nc, P, T, xB, outT if last else xA, W["mlp_norm"][l], W["mlp_w1"][l], W["mlp_w2"][l],
                   final_norm=W["final_norm"] if last else None)
        cur = xA
    P.finalize()
    return nc, P


def kernel(**inputs):
    x = np.asarray(inputs["x"], dtype=np.float32)
    mem = np.asarray(inputs["mem"], dtype=np.float32)
    B, T, _ = x.shape
    nc, _ = build_program(T)
    wts = {n: np.ascontiguousarray(np.asarray(inputs[n], dtype=np.float32)) for n in W_SHAPES}
    in_maps = []
    for b in range(B):
        m = dict(wts)
        m["xT"] = np.ascontiguousarray(x[b].T)
        m["memT"] = np.ascontiguousarray(mem[b].T)
        in_maps.append(m)
    res = run_bass_kernel_spmd(nc, in_maps, core_ids=list(range(B)))
    out = np.stack([np.ascontiguousarray(r["outT"].T) for r in res.results], axis=0)
    return out.astype(np.float32)
```

```python
import numpy as np
from contextlib import ExitStack
import concourse.bass as bass
import concourse.mybir as mybir
from concourse.bass_utils import run_bass_kernel_spmd

F32 = mybir.dt.float32
BF16 = mybir.dt.bfloat16
AF = mybir.ActivationFunctionType
ALU = mybir.AluOpType

D = 1024
DEPTH = 4
MEM = 256
EPS = 1e-6
AB_IN = 3080
C_IN = 4112
DFF = 4096
LIM = 30000
DLIM = 1800
SAME_ENGINE_SYNC = True


class Res:
    __slots__ = ("w", "r", "x")

    def __init__(self, x=False):
        self.w = {}
        self.r = {}
        self.x = x


class V:
    __slots__ = ("ap", "res")

    def __init__(self, ap, res):
        self.ap = ap
        self.res = res


class Tl:
    def __init__(self, t, nres=1, x=False):
        self.t = t
        self.res = [Res(x) for _ in range(nres)]

    def __getitem__(self, idx):
        return V(self.t[idx], self.res[0])

    def v(self, idx, k=0):
        return V(self.t[idx], self.res[k])


class Prog:
    ENGS = ("pe", "act", "dve", "pool", "sp")

    def __init__(self, nc):
        self.nc = nc
        self.lists = {e: [] for e in self.ENGS}
        self.cnt = {}
        self.seen = {e: {} for e in self.ENGS}

    DMA_SLOTS = {"sp": 24, "pool": 12, "act": 8}

    def op(self, eng, fn, reads=(), writes=(), dma=False):
        if dma:
            tot = self.cnt.get(("dq", eng), 0)
            self.cnt[("dq", eng)] = tot + 1
            key = ("d", eng, tot % self.DMA_SLOTS[eng])
        else:
            key = ("c", eng)
        n = self.cnt.get(key, 0) + 1
        self.cnt[key] = n
        waits = {}
        seen = self.seen[eng]
        if dma and n > 1 and n - 1 > seen.get(key, 0):
            waits[key] = n - 1

        def need(k, v):
            if k[0] == "c" and k[1] == eng and (eng == "pe" or not SAME_ENGINE_SYNC):
                return
            if v > seen.get(k, 0) and v > waits.get(k, 0):
                waits[k] = v

        xr = [r for r in reads if r.res.x]
        if xr:
            writes = tuple(writes) + tuple(xr)
        for r in reads:
            for k, v in r.res.w.items():
                need(k, v)
        for w in writes:
            for k, v in w.res.w.items():
                need(k, v)
            for k, v in w.res.r.items():
                need(k, v)
        for k, v in waits.items():
            seen[k] = v
        self.lists[eng].append((list(waits.items()), fn, key, n))
        for r in reads:
            if r.res.r.get(key, 0) < n:
                r.res.r[key] = n
        for w in writes:
            w.res.w = {key: n}
            w.res.r = {}

    def barrier(self):
        snap = {k: v for k, v in self.cnt.items() if k[0] != "dq"}
        for e in self.ENGS:
            waits = []
            for k, v in snap.items():
                if k[0] == "c" and k[1] == e:
                    continue
                if v > self.seen[e].get(k, 0):
                    waits.append((k, v))
                    self.seen[e][k] = v
            if waits:
                self.lists[e].append((waits, None, None, 0))

    def finalize(self):
        nc = self.nc
        self.barrier()
        with ExitStack() as es:
            sems = {}
            for key, n in self.cnt.items():
                if key[0] == "dq":
                    continue
                lim = DLIM if key[0] == "d" else LIM
                for g in range((n - 1) // lim + 1):
                    sems[(key, g)] = es.enter_context(nc.semaphore("s" + "_".join(str(z) for z in key) + f"_{g}"))
            block = es.enter_context(nc.Block())

            def run(name, eng):
                for waits, fn, key, n in self.lists[name]:
                    for (k, v) in waits:
                        lim = DLIM if k[0] == "d" else LIM
                        g = (v - 1) // lim
                        val = v - g * lim
                        if k[0] == "d":
                            if g > 0:
                                eng.wait_ge(sems[(k, g - 1)], lim * 16)
                            eng.wait_ge(sems[(k, g)], val * 16)
                        else:
                            eng.wait_ge(sems[(k, g)], val)
                    if fn is not None:
                        inst = fn(eng)
                        lim = DLIM if key[0] == "d" else LIM
                        g = (n - 1) // lim
                        inst.then_inc(sems[(key, g)], 16 if key[0] == "d" else 1)

            @block.tensor
            def _(e):
                run("pe", e)

            @block.scalar
            def _(e):
                run("act", e)

            @block.vector
            def _(e):
                run("dve", e)

            @block.gpsimd
            def _(e):
                run("pool", e)

            @block.sync
            def _(e):
                run("sp", e)

    def mm(self, out, lhsT, rhs, start=True, stop=True):
        self.op("pe", lambda e: e.matmul(out.ap, lhsT.ap, rhs.ap, start=start, stop=stop),
                reads=(lhsT, rhs) if start else (lhsT, rhs, out), writes=(out,))

    def tr(self, out, in_, ident):
        self.op("pe", lambda e: e.transpose(out.ap, in_.ap, ident.ap), reads=(in_, ident), writes=(out,))

    def act(self, out, in_, func, scale=1.0, bias=0.0, accum=None, eng="act"):
        rd = [in_]
        if isinstance(scale, V):
            rd.append(scale)
        if isinstance(bias, V):
            rd.append(bias)
        sc = scale.ap if isinstance(scale, V) else scale
        bi = bias.ap if isinstance(bias, V) else bias
        wr = [out]
        if accum is not None:
            wr.append(accum)
        acc = accum.ap if accum is not None else None
        self.op("act", lambda e: e.activation(out.ap, in_.ap, func, bias=bi, scale=sc, accum_out=acc),
                reads=rd, writes=wr)

    def tt(self, out, a, b, op, eng="dve"):
        self.op(eng, lambda e: e.tensor_tensor(out.ap, a.ap, b.ap, op), reads=(a, b), writes=(out,))

    def ts(self, out, a, s1, s2, op0, op1=None, eng="dve", accum=None):
        rd = [a]
        if isinstance(s1, V):
            rd.append(s1)
        if isinstance(s2, V):
            rd.append(s2)
        v1 = s1.ap if isinstance(s1, V) else s1
        v2 = s2.ap if isinstance(s2, V) else s2
        wr = [out]
        if accum is not None:
            wr.append(accum)
        acc = accum.ap if accum is not None else None
        if op1 is None:
            self.op(eng, lambda e: e.tensor_scalar(out.ap, a.ap, v1, v2, op0, accum_out=acc) if acc is not None
                    else e.tensor_scalar(out.ap, a.ap, v1, v2, op0), reads=rd, writes=wr)
        else:
            self.op(eng, lambda e: e.tensor_scalar(out.ap, a.ap, v1, v2, op0, op1, accum_out=acc) if acc is not None
                    else e.tensor_scalar(out.ap, a.ap, v1, v2, op0, op1), reads=rd, writes=wr)

    def stt(self, out, a, s, b, op0, op1):
        rd = [a, b]
        if isinstance(s, V):
            rd.append(s)
        sv = s.ap if isinstance(s, V) else s
        self.op("dve", lambda e: e.scalar_tensor_tensor(out.ap, a.ap, sv, b.ap, op0, op1), reads=rd, writes=(out,))

    def scan(self, out, d0, d1, initial, op0, op1):
        rd = [d0, d1]
        if isinstance(initial, V):
            rd.append(initial)
        iv = initial.ap if isinstance(initial, V) else initial
        self.op("dve", lambda e: e.tensor_tensor_scan(out.ap, d0.ap, d1.ap, iv, op0, op1), reads=rd, writes=(out,))

    def copy(self, out, in_, eng="dve"):
        self.op(eng, lambda e: e.tensor_copy(out.ap, in_.ap), reads=(in_,), writes=(out,))

    def recip(self, out, in_):
        self.op("dve", lambda e: e.reciprocal(out.ap, in_.ap), reads=(in_,), writes=(out,))

    def memset(self, out, val, eng="dve"):
        self.op(eng, lambda e: e.memset(out.ap, val), writes=(out,))

    def dma(self, out, in_, q="sp", **kw):
        self.op(q, lambda e: e.dma_start(out=out.ap, in_=in_.ap, **kw), reads=(in_,), writes=(out,), dma=True)


class Ctx:
    _uid = [0]

    def __init__(self, nc, P, es):
        self.nc, self.P, self.es = nc, P, es
        self.n = 0
        Ctx._uid[0] += 1
        self.uid = Ctx._uid[0]

    def sb(self, shape, dt, nres=1, name=None):
        self.n += 1
        t = self.es.enter_context(self.nc.sbuf_tensor(f"{name or 't'}_{self.uid}_{self.n}", list(shape), dt))
        return Tl(t, nres)

    def ps(self, shape, dt=F32, name=None):
        self.n += 1
        assert shape[0] == 128 and shape[1] * (4 if dt == F32 else 2) == 2048, "PSUM tiles are whole banks"
        t = self.es.enter_context(self.nc.psum_tensor(f"{name or 'p'}_{self.uid}_{self.n}", list(shape), dt))
        return Tl(t, x=True)


class Rot:
    def __init__(self, items):
        self.items = items
        self.i = 0

    def next(self):
        t = self.items[self.i % len(self.items)]
        self.i += 1
        return t


def dram(nc, name, shape, dt, kind=None):
    if kind is None:
        t = nc.dram_tensor(name, list(shape), dt)
    else:
        t = nc.dram_tensor(name, list(shape), dt, kind=kind)
    return Tl(t.ap())


def load_w(P, C, w_dram_ap, K, N, name, q="pool", res=None):
    kc = K // 128
    w = C.sb([128, kc, N], BF16, name=name)
    src = w_dram_ap.rearrange("(c p) n -> p c n", p=128)
    step = 2048
    for c in range(kc):
        for n0 in range(0, N, step):
            n1 = min(N, n0 + step)
            P.dma(V(w.t[:, c, n0:n1], w.res[0]), V(src[:, c, n0:n1], res or Res()), q=q)
    return w


def rmsnorm_tile(P, C, x, g, h, N, ones, ps_rot, sq, rstd, lnt):
    if isinstance(sq, Tl):
        P.act(sq[:, :, :], x[:, :, :], AF.Square)
        sq = [sq[:, c, :] for c in range(8)]
    else:
        for c in range(8):
            P.act(sq[c], x[:, c, :], AF.Square)
    ps = ps_rot.next()
    for c in range(8):
        P.mm(ps[:, 0:N], ones[:, :], sq[c], start=(c == 0), stop=(c == 7))
    P.act(lnt[:, :], ps[:, 0:N], AF.Ln, scale=1.0 / D, bias=C.eps[:, 0:1])
    P.act(rstd[:, :], lnt[:, :], AF.Exp, scale=-0.5)
    for c in range(8):
        P.stt(h[:, c, :], x[:, c, :], g[:, c:c + 1], rstd[:, :], ALU.mult, ALU.mult)


def setup_consts(P, C):
    C.ones = C.sb([128, 128], BF16, name="ones")
    P.memset(C.ones[:, :], 1.0)
    C.eps = C.sb([128, 1], F32, name="eps")
    P.memset(C.eps[:, :], EPS)


def load_gain(P, C, g_dram_row_ap, name):
    g = C.sb([128, 8], F32, name=name)
    P.op("sp", lambda e: e.dma_start(out=g.t[:, :], in_=g_dram_row_ap.rearrange("(c p) -> p c", p=128),
                                     allow_slow_non_contiguous=True), writes=(g[:, :],), dma=True)
    return g


def sec_memn(nc, P, memT, mem_norm, memnT_out):
    with ExitStack() as es:
        C = Ctx(nc, P, es)
        setup_consts(P, C)
        g = load_gain(P, C, mem_norm, "gmem")
        x = C.sb([128, 8, MEM], F32)
        P.dma(x[:, :, :], V(memT.t.rearrange("(c p) t -> p c t", p=128), memT.res[0]))
        sq = C.sb([128, 8, MEM], BF16)
        rstd = C.sb([128, MEM], F32)
        lnt = C.sb([128, MEM], F32)
        h = C.sb([128, 8, MEM], BF16)
        ps = C.ps([128, 512])
        rmsnorm_tile(P, C, x, g, h, MEM, C.ones, Rot([ps]), sq, rstd, lnt)
        P.dma(V(memnT_out.t.rearrange("(c p) t -> p c t", p=128), memnT_out.res[0]), h[:, :, :])
        P.barrier()


def sec_post_a(nc, P, T, xin, hc, xout, w_out, xa_norm, wq, wk, wv, wo, memnT, N=512):
    with ExitStack() as es:
        C = Ctx(nc, P, es)
        setup_consts(P, C)
        g = load_gain(P, C, xa_norm, "gxa")
        wout_sb = load_w(P, C, w_out, D, D, "wout")
        wq_sb = load_w(P, C, wq, D, D, "wq")
        wo_sb = load_w(P, C, wo, D, D, "wo")
        kT = C.sb([128, 8, MEM], BF16, name="kT")
        v_sb = C.sb([128, 2, D], BF16, name="vsb")
        pss = Rot([C.ps([128, 512]) for _ in range(8)])
        with ExitStack() as es2:
            C2 = Ctx(nc, P, es2)
            wk_sb = load_w(P, C2, wk, D, D, "wk")
            wv_sb = load_w(P, C2, wv, D, D, "wv")
            mn = C2.sb([128, 8, MEM], BF16, name="mn")
            P.dma(mn[:, :, :], V(memnT.t.rearrange("(c p) t -> p c t", p=128), memnT.res[0]))
            for o in range(8):
                ps = pss.next()
                for c in range(8):
                    P.mm(ps[:, 0:MEM], wk_sb[:, c, o * 128:(o + 1) * 128], mn[:, c, :], start=(c == 0), stop=(c == 7))
                P.act(kT[:, o, :], ps[:, 0:MEM], AF.Copy)
            for mb in range(2):
                for half in range(2):
                    ps = pss.next()
                    for c in range(8):
                        P.mm(ps[:, :], mn[:, c, mb * 128:(mb + 1) * 128], wv_sb[:, c, half * 512:(half + 1) * 512],
                             start=(c == 0), stop=(c == 7))
                    P.copy(v_sb[:, mb, half * 512:(half + 1) * 512], ps[:, :])
            P.barrier()
        xin_v = xin.t.rearrange("(c p) t -> p c t", p=128)
        hc_v = hc.t.rearrange("(c p) t -> p c t", p=128)
        xout_v = xout.t.rearrange("(c p) t -> p c t", p=128)
        NCHAIN = 2

        def tile_chain(k):
            x = C.sb([128, 8, N], F32, name=f"x{k}")
            hcx = C.sb([128, 8, N], BF16, name=f"hc{k}")
            sq = C.sb([128, 8, N], BF16, name=f"sq{k}")
            h = C.sb([128, 8, N], BF16, name=f"h{k}")
            qs = C.sb([128, 8, N], BF16, name=f"q{k}", nres=8)
            os_ = C.sb([128, 8, N], BF16, name=f"o{k}", nres=8)
            rstd = C.sb([128, N], F32, name=f"rstd{k}")
            lnt = C.sb([128, N], F32, name=f"lnt{k}")
            pT = [C.sb([128, N], BF16, name=f"pT{k}") for _ in range(4)]
            rden = [C.sb([128, N], F32, name=f"rden{k}") for _ in range(2)]
            yield
            for it in range(k, T // N, NCHAIN):
                t0 = it * N
                P.dma(x[:, :, :], V(xin_v[:, :, t0:t0 + N], xin.res[0]))
                P.dma(hcx[:, :, :], V(hc_v[:, :, t0:t0 + N], hc.res[0]))
                for o in range(8):
                    ps = pss.next()
                    for c in range(8):
                        P.mm(ps[:, 0:N], wout_sb[:, c, o * 128:(o + 1) * 128], hcx[:, c, :], start=(c == 0), stop=(c == 7))
                    P.tt(x[:, o, :], x[:, o, :], ps[:, 0:N], ALU.add)
                    yield
                P.act(sq[:, :, :], x[:, :, :], AF.Square)
                yield
                ps = pss.next()
                for c in range(8):
                    P.mm(ps[:, 0:N], C.ones[:, :], sq[:, c, :], start=(c == 0), stop=(c == 7))
                P.act(lnt[:, :], ps[:, 0:N], AF.Ln, scale=1.0 / D, bias=C.eps[:, 0:1])
                yield
                P.act(rstd[:, :], lnt[:, :], AF.Exp, scale=-0.5)
                yield
                for c in range(8):
                    P.stt(h[:, c, :], x[:, c, :], g[:, c:c + 1], rstd[:, :], ALU.mult, ALU.mult)
                    if c % 2 == 1:
                        yield
                for o in range(8):
                    ps = pss.next()
                    for c in range(8):
                        P.mm(ps[:, 0:N], wq_sb[:, c, o * 128:(o + 1) * 128], h[:, c, :], start=(c == 0), stop=(c == 7))
                    P.act(qs.v((slice(None), o, slice(None)), o), ps[:, 0:N], AF.Copy, scale=1.0 / 16.0)
                    yield
                for hd in range(4):
                    pts = []
                    for mb in range(2):
                        ps = pss.next()
                        for j in range(2):
                            P.mm(ps[:, 0:N], kT[:, 2 * hd + j, mb * 128:(mb + 1) * 128],
                                 qs.v((slice(None), 2 * hd + j, slice(None)), 2 * hd + j), start=(j == 0), stop=(j == 1))
                        pt = pT[(2 * hd + mb) % 4]
                        P.act(pt[:, :], ps[:, 0:N], AF.Exp)
                        pts.append(pt)
                    yield
                    ps = pss.next()
                    for mb in range(2):
                        P.mm(ps[:, 0:N], C.ones[:, :], pts[mb][:, :], start=(mb == 0), stop=(mb == 1))
                    rd = rden[hd % 2]
                    P.recip(rd[:, :], ps[:, 0:N])
                    yield
                    for j in range(2):
                        ps = pss.next()
                        for mb in range(2):
                            P.mm(ps[:, 0:N], v_sb[:, mb, (2 * hd + j) * 128:(2 * hd + j + 1) * 128], pts[mb][:, :],
                                 start=(mb == 0), stop=(mb == 1))
                        P.tt(os_.v((slice(None), 2 * hd + j, slice(None)), 2 * hd + j), ps[:, 0:N], rd[:, :], ALU.mult)
                    yield
                for o in range(8):
                    ps = pss.next()
                    for c in range(8):
                        P.mm(ps[:, 0:N], wo_sb[:, c, o * 128:(o + 1) * 128], os_.v((slice(None), c, slice(None)), c),
                             start=(c == 0), stop=(c == 7))
                    P.tt(x[:, o, :], x[:, o, :], ps[:, 0:N], ALU.add)
                    yield
                P.dma(V(xout_v[:, :, t0:t0 + N], xout.res[0]), x[:, :, :], q="sp")
                yield

        chains = [tile_chain(k) for k in range(NCHAIN)]
        for ch_ in chains:
            next(ch_)
        active = list(chains)
        stagger = 20
        steps = 0
        while active:
            for idx, ch_ in enumerate(list(active)):
                if ch_ is chains[1] and steps < stagger:
                    continue
                try:
                    next(ch_)
                except StopIteration:
                    active.remove(ch_)
            steps += 1
        P.barrier()


def sec_post_b(nc, P, T, xin, xout, mlp_norm, w1, w2, final_norm=None, N=512):
    with ExitStack() as es:
        C = Ctx(nc, P, es)
        setup_consts(P, C)
        g = load_gain(P, C, mlp_norm, "gmlp")
        gf = load_gain(P, C, final_norm, "gfin") if final_norm is not None else None
        w1_sb = load_w(P, C, w1, D, DFF, "w1")
        w2_sb = load_w(P, C, w2, DFF, D, "w2")
        pss = Rot([C.ps([128, 512]) for _ in range(8)])
        xs = [C.sb([128, 8, N], F32, name="x") for _ in range(1)]
        h = C.sb([128, 8, N], BF16, name="h")
        gg = C.sb([128, 32, N], BF16, name="gg", nres=32)
        sq = [gg.v((slice(None), 24 + c, slice(None)), 24 + c) for c in range(8)]
        rr = [C.sb([128, N], F32, name="rr") for _ in range(3)]
        rstd = C.sb([128, N], F32, name="rstd")
        lnt = C.sb([128, N], F32, name="lnt")
        xin_v = xin.t.rearrange("(c p) t -> p c t", p=128)
        xout_v = xout.t.rearrange("(c p) t -> p c t", p=128)
        for it in range(T // N):
            t0 = it * N
            x = xs[0]
            P.dma(x[:, :, :], V(xin_v[:, :, t0:t0 + N], xin.res[0]))
            rmsnorm_tile(P, C, x, g, h, N, C.ones, pss, sq, rstd, lnt)
            for f in range(32):
                ps = pss.next()
                for c in range(8):
                    P.mm(ps[:, 0:N], w1_sb[:, c, f * 128:(f + 1) * 128], h[:, c, :], start=(c == 0), stop=(c == 7))
                r = rr[f % 3]
                P.act(r[:, :], ps[:, 0:N], AF.Relu)
                P.tt(gg.v((slice(None), f, slice(None)), f), r[:, :], r[:, :], ALU.mult, eng="pool")
            for o in range(8):
                ps = pss.next()
                for f in range(32):
                    P.mm(ps[:, 0:N], w2_sb[:, f, o * 128:(o + 1) * 128], gg.v((slice(None), f, slice(None)), f),
                         start=(f == 0), stop=(f == 31))
                P.tt(x[:, o, :], x[:, o, :], ps[:, 0:N], ALU.add)
            if gf is not None:
                for c in range(8):
                    P.act(sq[c], x[:, c, :], AF.Square)
                ps = pss.next()
                for c in range(8):
                    P.mm(ps[:, 0:N], C.ones[:, :], sq[c], start=(c == 0), stop=(c == 7))
                P.act(lnt[:, :], ps[:, 0:N], AF.Ln, scale=1.0 / D, bias=C.eps[:, 0:1])
                P.act(rstd[:, :], lnt[:, :], AF.Exp, scale=-0.5)
                for c in range(8):
                    P.stt(x[:, c, :], x[:, c, :], gf[:, c:c + 1], rstd[:, :], ALU.mult, ALU.mult)
            P.dma(V(xout_v[:, :, t0:t0 + N], xout.res[0]), x[:, :, :], q="sp")
        P.barrier()


def sec_pre_ab(nc, P, T, xin, mix_norm, w_in, S, N=512):
    with ExitStack() as es:
        C = Ctx(nc, P, es)
        setup_consts(P, C)
        g = load_gain(P, C, mix_norm, "gmix")
        w = load_w(P, C, w_in, D, AB_IN, "win")
        pss = Rot([C.ps([128, 512]) for _ in range(8)])
        xs = [C.sb([128, 8, N], F32, name="x") for _ in range(2)]
        sq = C.sb([128, 8, N], BF16, name="sq")
        h = C.sb([128, 8, N], BF16, name="h")
        rstd = C.sb([128, N], F32, name="rstd")
        lnt = C.sb([128, N], F32, name="lnt")
        mq_sb = [C.sb([64, 4, N], BF16, name="mq") for _ in range(2)]
        mk_sb = [C.sb([64, 4, N], BF16, name="mk") for _ in range(2)]
        g_sb = [C.sb([8, N], F32, name="gs") for _ in range(2)]
        sq_sb = [C.sb([128, 4, N], BF16, name="sqs") for _ in range(2)]
        sk_sb = [C.sb([128, 4, N], BF16, name="sks") for _ in range(2)]
        tmk = [C.sb([128, 256], BF16, name="tmk") for _ in range(2)]
        tmv = [C.sb([128, 512], BF16, name="tmv") for _ in range(2)]
        tsv = [C.sb([128, 512], BF16, name="tsv") for _ in range(2)]
        tsg = [C.sb([128, 512], BF16, name="tsg") for _ in range(2)]
        ex = [C.sb([128, 512], F32, name="ex") for _ in range(2)]
        xin_v = xin.t.rearrange("(c p) t -> p c t", p=128)
        for it in range(T // N):
            t0 = it * N
            b = it % 2
            x = xs[b]
            P.dma(x[:, :, :], V(xin_v[:, :, t0:t0 + N], xin.res[0]))
            rmsnorm_tile(P, C, x, g, h, N, C.ones, pss, sq, rstd, lnt)

            def fm(col0, M, dst, scale=1.0):
                ps = pss.next()
                for c in range(8):
                    P.mm(ps[0:M, 0:N], w[:, c, col0:col0 + M], h[:, c, :], start=(c == 0), stop=(c == 7))
                P.act(dst, ps[0:M, 0:N], AF.Copy, scale=scale)

            for hd in range(4):
                fm(64 * hd, 64, mq_sb[b][:, hd, :])
                fm(256 + 64 * hd, 64, mk_sb[b][:, hd, :], 0.125)
                fm(1544 + 128 * hd, 128, sq_sb[b][:, hd, :], 128 ** -0.5)
                fm(2056 + 128 * hd, 128, sk_sb[b][:, hd, :])
            fm(1536, 8, g_sb[b][:, :])
            P.dma(V(S["mqT"].t.rearrange("h d t -> d h t")[:, :, t0:t0 + N], S["mqT"].res[0]), mq_sb[b][:, :, :])
            P.dma(V(S["mkT"].t.rearrange("h d t -> d h t")[:, :, t0:t0 + N], S["mkT"].res[0]), mk_sb[b][:, :, :])
            P.dma(V(S["sqT"].t.rearrange("h d t -> d h t")[:, :, t0:t0 + N], S["sqT"].res[0]), sq_sb[b][:, :, :])
            P.dma(V(S["skT"].t.rearrange("h d t -> d h t")[:, :, t0:t0 + N], S["skT"].res[0]), sk_sb[b][:, :, :])
            P.dma(V(S["gT"].t[:, t0:t0 + N], S["gT"].res[0]), g_sb[b][:, :])
            for tb in range(N // 128):
                r0 = t0 + tb * 128
                bb = tb % 2

                def tm(col0, W):
                    ps = pss.next()
                    for c in range(8):
                        P.mm(ps[:, 0:W], h[:, c, tb * 128:(tb + 1) * 128], w[:, c, col0:col0 + W],
                             start=(c == 0), stop=(c == 7))
                    return ps

                ps = tm(256, 256)
                P.act(tmk[bb][:, :], ps[:, 0:256], AF.Copy, scale=0.125)
                P.dma(V(S["mk_tm"].t[r0:r0 + 128, :], S["mk_tm"].res[0]), tmk[bb][:, :])
                ps = tm(512, 512)
                P.copy(tmv[bb][:, :], ps[:, :])
                P.dma(V(S["mv_tm"].t[r0:r0 + 128, :], S["mv_tm"].res[0]), tmv[bb][:, :])
                ps = tm(2568, 512)
                P.copy(tsv[bb][:, :], ps[:, :])
                P.dma(V(S["sv_tm"].t[r0:r0 + 128, :], S["sv_tm"].res[0]), tsv[bb][:, :])
                ps = tm(1024, 512)
                P.act(ex[bb][:, :], ps[:, :], AF.Exp, scale=-1.0)
                P.ts(ex[bb][:, :], ex[bb][:, :], 1.0, None, ALU.add)
                P.recip(ex[bb][:, :], ex[bb][:, :])
                P.copy(tsg[bb][:, :], ex[bb][:, :], eng="pool")
                P.dma(V(S["sg_tm"].t[r0:r0 + 128, :], S["sg_tm"].res[0]), tsg[bb][:, :])
        P.barrier()


def make_mask(P, C, shape, pattern, base, cm, op, val=1.0, dt=BF16, name="mask"):
    src = C.sb(shape, F32, name=name + "s")
    P.memset(src[:, :], val, eng="pool")
    m = C.sb(shape, dt, name=name)
    P.op("pool", lambda e: e.affine_select(m.t[:, :], src.t[:, :], pattern, op, 0.0, base=base, channel_multiplier=cm),
         reads=(src[:, :],), writes=(m[:, :],))
    return m


def sec_sb(nc, P, T, S, hcT, N=512):
    NB = T // 128
    QB = N // 128
    with ExitStack() as es:
        C = Ctx(nc, P, es)
        setup_consts(P, C)
        one_col = C.sb([128, 1], F32, name="onec")
        P.memset(one_col[:, :], 1.0)
        negtri = make_mask(P, C, [128, 128], [[-1, 128]], 0, 1, ALU.is_ge, val=-1.0, name="ntri")
        negones = C.sb([128, 128], BF16, name="nones")
        P.memset(negones[:, :], -1.0)
        masks = [make_mask(P, C, [128, N], [[1, N]], -r * 128, -1, ALU.is_gt, name=f"m{r}") for r in range(QB)]
        KT = [C.sb([128, T], BF16, name="KT") for _ in range(2)]
        QT = [C.sb([128, T], BF16, name="QT") for _ in range(2)]
        Vv = [C.sb([128, NB, 128], BF16, name="Vv") for _ in range(2)]
        NSLOT = 4
        bankAB = [C.ps([128, 512]) for _ in range(NSLOT)]
        bankO = [C.ps([128, 512]) for _ in range(NSLOT)]
        ezs = [C.sb([128, N], F32, name="ez") for _ in range(NSLOT)]
        sps = [Rot([C.sb([128, N], BF16, name="sp") for _ in range(2)]) for _ in range(NSLOT)]
        wts = [Rot([C.sb([128, N], BF16, name="wt") for _ in range(2)]) for _ in range(NSLOT)]
        Sfs = [C.sb([128, N], F32, name="Sf") for _ in range(NSLOT)]
        Sbs = [Rot([C.sb([128, N], BF16, name="Sb") for _ in range(2)]) for _ in range(NSLOT)]
        obs = [C.sb([128, N], BF16, name="ob") for _ in range(NSLOT)]
        loaded = set()

        def load_head(hd):
            b = hd % 2
            P.dma(KT[b][:, :], V(S["skT"].t[hd, :, :], S["skT"].res[0]))
            P.dma(QT[b][:, :], V(S["sqT"].t[hd, :, :], S["sqT"].res[0]))
            P.dma(Vv[b][:, :, :], V(S["sv_tm"].t.rearrange("(j p) c -> p j c", p=128)[:, :, hd * 128:(hd + 1) * 128],
                                   S["sv_tm"].res[0]))

        def chain(slot, hd, gq):
            b = hd % 2
            q = QT[b][:, gq * N:(gq + 1) * N]
            jmax = gq * QB + QB - 1
            pab, po = bankAB[slot], bankO[slot]
            e, Sf, o = ezs[slot], Sfs[slot], obs[slot]
            sb_prev = None
            for j in range(jmax, -1, -1):
                k = KT[b][:, j * 128:(j + 1) * 128]
                P.mm(pab[:, 0:N], k, q)
                yield
                P.act(e[:, :], pab[:, 0:N], AF.Exp)
                yield
                s_ = sps[slot].next()
                P.act(s_[:, :], e[:, :], AF.Ln, bias=one_col[:, 0:1])
                yield
                r = j - gq * QB
                if r >= 0:
                    P.tt(s_[:, :], s_[:, :], masks[r][:, :], ALU.mult, eng="pool")
                    yield
                P.mm(pab[:, 0:N], negtri[:, :], s_[:, :], start=True, stop=False)
                if sb_prev is not None:
                    P.mm(pab[:, 0:N], negones[:, :], sb_prev[:, :], start=False, stop=False)
                P.mm(pab[:, 0:N], k, q, start=False, stop=True)
                yield
                w = wts[slot].next()
                P.act(w[:, :], pab[:, 0:N], AF.Exp)
                yield
                if r >= 0:
                    P.tt(w[:, :], w[:, :], masks[r][:, :], ALU.mult, eng="pool")
                    yield
                P.mm(po[:, 0:N], Vv[b][:, j, :], w[:, :], start=(j == jmax), stop=(j == 0))
                if j > 0:
                    if j == jmax:
                        P.copy(Sf[:, :], s_[:, :])
                    else:
                        P.tt(Sf[:, :], Sf[:, :], s_[:, :], ALU.add)
                    yield
                    sb_prev = Sbs[slot].next()
                    P.copy(sb_prev[:, :], Sf[:, :])
                yield
            P.copy(o[:, :], po[:, 0:N])
            yield
            P.dma(V(hcT.t[512 + hd * 128:512 + (hd + 1) * 128, gq * N:(gq + 1) * N], hcT.res[0]), o[:, :])

        work = [(hd, gq) for hd in range(4) for gq in range(T // N - 1, -1, -1)]
        slots = [None] * NSLOT
        slot_head = [None] * NSLOT
        wi = 0
        while True:
            busy = False
            for sl_ in range(NSLOT):
                if slots[sl_] is None and wi < len(work):
                    hd, gq = work[wi]
                    if not any(slots[z] is not None and slot_head[z] == hd - 2 for z in range(NSLOT)):
                        wi += 1
                        if hd not in loaded:
                            load_head(hd)
                            loaded.add(hd)
                        slots[sl_] = chain(sl_, hd, gq)
                        slot_head[sl_] = hd
                if slots[sl_] is not None:
                    busy = True
                    try:
                        next(slots[sl_])
                    except StopIteration:
                        slots[sl_] = None
            if not busy and wi >= len(work):
                break
        P.barrier()


def sec_mlstm(nc, P, T, S, b_i, b_f, head_gain, hcT):
    NCH = T // 128
    with ExitStack() as es:
        C = Ctx(nc, P, es)
        setup_consts(P, C)
        one4 = C.sb([4, 1], F32, name="one4")
        P.memset(one4[:, :], 1.0)
        ident = make_mask(P, C, [128, 128], [[-1, 128]], 0, 1, ALU.is_equal, dt=F32, name="identf")
        identb = C.sb([128, 128], BF16, name="identb")
        P.copy(identb[:, :], ident[:, :])
        maskLT = make_mask(P, C, [128, 128], [[1, 128]], 0, -1, ALU.is_ge, name="mlt")
        gain_bc = C.sb([128, 512], F32, name="gainbc")
        P.op("sp", lambda e: e.dma_start(out=gain_bc.t[:, :],
                                         in_=head_gain.rearrange("h v -> (h v)").partition_broadcast(128)),
             writes=(gain_bc[:, :],), dma=True)
        tok = C.sb([128, NCH, 12], F32, name="tok")
        with ExitStack() as es2:
            C2 = Ctx(nc, P, es2)
            mi = C2.sb([4, T], F32, name="mi")
            mf = C2.sb([4, T], F32, name="mf")
            P.dma(mi[:, :], V(S["gT"].t[0:4, :], S["gT"].res[0]))
            P.dma(mf[:, :], V(S["gT"].t[4:8, :], S["gT"].res[0]))
            bi = C2.sb([4, 1], F32, name="bi")
            bf = C2.sb([4, 1], F32, name="bf")
            P.op("sp", lambda e: e.dma_start(out=bi.t[:, :], in_=b_i.rearrange("(h o) -> h o", o=1)),
                 writes=(bi[:, :],), dma=True)
            P.op("sp", lambda e: e.dma_start(out=bf.t[:, :], in_=b_f.rearrange("(h o) -> h o", o=1)),
                 writes=(bf[:, :],), dma=True)
            P.ts(bi[:, :], bi[:, :], 1.0 / 15.0, None, ALU.mult)
            P.ts(bf[:, :], bf[:, :], -1.0, None, ALU.mult)
            t1 = C2.sb([4, T], F32, name="t1")
            P.act(t1[:, :], mi[:, :], AF.Tanh, scale=1.0 / 15.0, bias=bi[:, 0:1])
            e1 = C2.sb([4, T], F32, name="e1")
            P.act(e1[:, :], mf[:, :], AF.Exp, scale=-1.0, bias=bf[:, 0:1])
            P.act(e1[:, :], e1[:, :], AF.Ln, bias=one4[:, 0:1])
            P.ts(e1[:, :], e1[:, :], -1.0, None, ALU.mult)
            ones_r = mf
            P.memset(ones_r[:, :], 1.0)
            Fc = C2.sb([4, T], F32, name="Fc")
            P.scan(Fc[:, :], ones_r[:, :], e1[:, :], 0.0, ALU.mult, ALU.add)
            a = mi
            P.stt(a[:, :], t1[:, :], 15.0, Fc[:, :], ALU.mult, ALU.subtract)
            M = t1
            P.scan(M[:, :], a[:, :], a[:, :], 0.0, ALU.max, ALU.max)
            Er = C2.sb([4, T], F32, name="Er")
            Dr = e1
            Gr = ones_r
            gd = C2.sb([4, NCH], F32, name="gd")
            for c in range(NCH):
                sl = slice(c * 128, (c + 1) * 128)
                me = M[:, c * 128 + 127:c * 128 + 128]
                P.ts(Er[:, sl], a[:, sl], me, None, ALU.subtract)
                P.ts(Dr[:, sl], Fc[:, sl], -1.0, me, ALU.mult, ALU.subtract)
                if c == 0:
                    P.ts(gd[:, 0:1], me, -1.0, None, ALU.mult)
                else:
                    P.tt(gd[:, c:c + 1], M[:, c * 128 - 1:c * 128], me, ALU.subtract)
                P.ts(Gr[:, sl], Fc[:, sl], 0.0, gd[:, c:c + 1], ALU.mult, ALU.add)
            P.act(Er[:, :], Er[:, :], AF.Exp)
            P.act(Dr[:, :], Dr[:, :], AF.Exp, scale=2.0)
            P.act(Gr[:, :], Gr[:, :], AF.Exp)
            pst = Rot([C2.ps([128, 512]) for _ in range(2)])
            for c in range(NCH):
                sl = slice(c * 128, (c + 1) * 128)
                ps = pst.next()
                for i, rw in enumerate((Er, Dr, Gr)):
                    P.tr(ps[:, 4 * i:4 * i + 4], rw[:, sl], ident[0:4, 0:4])
                P.copy(tok[:, c, :], ps[:, 0:12])
            P.barrier()
        qT = [C.sb([64, 4, 128], BF16, name="qT") for _ in range(2)]
        kT = [C.sb([64, 4, 128], BF16, name="kT") for _ in range(2)]
        ktm = [C.sb([128, 256], BF16, name="ktm") for _ in range(2)]
        vtm = [C.sb([128, 512], BF16, name="vtm") for _ in range(2)]
        sgt = [C.sb([128, 512], BF16, name="sgt") for _ in range(2)]
        hm = [C.sb([128, 4, 128], BF16, name="hm", nres=4) for _ in range(2)]
        bankA = [C.ps([128, 512]) for _ in range(4)]
        bankB = [C.ps([128, 512]) for _ in range(4)]

        def load_chunk(c):
            b = c % 2
            sl = slice(c * 128, (c + 1) * 128)
            P.dma(qT[b][:, :, :], V(S["mqT"].t.rearrange("h d t -> d h t")[:, :, sl], S["mqT"].res[0]))
            P.dma(kT[b][:, :, :], V(S["mkT"].t.rearrange("h d t -> d h t")[:, :, sl], S["mkT"].res[0]))
            P.dma(ktm[b][:, :], V(S["mk_tm"].t[sl, :], S["mk_tm"].res[0]))
            P.dma(vtm[b][:, :], V(S["mv_tm"].t[sl, :], S["mv_tm"].res[0]))
            P.dma(sgt[b][:, :], V(S["sg_tm"].t[sl, :], S["sg_tm"].res[0]))

        def head_chain(hd):
            pa, pb = bankA[hd], bankB[hd]
            Cst = C.sb([64, 129], F32, name=f"Cst{hd}")
            Cbf = C.sb([64, 129], BF16, name=f"Cbf{hd}")
            s_m = C.sb([128, 128], BF16, name=f"sm{hd}")
            v_e = C.sb([128, 129], BF16, name=f"vt{hd}")
            jk = C.sb([128, 128], BF16, name=f"jk{hd}")
            s_ = C.sb([128, 8], F32, name=f"sc{hd}")
            o_1 = C.sb([128, 128], F32, name=f"o1{hd}")
            o_2 = C.sb([128, 128], F32, name=f"o2{hd}")
            P.memset(Cst[:, :], 0.0)
            yield
            for c in range(NCH):
                b = c % 2
                ecol = tok[:, c, hd:hd + 1]
                dcol = tok[:, c, 4 + hd:5 + hd]
                gcol = tok[0:64, c, 8 + hd:9 + hd]
                P.mm(pa[:, 0:128], kT[b][:, hd, :], qT[b][:, hd, :])
                P.ts(v_e[:, 0:128], vtm[b][:, hd * 128:(hd + 1) * 128], ecol, None, ALU.mult, eng="pool")
                yield
                P.tt(s_m[:, :], pa[:, 0:128], maskLT[:, :], ALU.mult)
                P.copy(v_e[:, 128:129], ecol, eng="pool")
                yield
                P.ts(Cbf[:, :], Cst[:, :], gcol, None, ALU.mult)
                yield
                P.mm(pb[:, 0:129], s_m[:, :], v_e[:, :], start=True, stop=False)
                P.mm(pb[:, 0:129], qT[b][:, hd, :], Cbf[:, :], start=False, stop=True)
                P.mm(pa[0:64, 128:257], ktm[b][:, hd * 64:(hd + 1) * 64], v_e[:, :])
                yield
                P.stt(Cst[:, :], Cst[:, :], gcol, pa[0:64, 128:257], ALU.mult, ALU.add)
                P.act(jk[:, :], pb[:, 0:128], AF.Square, accum=s_[:, 0:1])
                yield
                P.copy(s_[:, 1:2], pb[:, 128:129])
                yield
                P.ts(s_[:, 2:3], s_[:, 1:2], s_[:, 1:2], None, ALU.mult)
                yield
                P.ts(s_[:, 2:3], s_[:, 2:3], dcol, EPS * 128.0, ALU.max, ALU.mult)
                yield
                P.tt(s_[:, 3:4], s_[:, 2:3], s_[:, 0:1], ALU.add)
                yield
                P.act(s_[:, 4:5], s_[:, 3:4], AF.Ln, scale=1.0 / 128.0)
                yield
                P.act(s_[:, 5:6], s_[:, 4:5], AF.Exp, scale=-0.5)
                yield
                P.stt(o_1[:, :], pb[:, 0:128], s_[:, 5:6], gain_bc[:, hd * 128:(hd + 1) * 128], ALU.mult, ALU.mult)
                yield
                P.tt(o_2[:, :], o_1[:, :], sgt[b][:, hd * 128:(hd + 1) * 128], ALU.mult, eng="pool")
                yield
                P.tr(pa[:, 384:512], o_2[:, :], ident[:, :])
                yield
                P.copy(hm[b].v((slice(None), hd, slice(None)), hd), pa[:, 384:512])
                yield "chunk_done"

        chains = [head_chain(hd) for hd in range(4)]
        for ch_ in chains:
            next(ch_)
        load_chunk(0)
        for c in range(NCH):
            if c + 1 < NCH:
                load_chunk(c + 1)
            active = list(chains)
            while active:
                for ch_ in list(active):
                    if next(ch_) == "chunk_done":
                        active.remove(ch_)
            b = c % 2
            sl = slice(c * 128, (c + 1) * 128)
            P.op("sp", lambda e, b=b, sl=sl: e.dma_start(
                out=hcT.t[0:512, :].rearrange("(h p) t -> p h t", p=128)[:, :, sl], in_=hm[b].t[:, :, :]),
                reads=tuple(V(hm[b].t[:, :, :], hm[b].res[k]) for k in range(4)), writes=(hcT[:, :],), dma=True)
        P.barrier()


def sec_pre_c(nc, P, T, xin, mix_norm, w_in, conv_w, S, N=512):
    with ExitStack() as es:
        C = Ctx(nc, P, es)
        setup_consts(P, C)
        g = load_gain(P, C, mix_norm, "gmix")
        w = load_w(P, C, w_in, D, C_IN, "win")
        cw = C.sb([128, 24, 4], F32, name="cw")
        for j in range(4):
            P.op("sp", lambda e, j=j: e.dma_start(out=cw.t[:, :, j], in_=conv_w[j, :].rearrange("(c p) -> p c", p=128),
                                                 allow_slow_non_contiguous=True), writes=(cw[:, :, :],), dma=True)
        ident = make_mask(P, C, [128, 128], [[-1, 128]], 0, 1, ALU.is_equal, dt=BF16, name="identb")
        halo = C.sb([128, 24, 3], BF16, name="halo")
        P.memset(halo[:, :, :], 0.0)
        dg = C.sb([128, 24, 4, 128], BF16, name="dg")
        for ch in range(24):
            for j in range(4):
                P.ts(dg[:, ch, j, :], ident[:, :], cw[:, ch, j:j + 1], None, ALU.mult, eng=("pool" if (ch + j) % 2 else "dve"))
        pss = Rot([C.ps([128, 512]) for _ in range(6)])
        psT = Rot([C.ps([128, 1024], BF16) for _ in range(2)])
        xs = [C.sb([128, 8, N], F32, name="x") for _ in range(1)]
        sq = C.sb([128, 8, N], BF16, name="sq")
        h = C.sb([128, 8, N], BF16, name="h")
        rstd = C.sb([128, N], F32, name="rstd")
        lnt = C.sb([128, N], F32, name="lnt")
        cv = Rot([C.sb([128, N + 3], BF16, name="cv") for _ in range(3)])
        y = C.sb([128, 16, N], F32, name="y", nres=16)
        vb = C.sb([128, 8, N], BF16, name="vb", nres=8)
        kb = C.sb([128, 8, N], BF16, name="kb", nres=8)
        qb = C.sb([128, 8, N], BF16, name="qb")
        s2 = Rot([C.sb([128, N], BF16, name="s2") for _ in range(2)])
        l2 = Rot([C.sb([128, N], F32, name="l2") for _ in range(2)])
        ba = [C.sb([8, N], F32, name="ba") for _ in range(2)]
        tsg = Rot([C.sb([128, 512], BF16, name="tsg") for _ in range(2)])
        ttm = Rot([C.sb([128, 1024], BF16, name="ttm") for _ in range(2)])
        xin_v = xin.t.rearrange("(c p) t -> p c t", p=128)
        for it in range(T // N):
            t0 = it * N
            x = xs[0]
            P.dma(x[:, :, :], V(xin_v[:, :, t0:t0 + N], xin.res[0]))
            rmsnorm_tile(P, C, x, g, h, N, C.ones, pss, sq, rstd, lnt)
            for ch in range(24):
                ps = pss.next()
                for c in range(8):
                    P.mm(ps[:, 0:N], w[:, c, ch * 128:(ch + 1) * 128], h[:, c, :], start=(c == 0), stop=(c == 7))
                cvt = cv.next()
                P.act(cvt[:, 3:3 + N], ps[:, 0:N], AF.Copy)
                P.copy(cvt[:, 0:3], halo[:, ch, :], eng="pool")
                P.copy(halo[:, ch, :], cvt[:, N:N + 3], eng="pool")
                ps2 = pss.next()
                for j in range(4):
                    P.mm(ps2[:, 0:N], dg[:, ch, j, :], cvt[:, j:j + N], start=(j == 0), stop=(j == 3))
                if ch < 16:
                    P.act(y.v((slice(None), ch, slice(None)), ch), ps2[:, 0:N], AF.Silu)
                else:
                    P.act(vb.v((slice(None), ch - 16, slice(None)), ch - 16), ps2[:, 0:N], AF.Silu)
            for tb in range(N // 128):
                r0 = t0 + tb * 128
                for half in range(2):
                    ps = pss.next()
                    for c in range(8):
                        P.mm(ps[:, :], h[:, c, tb * 128:(tb + 1) * 128], w[:, c, 3072 + half * 512:3072 + (half + 1) * 512],
                             start=(c == 0), stop=(c == 7))
                    tg = tsg.next()
                    P.act(tg[:, :], ps[:, :], AF.Silu)
                    P.dma(V(S["sg_tm"].t[r0:r0 + 128, half * 512:(half + 1) * 512], S["sg_tm"].res[0]), tg[:, :])
            for i, nm in enumerate(("bT", "aT")):
                ps = pss.next()
                for c in range(8):
                    P.mm(ps[0:8, 0:N], w[:, c, 4096 + 8 * i:4104 + 8 * i], h[:, c, :], start=(c == 0), stop=(c == 7))
                P.act(ba[i][:, :], ps[0:8, 0:N], AF.Copy)
                P.dma(V(S[nm].t[:, t0:t0 + N], S[nm].res[0]), ba[i][:, :])
            for ch in range(16):
                yv = y.v((slice(None), ch, slice(None)), ch)
                s_ = s2.next()
                P.act(s_[:, :], yv, AF.Square)
                ps = pss.next()
                P.mm(ps[:, 0:N], C.ones[:, :], s_[:, :])
                l_ = l2.next()
                P.act(l_[:, :], ps[:, 0:N], AF.Ln, bias=C.eps[:, 0:1])
                P.act(l_[:, :], l_[:, :], AF.Exp, scale=-0.5)
                if ch < 8:
                    P.stt(qb[:, ch, :], yv, 128 ** -0.5, l_[:, :], ALU.mult, ALU.mult)
                else:
                    P.tt(kb.v((slice(None), ch - 8, slice(None)), ch - 8), yv, l_[:, :], ALU.mult)
            P.dma(V(S["qT"].t.rearrange("h d t -> d h t")[:, :, t0:t0 + N], S["qT"].res[0]), qb[:, :, :])
            P.dma(V(S["kT"].t.rearrange("h d t -> d h t")[:, :, t0:t0 + N], S["kT"].res[0]),
                  V(kb.t[:, :, :], kb.res[7]))
            for src, nm in ((kb, "k_tm"), (vb, "v_tm")):
                for tb in range(N // 128):
                    r0 = t0 + tb * 128
                    pt = psT.next()
                    for ch in range(8):
                        P.tr(pt[:, ch * 128:(ch + 1) * 128], src.v((slice(None), ch, slice(tb * 128, (tb + 1) * 128)), ch),
                             ident[:, :])
                    tt_ = ttm.next()
                    P.copy(tt_[:, :], pt[:, :])
                    P.dma(V(S[nm].t[r0:r0 + 128, :], S[nm].res[0]), tt_[:, :])
        P.barrier()


def sec_gdn(nc, P, T, S, a_log, dt_bias, head_gain, hcT, SCR):
    NSC = T // 128
    NCH = T // 64
    SEG = min(T, 2048)
    with ExitStack() as es:
        C = Ctx(nc, P, es)
        setup_consts(P, C)
        one8 = C.sb([8, 1], F32, name="one8")
        P.memset(one8[:, :], 1.0)
        identf = make_mask(P, C, [128, 128], [[-1, 128]], 0, 1, ALU.is_equal, dt=F32, name="identf")
        identb = C.sb([128, 128], BF16, name="identb")
        P.copy(identb[:, :], identf[:, :])
        mS = make_mask(P, C, [128, 128], [[-1, 128]], 0, 1, ALU.is_gt, dt=F32, name="mS")
        mI = make_mask(P, C, [128, 128], [[1, 128]], 0, -1, ALU.is_ge, dt=F32, name="mI")
        P.memset(mS[64:128, 0:64], 0.0, eng="pool")
        P.memset(mI[0:64, 64:128], 0.0, eng="pool")
        gain_bc = C.sb([128, 128], F32, name="gainbc")
        P.op("sp", lambda e: e.dma_start(out=gain_bc.t[:, :], in_=head_gain.partition_broadcast(128)),
             writes=(gain_bc[:, :],), dma=True)
        tokc = C.sb([128, NSC, 5, 8], F32, name="tokc")
        egl = C.sb([128, 8, NCH], F32, name="egl")
        with ExitStack() as es2:
            C2 = Ctx(nc, P, es2)
            bt = C2.sb([8, T], F32, name="bt")
            at = C2.sb([8, T], F32, name="at")
            P.dma(bt[:, :], V(S["bT"].t[:, :], S["bT"].res[0]))
            P.dma(at[:, :], V(S["aT"].t[:, :], S["aT"].res[0]))
            al = C2.sb([8, 1], F32, name="al")
            db = C2.sb([8, 1], F32, name="db")
            P.op("sp", lambda e: e.dma_start(out=al.t[:, :], in_=a_log.rearrange("(h o) -> h o", o=1)),
                 writes=(al[:, :],), dma=True)
            P.op("sp", lambda e: e.dma_start(out=db.t[:, :], in_=dt_bias.rearrange("(h o) -> h o", o=1)),
                 writes=(db[:, :],), dma=True)
            P.act(al[:, :], al[:, :], AF.Exp)
            P.ts(al[:, :], al[:, :], -1.0, None, ALU.mult)
            P.act(bt[:, :], bt[:, :], AF.Exp, scale=-1.0)
            P.ts(bt[:, :], bt[:, :], 1.0, None, ALU.add)
            P.recip(bt[:, :], bt[:, :])
            P.act(at[:, :], at[:, :], AF.Exp, bias=db[:, 0:1])
            P.act(at[:, :], at[:, :], AF.Ln, bias=one8[:, 0:1])
            P.ts(at[:, :], at[:, :], al[:, 0:1], None, ALU.mult)
            nf = C2.sb([8, T], F32, name="nf")
            P.memset(nf[:, :], 1.0)
            P.memset(V(nf.t[:, :].rearrange("p (c l) -> p c l", l=64)[:, :, 0:1], nf.res[0]), 0.0)
            gr = C2.sb([8, T], F32, name="gr")
            P.scan(gr[:, :], nf[:, :], at[:, :], 0.0, ALU.mult, ALU.add)
            P.dma(SCR["gD"][:, :], gr[:, :])
            eg = C2.sb([8, T], F32, name="eg")
            P.act(eg[:, :], gr[:, :], AF.Exp)
            beg = nf
            P.tt(beg[:, :], bt[:, :], eg[:, :], ALU.mult)
            egg = at
            for c in range(NCH):
                sl = slice(c * 64, (c + 1) * 64)
                P.ts(egg[:, sl], gr[:, sl], -1.0, gr[:, c * 64 + 63:c * 64 + 64], ALU.mult, ALU.add)
            P.act(egg[:, :], egg[:, :], AF.Exp)
            eG = C2.sb([8, NCH], F32, name="eG")
            P.copy(eG[:, :], V(eg.t[:, :].rearrange("p (c l) -> p c l", l=64)[:, :, 63], eg.res[0]))
            P.dma(SCR["eGD"][:, :], eG[:, :])
            P.op("sp", lambda e: e.dma_start(out=egl.t[:, :, :].rearrange("p h c -> p (h c)"),
                                             in_=SCR["eGD"].t.rearrange("h c -> (h c)").partition_broadcast(128)),
                 reads=(SCR["eGD"][:, :],), writes=(egl[:, :, :],), dma=True)
            pst = Rot([C2.ps([128, 512]) for _ in range(2)])
            for sc in range(NSC):
                sl = slice(sc * 128, (sc + 1) * 128)
                ps = pst.next()
                for i, rw in enumerate((gr, bt, beg, egg, eg)):
                    P.tr(ps[:, 8 * i:8 * i + 8], rw[:, sl], identf[0:8, 0:8])
                P.copy(V(tokc.t[:, sc, :, :].rearrange("p a b -> p (a b)"), tokc.res[0]), ps[:, 0:40])
            P.barrier()
        GT = 256
        NG = T // GT
        SPG = GT // 128
        qTg = [C.sb([128, 8, GT], BF16, name="qTg") for _ in range(2)]
        kTg = [C.sb([128, 8, GT], BF16, name="kTg") for _ in range(2)]
        ktg = [C.sb([128, SPG, D], BF16, name="ktg") for _ in range(2)]
        vtg = [C.sb([128, SPG, D], BF16, name="vtg") for _ in range(2)]
        sgg = [C.sb([128, SPG, D], BF16, name="sgg") for _ in range(2)]
        Gbg = [C.sb([128, 8, GT], F32, name="Gbg") for _ in range(2)]
        banks = [C.ps([128, 512]) for _ in range(8)]

        def load_group(gi):
            b = gi % 2
            t0 = gi * GT
            P.dma(qTg[b][:, :, :], V(S["qT"].t.rearrange("h d t -> d h t")[:, :, t0:t0 + GT], S["qT"].res[0]))
            P.dma(kTg[b][:, :, :], V(S["kT"].t.rearrange("h d t -> d h t")[:, :, t0:t0 + GT], S["kT"].res[0]))
            for tl, nm in ((ktg, "k_tm"), (vtg, "v_tm"), (sgg, "sg_tm")):
                P.dma(tl[b][:, :, :], V(S[nm].t[t0:t0 + GT, :].rearrange("(s p) c -> p s c", p=128), S[nm].res[0]))
            for hd in range(8):
                P.op("sp", lambda e, b=b, t0=t0, hd=hd: e.dma_start(
                    out=Gbg[b].t[:, hd, :], in_=SCR["gD"].t[hd, t0:t0 + GT].partition_broadcast(128)),
                    reads=(SCR["gD"][:, :],), writes=(Gbg[b][:, :, :],), dma=True)

        def head_chain(hd):
            bank = banks[hd]

            def t32(name):
                return C.sb([128, 128], F32, name=f"{name}{hd}")

            def t16(name):
                return C.sb([128, 128], BF16, name=f"{name}{hd}")

            dtmp, Da, Dt = t32("dtmp"), t32("Da"), t32("Dt")
            XYs = Rot([C.sb([128, 256], F32, name=f"XY{hd}") for _ in range(2)])
            Qs = Rot([t32("Q"), t32("Q")])
            af, u, t3, o_, o_1, o_2 = t32("af"), t32("u"), t32("t3"), t32("o"), t32("o1"), t32("o2")
            ab_, tb_, v_b, k_b, k_d, wT_, vn, jk, hb = (t16("ab"), t16("tb"), t16("vb"), t16("kb"), t16("kd"),
                                                       t16("wT"), t16("vn"), t16("jk"), t16("hb"))
            Sst = t32("Sst")
            Sbf = Rot([t16("Sbf"), t16("Sbf")])
            s_ = C.sb([128, 4], F32, name=f"scl{hd}")
            P.memset(Sst[:, :], 0.0)
            sbf = Sbf.next()
            P.memset(sbf[:, :], 0.0)
            s0, s1, s2, s3 = (slice(0, 128), slice(128, 256), slice(256, 384), slice(384, 512))
            yield
            for sc in range(NSC):
                gi = (sc * 128) // GT
                b = gi % 2
                si = sc % SPG
                lsl = slice(si * 128, (si + 1) * 128)
                hsl = slice(hd * 128, (hd + 1) * 128)
                kTs = kTg[b][:, hd, lsl]
                qTs = qTg[b][:, hd, lsl]
                gsl = Gbg[b][:, hd, lsl]
                gcol = tokc[:, sc, 0, hd:hd + 1]
                bcol = tokc[:, sc, 1, hd:hd + 1]
                begcol = tokc[:, sc, 2, hd:hd + 1]
                eggcol = tokc[:, sc, 3, hd:hd + 1]
                P.mm(bank[:, s0], kTs, kTs)
                P.mm(bank[:, s1], kTs, qTs)
                P.ts(dtmp[:, :], gsl, gcol, 0.0, ALU.subtract, ALU.max)
                yield
                P.act(Da[:, :], dtmp[:, :], AF.Exp, scale=-1.0)
                P.ts(v_b[:, :], vtg[b][:, si, hsl], bcol, None, ALU.mult, eng="pool")
                yield
                P.stt(Da[:, :], Da[:, :], bcol, mS[:, :], ALU.mult, ALU.mult)
                P.ts(k_b[:, :], ktg[b][:, si, hsl], begcol, None, ALU.mult, eng="pool")
                yield
                XY = XYs.next()
                X, Y = XY[:, 0:128], XY[:, 128:256]
                P.tt(X, bank[:, s0], Da[:, :], ALU.mult)
                P.ts(k_d[:, :], ktg[b][:, si, hsl], eggcol, None, ALU.mult, eng="pool")
                yield
                P.tr(bank[:, s2], X, identf[:, :])
                P.ts(dtmp[:, :], gsl, gcol, 0.0, ALU.subtract, ALU.min)
                yield
                P.act(Y, bank[:, s2], AF.Copy)
                yield
                P.act(Dt[:, :], dtmp[:, :], AF.Exp)
                Q = Qs.next()
                P.tt(Q[:, :], identf[:, :], Y, ALU.subtract)
                yield
                P.tt(af[:, :], bank[:, s1], Dt[:, :], ALU.mult)
                yield
                P.tt(ab_[:, :], af[:, :], mI[:, :], ALU.mult, eng="pool")
                for lvl in range(1, 6):
                    P.mm(bank[:, s0], Y, X)
                    if lvl < 5:
                        P.mm(bank[:, s1], X, Y)
                    yield
                    XYn = XYs.next()
                    if lvl < 5:
                        P.act(XYn[:, 0:256], bank[:, 0:256], AF.Copy)
                    else:
                        P.act(XYn[:, 0:128], bank[:, s0], AF.Copy)
                    Xn, Yn = XYn[:, 0:128], XYn[:, 128:256]
                    yield
                    P.mm(bank[:, s2], Xn, Q[:, :])
                    yield
                    Qn = Qs.next()
                    P.tt(Qn[:, :], bank[:, s2], Q[:, :], ALU.add)
                    yield
                    X, Y, Q = Xn, Yn, Qn
                P.copy(tb_[:, :], Q[:, :], eng="pool")
                yield
                P.mm(bank[:, s0], tb_[:, :], v_b[:, :])
                P.mm(bank[:, s1], k_b[:, :], tb_[:, :])
                yield
                P.act(u[:, :], bank[:, s0], AF.Copy)
                yield
                P.act(wT_[:, :], bank[:, s1], AF.Copy)
                yield
                for cc in range(2):
                    pr = slice(cc * 64, (cc + 1) * 64)
                    isl = slice(si * 128 + cc * 64, si * 128 + (cc + 1) * 64)
                    ch = sc * 2 + cc
                    P.mm(bank[pr, s2], wT_[:, pr], sbf[:, :])
                    P.mm(bank[pr, s3], qTg[b][:, hd, isl], sbf[:, :])
                    yield
                    P.tt(vn[pr, :], u[pr, :], bank[pr, s2], ALU.subtract)
                    yield
                    P.mm(bank[pr, s0], ab_[pr, pr], vn[pr, :])
                    P.mm(bank[:, s1], k_d[pr, :], vn[pr, :])
                    yield
                    P.stt(Sst[:, :], Sst[:, :], egl[:, hd, ch:ch + 1], bank[:, s1], ALU.mult, ALU.add)
                    yield
                    sbf = Sbf.next()
                    P.copy(sbf[:, :], Sst[:, :], eng="pool")
                    P.act(t3[pr, :], bank[pr, s0], AF.Copy)
                    yield
                    P.stt(o_[pr, :], bank[pr, s3], V(tokc.t[pr, sc, 4, hd:hd + 1], tokc.res[0]), t3[pr, :],
                          ALU.mult, ALU.add)
                    yield
                P.act(jk[:, :], o_[:, :], AF.Square, accum=s_[:, 0:1])
                yield
                P.act(s_[:, 1:2], s_[:, 0:1], AF.Ln, scale=1.0 / 128.0, bias=C.eps[:, 0:1])
                yield
                P.act(s_[:, 2:3], s_[:, 1:2], AF.Exp, scale=-0.5)
                yield
                P.stt(o_1[:, :], o_[:, :], s_[:, 2:3], gain_bc[:, :], ALU.mult, ALU.mult)
                yield
                P.tt(o_2[:, :], o_1[:, :], sgg[b][:, si, hsl], ALU.mult, eng="pool")
                yield
                P.tr(bank[:, s2], o_2[:, :], identf[:, :])
                yield
                P.act(hb[:, :], bank[:, s2], AF.Copy)
                yield
                P.dma(V(hcT.t[hd * 128:(hd + 1) * 128, sc * 128:(sc + 1) * 128], hcT.res[0]), hb[:, :])
                yield "sc_done"

        chains = [head_chain(hd) for hd in range(8)]
        for ch_ in chains:
            next(ch_)
        load_group(0)
        for sc in range(NSC):
            if (sc * 128) % GT == 0:
                gi = (sc * 128) // GT
                if gi + 1 < NG:
                    load_group(gi + 1)
            active = list(chains)
            while active:
                for ch_ in list(active):
                    if next(ch_) == "sc_done":
                        active.remove(ch_)
        P.barrier()


W_SHAPES = dict(
    mix_norm=[4, D], ab_w_in=[2, D, AB_IN], ab_b_i=[2, 4], ab_b_f=[2, 4], ab_head_gain=[2, 4, 128],
    ab_w_out=[2, D, D], c_w_in=[2, D, C_IN], c_conv_w=[2, 4, 3072], c_a_log=[2, 8], c_dt_bias=[2, 8],
    c_head_gain=[2, 128], c_w_out=[2, D, D], xa_norm=[4, D], mem_norm=[D], xa_wq=[4, D, D], xa_wk=[4, D, D],
    xa_wv=[4, D, D], xa_wo=[4, D, D], mlp_norm=[4, D], mlp_w1=[4, D, DFF], mlp_w2=[4, DFF, D], final_norm=[D])


def build_program(T, depth=DEPTH):
    nc = bass.Bass("TRN2", target_bir_lowering=False)
    P = Prog(nc)
    xT = dram(nc, "xT", [D, T], F32, kind="ExternalInput")
    memT = dram(nc, "memT", [D, MEM], F32, kind="ExternalInput")
    W = {n: nc.dram_tensor(n, shp, F32, kind="ExternalInput").ap() for n, shp in W_SHAPES.items()}
    outT = dram(nc, "outT", [D, T], F32, kind="ExternalOutput")
    xA = dram(nc, "xA", [D, T], F32)
    xB = dram(nc, "xB", [D, T], F32)
    hcT = dram(nc, "hcT", [D, T], BF16)
    memnT = dram(nc, "memnT", [D, MEM], BF16)
    SA = dict(mqT=dram(nc, "mqT", [4, 64, T], BF16), mkT=dram(nc, "mkT", [4, 64, T], BF16), gT=dram(nc, "gT", [8, T], F32),
              sqT=dram(nc, "sqT", [4, 128, T], BF16), skT=dram(nc, "skT", [4, 128, T], BF16),
              mk_tm=dram(nc, "mk_tm", [T, 256], BF16), mv_tm=dram(nc, "mv_tm", [T, 512], BF16),
              sg_tm=dram(nc, "sg_tm", [T, 512], BF16), sv_tm=dram(nc, "sv_tm", [T, 512], BF16))
    SC = dict(qT=dram(nc, "cqT", [8, 128, T], BF16), kT=dram(nc, "ckT", [8, 128, T], BF16),
              k_tm=dram(nc, "ck_tm", [T, D], BF16), v_tm=dram(nc, "cv_tm", [T, D], BF16),
              sg_tm=dram(nc, "csg_tm", [T, D], BF16), bT=dram(nc, "cbT", [8, T], F32), aT=dram(nc, "caT", [8, T], F32))
    SCR = dict(gD=dram(nc, "gD", [8, T], F32), eGD=dram(nc, "eGD", [8, T // 64], F32))
    sec_memn(nc, P, memT, W["mem_norm"], memnT)
    cur = xT
    for l in range(depth):
        j = l // 2
        if l % 2 == 0:
            sec_pre_ab(nc, P, T, cur, W["mix_norm"][l], W["ab_w_in"][j], SA)
            sec_sb(nc, P, T, SA, hcT)
            sec_mlstm(nc, P, T, SA, W["ab_b_i"][j], W["ab_b_f"][j], W["ab_head_gain"][j], hcT)
            w_out = W["ab_w_out"][j]
        else:
            sec_pre_c(nc, P, T, cur, W["mix_norm"][l], W["c_w_in"][j], W["c_conv_w"][j], SC)
            sec_gdn(nc, P, T, SC, W["c_a_log"][j], W["c_dt_bias"][j], W["c_head_gain"][j], hcT, SCR)
            w_out = W["c_w_out"][j]
        sec_post_a(nc, P, T, cur, hcT, xB, w_out, W["xa_norm"][l], W["xa_wq"][l], W["xa_wk"][l], W["xa_wv"][l],
                   W["xa_wo"][l], memnT)
        last = (l == depth - 1)
        sec_post_b(nc, P, T, xB, outT if last else xA, W["mlp_norm"][l], W["mlp_w1"][l], W["mlp_w2"][l],
                   final_norm=W["final_norm"] if last else None)
        cur = xA
    P.finalize()
    return nc, P


def kernel(**inputs):
    x = np.asarray(inputs["x"], dtype=np.float32)
    mem = np.asarray(inputs["mem"], dtype=np.float32)
    B, T, _ = x.shape
    nc, _ = build_program(T)
    wts = {n: np.ascontiguousarray(np.asarray(inputs[n], dtype=np.float32)) for n in W_SHAPES}
    in_maps = []
    for b in range(B):
        m = dict(wts)
        m["xT"] = np.ascontiguousarray(x[b].T)
        m["memT"] = np.ascontiguousarray(mem[b].T)
        in_maps.append(m)
    res = run_bass_kernel_spmd(nc, in_maps, core_ids=list(range(B)))
    out = np.stack([np.ascontiguousarray(r["outT"].T) for r in res.results], axis=0)
    return out.astype(np.float32)
```

```python
import numpy as np
from contextlib import ExitStack
import concourse.bass as bass
import concourse.mybir as mybir
from concourse.bass_utils import run_bass_kernel_spmd

F32 = mybir.dt.float32
BF16 = mybir.dt.bfloat16
AF = mybir.ActivationFunctionType
ALU = mybir.AluOpType

D = 1024
DEPTH = 4
MEM = 256
EPS = 1e-6
AB_IN = 3080
C_IN = 4112
DFF = 4096
LIM = 30000
DLIM = 1800
SAME_ENGINE_SYNC = True


class Res:
    __slots__ = ("w", "r", "x")

    def __init__(self, x=False):
        self.w = {}
        self.r = {}
        self.x = x


class V:
    __slots__ = ("ap", "res")

    def __init__(self, ap, res):
        self.ap = ap
        self.res = res


class Tl:
    def __init__(self, t, nres=1, x=False):
        self.t = t
        self.res = [Res(x) for _ in range(nres)]

    def __getitem__(self, idx):
        return V(self.t[idx], self.res[0])

    def v(self, idx, k=0):
        return V(self.t[idx], self.res[k])


class Prog:
    ENGS = ("pe", "act", "dve", "pool", "sp")

    def __init__(self, nc):
        self.nc = nc
        self.lists = {e: [] for e in self.ENGS}
        self.cnt = {}
        self.seen = {e: {} for e in self.ENGS}

    DMA_SLOTS = {"sp": 24, "pool": 12, "act": 8}

    def op(self, eng, fn, reads=(), writes=(), dma=False):
        if dma:
            tot = self.cnt.get(("dq", eng), 0)
            self.cnt[("dq", eng)] = tot + 1
            key = ("d", eng, tot % self.DMA_SLOTS[eng])
        else:
            key = ("c", eng)
        n = self.cnt.get(key, 0) + 1
        self.cnt[key] = n
        waits = {}
        seen = self.seen[eng]
        if dma and n > 1 and n - 1 > seen.get(key, 0):
            waits[key] = n - 1

        def need(k, v):
            if k[0] == "c" and k[1] == eng and (eng == "pe" or not SAME_ENGINE_SYNC):
                return
            if v > seen.get(k, 0) and v > waits.get(k, 0):
                waits[k] = v

        xr = [r for r in reads if r.res.x]
        if xr:
            writes = tuple(writes) + tuple(xr)
        for r in reads:
            for k, v in r.res.w.items():
                need(k, v)
        for w in writes:
            for k, v in w.res.w.items():
                need(k, v)
            for k, v in w.res.r.items():
                need(k, v)
        for k, v in waits.items():
            seen[k] = v
        self.lists[eng].append((list(waits.items()), fn, key, n))
        for r in reads:
            if r.res.r.get(key, 0) < n:
                r.res.r[key] = n
        for w in writes:
            w.res.w = {key: n}
            w.res.r = {}

    def barrier(self):
        snap = {k: v for k, v in self.cnt.items() if k[0] != "dq"}
        for e in self.ENGS:
            waits = []
            for k, v in snap.items():
                if k[0] == "c" and k[1] == e:
                    continue
                if v > self.seen[e].get(k, 0):
                    waits.append((k, v))
                    self.seen[e][k] = v
            if waits:
                self.lists[e].append((waits, None, None, 0))

    def finalize(self):
        nc = self.nc
        self.barrier()
        with ExitStack() as es:
            sems = {}
            for key, n in self.cnt.items():
                if key[0] == "dq":
                    continue
                lim = DLIM if key[0] == "d" else LIM
                for g in range((n - 1) // lim + 1):
                    sems[(key, g)] = es.enter_context(nc.semaphore("s" + "_".join(str(z) for z in key) + f"_{g}"))
            block = es.enter_context(nc.Block())

            def run(name, eng):
                for waits, fn, key, n in self.lists[name]:
                    for (k, v) in waits:
                        lim = DLIM if k[0] == "d" else LIM
                        g = (v - 1) // lim
                        val = v - g * lim
                        if k[0] == "d":
                            if g > 0:
                                eng.wait_ge(sems[(k, g - 1)], lim * 16)
                            eng.wait_ge(sems[(k, g)], val * 16)
                        else:
                            eng.wait_ge(sems[(k, g)], val)
                    if fn is not None:
                        inst = fn(eng)
                        lim = DLIM if key[0] == "d" else LIM
                        g = (n - 1) // lim
                        inst.then_inc(sems[(key, g)], 16 if key[0] == "d" else 1)

            @block.tensor
            def _(e):
                run("pe", e)

            @block.scalar
            def _(e):
                run("act", e)

            @block.vector
            def _(e):
                run("dve", e)

            @block.gpsimd
            def _(e):
                run("pool", e)

            @block.sync
            def _(e):
                run("sp", e)

    def mm(self, out, lhsT, rhs, start=True, stop=True):
        self.op("pe", lambda e: e.matmul(out.ap, lhsT.ap, rhs.ap, start=start, stop=stop),
                reads=(lhsT, rhs) if start else (lhsT, rhs, out), writes=(out,))

    def tr(self, out, in_, ident):
        self.op("pe", lambda e: e.transpose(out.ap, in_.ap, ident.ap), reads=(in_, ident), writes=(out,))

    def act(self, out, in_, func, scale=1.0, bias=0.0, accum=None, eng="act"):
        rd = [in_]
        if isinstance(scale, V):
            rd.append(scale)
        if isinstance(bias, V):
            rd.append(bias)
        sc = scale.ap if isinstance(scale, V) else scale
        bi = bias.ap if isinstance(bias, V) else bias
        wr = [out]
        if accum is not None:
            wr.append(accum)
        acc = accum.ap if accum is not None else None
        self.op("act", lambda e: e.activation(out.ap, in_.ap, func, bias=bi, scale=sc, accum_out=acc),
                reads=rd, writes=wr)

    def tt(self, out, a, b, op, eng="dve"):
        self.op(eng, lambda e: e.tensor_tensor(out.ap, a.ap, b.ap, op), reads=(a, b), writes=(out,))

    def ts(self, out, a, s1, s2, op0, op1=None, eng="dve", accum=None):
        rd = [a]
        if isinstance(s1, V):
            rd.append(s1)
        if isinstance(s2, V):
            rd.append(s2)
        v1 = s1.ap if isinstance(s1, V) else s1
        v2 = s2.ap if isinstance(s2, V) else s2
        wr = [out]
        if accum is not None:
            wr.append(accum)
        acc = accum.ap if accum is not None else None
        if op1 is None:
            self.op(eng, lambda e: e.tensor_scalar(out.ap, a.ap, v1, v2, op0, accum_out=acc) if acc is not None
                    else e.tensor_scalar(out.ap, a.ap, v1, v2, op0), reads=rd, writes=wr)
        else:
            self.op(eng, lambda e: e.tensor_scalar(out.ap, a.ap, v1, v2, op0, op1, accum_out=acc) if acc is not None
                    else e.tensor_scalar(out.ap, a.ap, v1, v2, op0, op1), reads=rd, writes=wr)

    def stt(self, out, a, s, b, op0, op1):
        rd = [a, b]
        if isinstance(s, V):
            rd.append(s)
        sv = s.ap if isinstance(s, V) else s
        self.op("dve", lambda e: e.scalar_tensor_tensor(out.ap, a.ap, sv, b.ap, op0, op1), reads=rd, writes=(out,))

    def scan(self, out, d0, d1, initial, op0, op1):
        rd = [d0, d1]
        if isinstance(initial, V):
            rd.append(initial)
        iv = initial.ap if isinstance(initial, V) else initial
        self.op("dve", lambda e: e.tensor_tensor_scan(out.ap, d0.ap, d1.ap, iv, op0, op1), reads=rd, writes=(out,))

    def copy(self, out, in_, eng="dve"):
        self.op(eng, lambda e: e.tensor_copy(out.ap, in_.ap), reads=(in_,), writes=(out,))

    def recip(self, out, in_):
        self.op("dve", lambda e: e.reciprocal(out.ap, in_.ap), reads=(in_,), writes=(out,))

    def memset(self, out, val, eng="dve"):
        self.op(eng, lambda e: e.memset(out.ap, val), writes=(out,))

    def dma(self, out, in_, q="sp", **kw):
        self.op(q, lambda e: e.dma_start(out=out.ap, in_=in_.ap, **kw), reads=(in_,), writes=(out,), dma=True)


class Ctx:
    _uid = [0]

    def __init__(self, nc, P, es):
        self.nc, self.P, self.es = nc, P, es
        self.n = 0
        Ctx._uid[0] += 1
        self.uid = Ctx._uid[0]

    def sb(self, shape, dt, nres=1, name=None):
        self.n += 1
        t = self.es.enter_context(self.nc.sbuf_tensor(f"{name or 't'}_{self.uid}_{self.n}", list(shape), dt))
        return Tl(t, nres)

    def ps(self, shape, dt=F32, name=None):
        self.n += 1
        assert shape[0] == 128 and shape[1] * (4 if dt == F32 else 2) == 2048, "PSUM tiles are whole banks"
        t = self.es.enter_context(self.nc.psum_tensor(f"{name or 'p'}_{self.uid}_{self.n}", list(shape), dt))
        return Tl(t, x=True)


class Rot:
    def __init__(self, items):
        self.items = items
        self.i = 0

    def next(self):
        t = self.items[self.i % len(self.items)]
        self.i += 1
        return t


def dram(nc, name, shape, dt, kind=None):
    if kind is None:
        t = nc.dram_tensor(name, list(shape), dt)
    else:
        t = nc.dram_tensor(name, list(shape), dt, kind=kind)
    return Tl(t.ap())


def load_w(P, C, w_dram_ap, K, N, name, q="pool", res=None):
    kc = K // 128
    w = C.sb([128, kc, N], BF16, name=name)
    src = w_dram_ap.rearrange("(c p) n -> p c n", p=128)
    step = 2048
    for c in range(kc):
        for n0 in range(0, N, step):
            n1 = min(N, n0 + step)
            P.dma(V(w.t[:, c, n0:n1], w.res[0]), V(src[:, c, n0:n1], res or Res()), q=q)
    return w


def rmsnorm_tile(P, C, x, g, h, N, ones, ps_rot, sq, rstd, lnt):
    if isinstance(sq, Tl):
        P.act(sq[:, :, :], x[:, :, :], AF.Square)
        sq = [sq[:, c, :] for c in range(8)]
    else:
        for c in range(8):
            P.act(sq[c], x[:, c, :], AF.Square)
    ps = ps_rot.next()
    for c in range(8):
        P.mm(ps[:, 0:N], ones[:, :], sq[c], start=(c == 0), stop=(c == 7))
    P.act(lnt[:, :], ps[:, 0:N], AF.Ln, scale=1.0 / D, bias=C.eps[:, 0:1])
    P.act(rstd[:, :], lnt[:, :], AF.Exp, scale=-0.5)
    for c in range(8):
        P.stt(h[:, c, :], x[:, c, :], g[:, c:c + 1], rstd[:, :], ALU.mult, ALU.mult)


def setup_consts(P, C):
    C.ones = C.sb([128, 128], BF16, name="ones")
    P.memset(C.ones[:, :], 1.0)
    C.eps = C.sb([128, 1], F32, name="eps")
    P.memset(C.eps[:, :], EPS)


def load_gain(P, C, g_dram_row_ap, name):
    g = C.sb([128, 8], F32, name=name)
    P.op("sp", lambda e: e.dma_start(out=g.t[:, :], in_=g_dram_row_ap.rearrange("(c p) -> p c", p=128),
                                     allow_slow_non_contiguous=True), writes=(g[:, :],), dma=True)
    return g


def sec_memn(nc, P, memT, mem_norm, memnT_out):
    with ExitStack() as es:
        C = Ctx(nc, P, es)
        setup_consts(P, C)
        g = load_gain(P, C, mem_norm, "gmem")
        x = C.sb([128, 8, MEM], F32)
        P.dma(x[:, :, :], V(memT.t.rearrange("(c p) t -> p c t", p=128), memT.res[0]))
        sq = C.sb([128, 8, MEM], BF16)
        rstd = C.sb([128, MEM], F32)
        lnt = C.sb([128, MEM], F32)
        h = C.sb([128, 8, MEM], BF16)
        ps = C.ps([128, 512])
        rmsnorm_tile(P, C, x, g, h, MEM, C.ones, Rot([ps]), sq, rstd, lnt)
        P.dma(V(memnT_out.t.rearrange("(c p) t -> p c t", p=128), memnT_out.res[0]), h[:, :, :])
        P.barrier()


def sec_post_a(nc, P, T, xin, hc, xout, w_out, xa_norm, wq, wk, wv, wo, memnT, N=512):
    with ExitStack() as es:
        C = Ctx(nc, P, es)
        setup_consts(P, C)
        g = load_gain(P, C, xa_norm, "gxa")
        wout_sb = load_w(P, C, w_out, D, D, "wout")
        wq_sb = load_w(P, C, wq, D, D, "wq")
        wo_sb = load_w(P, C, wo, D, D, "wo")
        kT = C.sb([128, 8, MEM], BF16, name="kT")
        v_sb = C.sb([128, 2, D], BF16, name="vsb")
        pss = Rot([C.ps([128, 512]) for _ in range(8)])
        with ExitStack() as es2:
            C2 = Ctx(nc, P, es2)
            wk_sb = load_w(P, C2, wk, D, D, "wk")
            wv_sb = load_w(P, C2, wv, D, D, "wv")
            mn = C2.sb([128, 8, MEM], BF16, name="mn")
            P.dma(mn[:, :, :], V(memnT.t.rearrange("(c p) t -> p c t", p=128), memnT.res[0]))
            for o in range(8):
                ps = pss.next()
                for c in range(8):
                    P.mm(ps[:, 0:MEM], wk_sb[:, c, o * 128:(o + 1) * 128], mn[:, c, :], start=(c == 0), stop=(c == 7))
                P.act(kT[:, o, :], ps[:, 0:MEM], AF.Copy)
            for mb in range(2):
                for half in range(2):
                    ps = pss.next()
                    for c in range(8):
                        P.mm(ps[:, :], mn[:, c, mb * 128:(mb + 1) * 128], wv_sb[:, c, half * 512:(half + 1) * 512],
                             start=(c == 0), stop=(c == 7))
                    P.copy(v_sb[:, mb, half * 512:(half + 1) * 512], ps[:, :])
            P.barrier()
        xin_v = xin.t.rearrange("(c p) t -> p c t", p=128)
        hc_v = hc.t.rearrange("(c p) t -> p c t", p=128)
        xout_v = xout.t.rearrange("(c p) t -> p c t", p=128)
        NCHAIN = 2

        def tile_chain(k):
            x = C.sb([128, 8, N], F32, name=f"x{k}")
            hcx = C.sb([128, 8, N], BF16, name=f"hc{k}")
            sq = C.sb([128, 8, N], BF16, name=f"sq{k}")
            h = C.sb([128, 8, N], BF16, name=f"h{k}")
            qs = C.sb([128, 8, N], BF16, name=f"q{k}", nres=8)
            os_ = C.sb([128, 8, N], BF16, name=f"o{k}", nres=8)
            rstd = C.sb([128, N], F32, name=f"rstd{k}")
            lnt = C.sb([128, N], F32, name=f"lnt{k}")
            pT = [C.sb([128, N], BF16, name=f"pT{k}") for _ in range(4)]
            rden = [C.sb([128, N], F32, name=f"rden{k}") for _ in range(2)]
            yield
            for it in range(k, T // N, NCHAIN):
                t0 = it * N
                P.dma(x[:, :, :], V(xin_v[:, :, t0:t0 + N], xin.res[0]))
                P.dma(hcx[:, :, :], V(hc_v[:, :, t0:t0 + N], hc.res[0]))
                for o in range(8):
                    ps = pss.next()
                    for c in range(8):
                        P.mm(ps[:, 0:N], wout_sb[:, c, o * 128:(o + 1) * 128], hcx[:, c, :], start=(c == 0), stop=(c == 7))
                    P.tt(x[:, o, :], x[:, o, :], ps[:, 0:N], ALU.add)
                    yield
                P.act(sq[:, :, :], x[:, :, :], AF.Square)
                yield
                ps = pss.next()
                for c in range(8):
                    P.mm(ps[:, 0:N], C.ones[:, :], sq[:, c, :], start=(c == 0), stop=(c == 7))
                P.act(lnt[:, :], ps[:, 0:N], AF.Ln, scale=1.0 / D, bias=C.eps[:, 0:1])
                yield
                P.act(rstd[:, :], lnt[:, :], AF.Exp, scale=-0.5)
                yield
                for c in range(8):
                    P.stt(h[:, c, :], x[:, c, :], g[:, c:c + 1], rstd[:, :], ALU.mult, ALU.mult)
                    if c % 2 == 1:
                        yield
                for o in range(8):
                    ps = pss.next()
                    for c in range(8):
                        P.mm(ps[:, 0:N], wq_sb[:, c, o * 128:(o + 1) * 128], h[:, c, :], start=(c == 0), stop=(c == 7))
                    P.act(qs.v((slice(None), o, slice(None)), o), ps[:, 0:N], AF.Copy, scale=1.0 / 16.0)
                    yield
                for hd in range(4):
                    pts = []
                    for mb in range(2):
                        ps = pss.next()
                        for j in range(2):
                            P.mm(ps[:, 0:N], kT[:, 2 * hd + j, mb * 128:(mb + 1) * 128],
                                 qs.v((slice(None), 2 * hd + j, slice(None)), 2 * hd + j), start=(j == 0), stop=(j == 1))
                        pt = pT[(2 * hd + mb) % 4]
                        P.act(pt[:, :], ps[:, 0:N], AF.Exp)
                        pts.append(pt)
                    yield
                    ps = pss.next()
                    for mb in range(2):
                        P.mm(ps[:, 0:N], C.ones[:, :], pts[mb][:, :], start=(mb == 0), stop=(mb == 1))
                    rd = rden[hd % 2]
                    P.recip(rd[:, :], ps[:, 0:N])
                    yield
                    for j in range(2):
                        ps = pss.next()
                        for mb in range(2):
                            P.mm(ps[:, 0:N], v_sb[:, mb, (2 * hd + j) * 128:(2 * hd + j + 1) * 128], pts[mb][:, :],
                                 start=(mb == 0), stop=(mb == 1))
                        P.tt(os_.v((slice(None), 2 * hd + j, slice(None)), 2 * hd + j), ps[:, 0:N], rd[:, :], ALU.mult)
                    yield
                for o in range(8):
                    ps = pss.next()
                    for c in range(8):
                        P.mm(ps[:, 0:N], wo_sb[:, c, o * 128:(o + 1) * 128], os_.v((slice(None), c, slice(None)), c),
                             start=(c == 0), stop=(c == 7))
                    P.tt(x[:, o, :], x[:, o, :], ps[:, 0:N], ALU.add)
                    yield
                P.dma(V(xout_v[:, :, t0:t0 + N], xout.res[0]), x[:, :, :], q="sp")
                yield

        chains = [tile_chain(k) for k in range(NCHAIN)]
        for ch_ in chains:
            next(ch_)
        active = list(chains)
        stagger = 20
        steps = 0
        while active:
            for idx, ch_ in enumerate(list(active)):
                if ch_ is chains[1] and steps < stagger:
                    continue
                try:
                    next(ch_)
                except StopIteration:
                    active.remove(ch_)
            steps += 1
        P.barrier()


def sec_post_b(nc, P, T, xin, xout, mlp_norm, w1, w2, final_norm=None, N=512):
    with ExitStack() as es:
        C = Ctx(nc, P, es)
        setup_consts(P, C)
        g = load_gain(P, C, mlp_norm, "gmlp")
        gf = load_gain(P, C, final_norm, "gfin") if final_norm is not None else None
        w1_sb = load_w(P, C, w1, D, DFF, "w1")
        w2_sb = load_w(P, C, w2, DFF, D, "w2")
        pss = Rot([C.ps([128, 512]) for _ in range(8)])
        xs = [C.sb([128, 8, N], F32, name="x") for _ in range(1)]
        h = C.sb([128, 8, N], BF16, name="h")
        gg = C.sb([128, 32, N], BF16, name="gg", nres=32)
        sq = [gg.v((slice(None), 24 + c, slice(None)), 24 + c) for c in range(8)]
        rr = [C.sb([128, N], F32, name="rr") for _ in range(3)]
        rstd = C.sb([128, N], F32, name="rstd")
        lnt = C.sb([128, N], F32, name="lnt")
        xin_v = xin.t.rearrange("(c p) t -> p c t", p=128)
        xout_v = xout.t.rearrange("(c p) t -> p c t", p=128)
        for it in range(T // N):
            t0 = it * N
            x = xs[0]
            P.dma(x[:, :, :], V(xin_v[:, :, t0:t0 + N], xin.res[0]))
            rmsnorm_tile(P, C, x, g, h, N, C.ones, pss, sq, rstd, lnt)
            for f in range(32):
                ps = pss.next()
                for c in range(8):
                    P.mm(ps[:, 0:N], w1_sb[:, c, f * 128:(f + 1) * 128], h[:, c, :], start=(c == 0), stop=(c == 7))
                r = rr[f % 3]
                P.act(r[:, :], ps[:, 0:N], AF.Relu)
                P.tt(gg.v((slice(None), f, slice(None)), f), r[:, :], r[:, :], ALU.mult, eng="pool")
            for o in range(8):
                ps = pss.next()
                for f in range(32):
                    P.mm(ps[:, 0:N], w2_sb[:, f, o * 128:(o + 1) * 128], gg.v((slice(None), f, slice(None)), f),
                         start=(f == 0), stop=(f == 31))
                P.tt(x[:, o, :], x[:, o, :], ps[:, 0:N], ALU.add)
            if gf is not None:
                for c in range(8):
                    P.act(sq[c], x[:, c, :], AF.Square)
                ps = pss.next()
                for c in range(8):
                    P.mm(ps[:, 0:N], C.ones[:, :], sq[c], start=(c == 0), stop=(c == 7))
                P.act(lnt[:, :], ps[:, 0:N], AF.Ln, scale=1.0 / D, bias=C.eps[:, 0:1])
                P.act(rstd[:, :], lnt[:, :], AF.Exp, scale=-0.5)
                for c in range(8):
                    P.stt(x[:, c, :], x[:, c, :], gf[:, c:c + 1], rstd[:, :], ALU.mult, ALU.mult)
            P.dma(V(xout_v[:, :, t0:t0 + N], xout.res[0]), x[:, :, :], q="sp")
        P.barrier()


def sec_pre_ab(nc, P, T, xin, mix_norm, w_in, S, N=512):
    with ExitStack() as es:
        C = Ctx(nc, P, es)
        setup_consts(P, C)
        g = load_gain(P, C, mix_norm, "gmix")
        w = load_w(P, C, w_in, D, AB_IN, "win")
        pss = Rot([C.ps([128, 512]) for _ in range(8)])
        xs = [C.sb([128, 8, N], F32, name="x") for _ in range(2)]
        sq = C.sb([128, 8, N], BF16, name="sq")
        h = C.sb([128, 8, N], BF16, name="h")
        rstd = C.sb([128, N], F32, name="rstd")
        lnt = C.sb([128, N], F32, name="lnt")
        mq_sb = [C.sb([64, 4, N], BF16, name="mq") for _ in range(2)]
        mk_sb = [C.sb([64, 4, N], BF16, name="mk") for _ in range(2)]
        g_sb = [C.sb([8, N], F32, name="gs") for _ in range(2)]
        sq_sb = [C.sb([128, 4, N], BF16, name="sqs") for _ in range(2)]
        sk_sb = [C.sb([128, 4, N], BF16, name="sks") for _ in range(2)]
        tmk = [C.sb([128, 256], BF16, name="tmk") for _ in range(2)]
        tmv = [C.sb([128, 512], BF16, name="tmv") for _ in range(2)]
        tsv = [C.sb([128, 512], BF16, name="tsv") for _ in range(2)]
        tsg = [C.sb([128, 512], BF16, name="tsg") for _ in range(2)]
        ex = [C.sb([128, 512], F32, name="ex") for _ in range(2)]
        xin_v = xin.t.rearrange("(c p) t -> p c t", p=128)
        for it in range(T // N):
            t0 = it * N
            b = it % 2
            x = xs[b]
            P.dma(x[:, :, :], V(xin_v[:, :, t0:t0 + N], xin.res[0]))
            rmsnorm_tile(P, C, x, g, h, N, C.ones, pss, sq, rstd, lnt)

            def fm(col0, M, dst, scale=1.0):
                ps = pss.next()
                for c in range(8):
                    P.mm(ps[0:M, 0:N], w[:, c, col0:col0 + M], h[:, c, :], start=(c == 0), stop=(c == 7))
                P.act(dst, ps[0:M, 0:N], AF.Copy, scale=scale)

            for hd in range(4):
                fm(64 * hd, 64, mq_sb[b][:, hd, :])
                fm(256 + 64 * hd, 64, mk_sb[b][:, hd, :], 0.125)
                fm(1544 + 128 * hd, 128, sq_sb[b][:, hd, :], 128 ** -0.5)
                fm(2056 + 128 * hd, 128, sk_sb[b][:, hd, :])
            fm(1536, 8, g_sb[b][:, :])
            P.dma(V(S["mqT"].t.rearrange("h d t -> d h t")[:, :, t0:t0 + N], S["mqT"].res[0]), mq_sb[b][:, :, :])
            P.dma(V(S["mkT"].t.rearrange("h d t -> d h t")[:, :, t0:t0 + N], S["mkT"].res[0]), mk_sb[b][:, :, :])
            P.dma(V(S["sqT"].t.rearrange("h d t -> d h t")[:, :, t0:t0 + N], S["sqT"].res[0]), sq_sb[b][:, :, :])
            P.dma(V(S["skT"].t.rearrange("h d t -> d h t")[:, :, t0:t0 + N], S["skT"].res[0]), sk_sb[b][:, :, :])
            P.dma(V(S["gT"].t[:, t0:t0 + N], S["gT"].res[0]), g_sb[b][:, :])
            for tb in range(N // 128):
                r0 = t0 + tb * 128
                bb = tb % 2

                def tm(col0, W):
                    ps = pss.next()
                    for c in range(8):
                        P.mm(ps[:, 0:W], h[:, c, tb * 128:(tb + 1) * 128], w[:, c, col0:col0 + W],
                             start=(c == 0), stop=(c == 7))
                    return ps

                ps = tm(256, 256)
                P.act(tmk[bb][:, :], ps[:, 0:256], AF.Copy, scale=0.125)
                P.dma(V(S["mk_tm"].t[r0:r0 + 128, :], S["mk_tm"].res[0]), tmk[bb][:, :])
                ps = tm(512, 512)
                P.copy(tmv[bb][:, :], ps[:, :])
                P.dma(V(S["mv_tm"].t[r0:r0 + 128, :], S["mv_tm"].res[0]), tmv[bb][:, :])
                ps = tm(2568, 512)
                P.copy(tsv[bb][:, :], ps[:, :])
                P.dma(V(S["sv_tm"].t[r0:r0 + 128, :], S["sv_tm"].res[0]), tsv[bb][:, :])
                ps = tm(1024, 512)
                P.act(ex[bb][:, :], ps[:, :], AF.Exp, scale=-1.0)
                P.ts(ex[bb][:, :], ex[bb][:, :], 1.0, None, ALU.add)
                P.recip(ex[bb][:, :], ex[bb][:, :])
                P.copy(tsg[bb][:, :], ex[bb][:, :], eng="pool")
                P.dma(V(S["sg_tm"].t[r0:r0 + 128, :], S["sg_tm"].res[0]), tsg[bb][:, :])
        P.barrier()


def make_mask(P, C, shape, pattern, base, cm, op, val=1.0, dt=BF16, name="mask"):
    src = C.sb(shape, F32, name=name + "s")
    P.memset(src[:, :], val, eng="pool")
    m = C.sb(shape, dt, name=name)
    P.op("pool", lambda e: e.affine_select(m.t[:, :], src.t[:, :], pattern, op, 0.0, base=base, channel_multiplier=cm),
         reads=(src[:, :],), writes=(m[:, :],))
    return m


def sec_sb(nc, P, T, S, hcT, N=512):
    NB = T // 128
    QB = N // 128
    with ExitStack() as es:
        C = Ctx(nc, P, es)
        setup_consts(P, C)
        one_col = C.sb([128, 1], F32, name="onec")
        P.memset(one_col[:, :], 1.0)
        negtri = make_mask(P, C, [128, 128], [[-1, 128]], 0, 1, ALU.is_ge, val=-1.0, name="ntri")
        negones = C.sb([128, 128], BF16, name="nones")
        P.memset(negones[:, :], -1.0)
        masks = [make_mask(P, C, [128, N], [[1, N]], -r * 128, -1, ALU.is_gt, name=f"m{r}") for r in range(QB)]
        KT = [C.sb([128, T], BF16, name="KT") for _ in range(2)]
        QT = [C.sb([128, T], BF16, name="QT") for _ in range(2)]
        Vv = [C.sb([128, NB, 128], BF16, name="Vv") for _ in range(2)]
        NSLOT = 4
        bankAB = [C.ps([128, 512]) for _ in range(NSLOT)]
        bankO = [C.ps([128, 512]) for _ in range(NSLOT)]
        ezs = [C.sb([128, N], F32, name="ez") for _ in range(NSLOT)]
        sps = [Rot([C.sb([128, N], BF16, name="sp") for _ in range(2)]) for _ in range(NSLOT)]
        wts = [Rot([C.sb([128, N], BF16, name="wt") for _ in range(2)]) for _ in range(NSLOT)]
        Sfs = [C.sb([128, N], F32, name="Sf") for _ in range(NSLOT)]
        Sbs = [Rot([C.sb([128, N], BF16, name="Sb") for _ in range(2)]) for _ in range(NSLOT)]
        obs = [C.sb([128, N], BF16, name="ob") for _ in range(NSLOT)]
        loaded = set()

        def load_head(hd):
            b = hd % 2
            P.dma(KT[b][:, :], V(S["skT"].t[hd, :, :], S["skT"].res[0]))
            P.dma(QT[b][:, :], V(S["sqT"].t[hd, :, :], S["sqT"].res[0]))
            P.dma(Vv[b][:, :, :], V(S["sv_tm"].t.rearrange("(j p) c -> p j c", p=128)[:, :, hd * 128:(hd + 1) * 128],
                                   S["sv_tm"].res[0]))

        def chain(slot, hd, gq):
            b = hd % 2
            q = QT[b][:, gq * N:(gq + 1) * N]
            jmax = gq * QB + QB - 1
            pab, po = bankAB[slot], bankO[slot]
            e, Sf, o = ezs[slot], Sfs[slot], obs[slot]
            sb_prev = None
            for j in range(jmax, -1, -1):
                k = KT[b][:, j * 128:(j + 1) * 128]
                P.mm(pab[:, 0:N], k, q)
                yield
                P.act(e[:, :], pab[:, 0:N], AF.Exp)
                yield
                s_ = sps[slot].next()
                P.act(s_[:, :], e[:, :], AF.Ln, bias=1.0)
                yield
                r = j - gq * QB
                if r >= 0:
                    P.tt(s_[:, :], s_[:, :], masks[r][:, :], ALU.mult, eng="pool")
                    yield
                P.mm(pab[:, 0:N], negtri[:, :], s_[:, :], start=True, stop=False)
                if sb_prev is not None:
                    P.mm(pab[:, 0:N], negones[:, :], sb_prev[:, :], start=False, stop=False)
                P.mm(pab[:, 0:N], k, q, start=False, stop=True)
                yield
                w = wts[slot].next()
                P.act(w[:, :], pab[:, 0:N], AF.Exp)
                yield
                if r >= 0:
                    P.tt(w[:, :], w[:, :], masks[r][:, :], ALU.mult, eng="pool")
                    yield
                P.mm(po[:, 0:N], Vv[b][:, j, :], w[:, :], start=(j == jmax), stop=(j == 0))
                if j > 0:
                    if j == jmax:
                        P.copy(Sf[:, :], s_[:, :])
                    else:
                        P.tt(Sf[:, :], Sf[:, :], s_[:, :], ALU.add)
                    yield
                    sb_prev = Sbs[slot].next()
                    P.copy(sb_prev[:, :], Sf[:, :])
                yield
            P.copy(o[:, :], po[:, 0:N])
            yield
            P.dma(V(hcT.t[512 + hd * 128:512 + (hd + 1) * 128, gq * N:(gq + 1) * N], hcT.res[0]), o[:, :])

        work = [(hd, gq) for hd in range(4) for gq in range(T // N - 1, -1, -1)]
        slots = [None] * NSLOT
        slot_head = [None] * NSLOT
        wi = 0
        while True:
            busy = False
            for sl_ in range(NSLOT):
                if slots[sl_] is None and wi < len(work):
                    hd, gq = work[wi]
                    if not any(slots[z] is not None and slot_head[z] == hd - 2 for z in range(NSLOT)):
                        wi += 1
                        if hd not in loaded:
                            load_head(hd)
                            loaded.add(hd)
                        slots[sl_] = chain(sl_, hd, gq)
                        slot_head[sl_] = hd
                if slots[sl_] is not None:
                    busy = True
                    try:
                        next(slots[sl_])
                    except StopIteration:
                        slots[sl_] = None
            if not busy and wi >= len(work):
                break
        P.barrier()


def sec_mlstm(nc, P, T, S, b_i, b_f, head_gain, hcT):
    NCH = T // 128
    with ExitStack() as es:
        C = Ctx(nc, P, es)
        setup_consts(P, C)
        one4 = C.sb([4, 1], F32, name="one4")
        P.memset(one4[:, :], 1.0)
        ident = make_mask(P, C, [128, 128], [[-1, 128]], 0, 1, ALU.is_equal, dt=F32, name="identf")
        identb = C.sb([128, 128], BF16, name="identb")
        P.copy(identb[:, :], ident[:, :])
        maskLT = make_mask(P, C, [128, 128], [[1, 128]], 0, -1, ALU.is_ge, name="mlt")
        gain_bc = C.sb([128, 512], F32, name="gainbc")
        P.op("sp", lambda e: e.dma_start(out=gain_bc.t[:, :],
                                         in_=head_gain.rearrange("h v -> (h v)").partition_broadcast(128)),
             writes=(gain_bc[:, :],), dma=True)
        tok = C.sb([128, NCH, 12], F32, name="tok")
        with ExitStack() as es2:
            C2 = Ctx(nc, P, es2)
            mi = C2.sb([4, T], F32, name="mi")
            mf = C2.sb([4, T], F32, name="mf")
            P.dma(mi[:, :], V(S["gT"].t[0:4, :], S["gT"].res[0]))
            P.dma(mf[:, :], V(S["gT"].t[4:8, :], S["gT"].res[0]))
            bi = C2.sb([4, 1], F32, name="bi")
            bf = C2.sb([4, 1], F32, name="bf")
            P.op("sp", lambda e: e.dma_start(out=bi.t[:, :], in_=b_i.rearrange("(h o) -> h o", o=1)),
                 writes=(bi[:, :],), dma=True)
            P.op("sp", lambda e: e.dma_start(out=bf.t[:, :], in_=b_f.rearrange("(h o) -> h o", o=1)),
                 writes=(bf[:, :],), dma=True)
            P.ts(bi[:, :], bi[:, :], 1.0 / 15.0, None, ALU.mult)
            P.ts(bf[:, :], bf[:, :], -1.0, None, ALU.mult)
            t1 = C2.sb([4, T], F32, name="t1")
            P.act(t1[:, :], mi[:, :], AF.Tanh, scale=1.0 / 15.0, bias=bi[:, 0:1])
            e1 = C2.sb([4, T], F32, name="e1")
            P.act(e1[:, :], mf[:, :], AF.Exp, scale=-1.0, bias=bf[:, 0:1])
            P.act(e1[:, :], e1[:, :], AF.Ln, bias=one4[:, 0:1])
            P.ts(e1[:, :], e1[:, :], -1.0, None, ALU.mult)
            ones_r = mf
            P.memset(ones_r[:, :], 1.0)
            Fc = C2.sb([4, T], F32, name="Fc")
            P.scan(Fc[:, :], ones_r[:, :], e1[:, :], 0.0, ALU.mult, ALU.add)
            a = mi
            P.stt(a[:, :], t1[:, :], 15.0, Fc[:, :], ALU.mult, ALU.subtract)
            M = t1
            P.scan(M[:, :], a[:, :], a[:, :], 0.0, ALU.max, ALU.max)
            Er = C2.sb([4, T], F32, name="Er")
            Dr = e1
            Gr = ones_r
            gd = C2.sb([4, NCH], F32, name="gd")
            for c in range(NCH):
                sl = slice(c * 128, (c + 1) * 128)
                me = M[:, c * 128 + 127:c * 128 + 128]
                P.ts(Er[:, sl], a[:, sl], me, None, ALU.subtract)
                P.ts(Dr[:, sl], Fc[:, sl], -1.0, me, ALU.mult, ALU.subtract)
                if c == 0:
                    P.ts(gd[:, 0:1], me, -1.0, None, ALU.mult)
                else:
                    P.tt(gd[:, c:c + 1], M[:, c * 128 - 1:c * 128], me, ALU.subtract)
                P.ts(Gr[:, sl], Fc[:, sl], 0.0, gd[:, c:c + 1], ALU.mult, ALU.add)
            P.act(Er[:, :], Er[:, :], AF.Exp)
            P.act(Dr[:, :], Dr[:, :], AF.Exp, scale=2.0)
            P.act(Gr[:, :], Gr[:, :], AF.Exp)
            pst = Rot([C2.ps([128, 512]) for _ in range(2)])
            for c in range(NCH):
                sl = slice(c * 128, (c + 1) * 128)
                ps = pst.next()
                for i, rw in enumerate((Er, Dr, Gr)):
                    P.tr(ps[:, 4 * i:4 * i + 4], rw[:, sl], ident[0:4, 0:4])
                P.copy(tok[:, c, :], ps[:, 0:12])
            P.barrier()
        qT = [C.sb([64, 4, 128], BF16, name="qT") for _ in range(2)]
        kT = [C.sb([64, 4, 128], BF16, name="kT") for _ in range(2)]
        ktm = [C.sb([128, 256], BF16, name="ktm") for _ in range(2)]
        vtm = [C.sb([128, 512], BF16, name="vtm") for _ in range(2)]
        sgt = [C.sb([128, 512], BF16, name="sgt") for _ in range(2)]
        hm = [C.sb([128, 4, 128], BF16, name="hm", nres=4) for _ in range(2)]
        bankA = [C.ps([128, 512]) for _ in range(4)]
        bankB = [C.ps([128, 512]) for _ in range(4)]

        def load_chunk(c):
            b = c % 2
            sl = slice(c * 128, (c + 1) * 128)
            P.dma(qT[b][:, :, :], V(S["mqT"].t.rearrange("h d t -> d h t")[:, :, sl], S["mqT"].res[0]))
            P.dma(kT[b][:, :, :], V(S["mkT"].t.rearrange("h d t -> d h t")[:, :, sl], S["mkT"].res[0]))
            P.dma(ktm[b][:, :], V(S["mk_tm"].t[sl, :], S["mk_tm"].res[0]))
            P.dma(vtm[b][:, :], V(S["mv_tm"].t[sl, :], S["mv_tm"].res[0]))
            P.dma(sgt[b][:, :], V(S["sg_tm"].t[sl, :], S["sg_tm"].res[0]))

        def head_chain(hd):
            pa, pb = bankA[hd], bankB[hd]
            Cst = C.sb([64, 129], F32, name=f"Cst{hd}")
            Cbf = C.sb([64, 129], BF16, name=f"Cbf{hd}")
            s_m = C.sb([128, 128], BF16, name=f"sm{hd}")
            v_e = C.sb([128, 129], BF16, name=f"vt{hd}")
            jk = C.sb([128, 128], BF16, name=f"jk{hd}")
            s_ = C.sb([128, 8], F32, name=f"sc{hd}")
            o_1 = C.sb([128, 128], F32, name=f"o1{hd}")
            o_2 = C.sb([128, 128], F32, name=f"o2{hd}")
            P.memset(Cst[:, :], 0.0)
            yield
            for c in range(NCH):
                b = c % 2
                ecol = tok[:, c, hd:hd + 1]
                dcol = tok[:, c, 4 + hd:5 + hd]
                gcol = tok[0:64, c, 8 + hd:9 + hd]
                P.mm(pa[:, 0:128], kT[b][:, hd, :], qT[b][:, hd, :])
                P.ts(v_e[:, 0:128], vtm[b][:, hd * 128:(hd + 1) * 128], ecol, None, ALU.mult, eng="pool")
                yield
                P.tt(s_m[:, :], pa[:, 0:128], maskLT[:, :], ALU.mult)
                P.copy(v_e[:, 128:129], ecol, eng="pool")
                yield
                P.ts(Cbf[:, :], Cst[:, :], gcol, None, ALU.mult)
                yield
                P.mm(pb[:, 0:129], s_m[:, :], v_e[:, :], start=True, stop=False)
                P.mm(pb[:, 0:129], qT[b][:, hd, :], Cbf[:, :], start=False, stop=True)
                P.mm(pa[0:64, 128:257], ktm[b][:, hd * 64:(hd + 1) * 64], v_e[:, :])
                yield
                P.stt(Cst[:, :], Cst[:, :], gcol, pa[0:64, 128:257], ALU.mult, ALU.add)
                P.act(jk[:, :], pb[:, 0:128], AF.Square, accum=s_[:, 0:1])
                yield
                P.copy(s_[:, 1:2], pb[:, 128:129])
                yield
                P.ts(s_[:, 2:3], s_[:, 1:2], s_[:, 1:2], None, ALU.mult)
                yield
                P.ts(s_[:, 2:3], s_[:, 2:3], dcol, EPS * 128.0, ALU.max, ALU.mult)
                yield
                P.tt(s_[:, 3:4], s_[:, 2:3], s_[:, 0:1], ALU.add)
                yield
                P.act(s_[:, 4:5], s_[:, 3:4], AF.Ln, scale=1.0 / 128.0)
                yield
                P.act(s_[:, 5:6], s_[:, 4:5], AF.Exp, scale=-0.5)
                yield
                P.stt(o_1[:, :], pb[:, 0:128], s_[:, 5:6], gain_bc[:, hd * 128:(hd + 1) * 128], ALU.mult, ALU.mult)
                yield
                P.tt(o_2[:, :], o_1[:, :], sgt[b][:, hd * 128:(hd + 1) * 128], ALU.mult, eng="pool")
                yield
                P.tr(pa[:, 384:512], o_2[:, :], ident[:, :])
                yield
                P.copy(hm[b].v((slice(None), hd, slice(None)), hd), pa[:, 384:512])
                yield "chunk_done"

        chains = [head_chain(hd) for hd in range(4)]
        for ch_ in chains:
            next(ch_)
        load_chunk(0)
        for c in range(NCH):
            if c + 1 < NCH:
                load_chunk(c + 1)
            active = list(chains)
            while active:
                for ch_ in list(active):
                    if next(ch_) == "chunk_done":
                        active.remove(ch_)
            b = c % 2
            sl = slice(c * 128, (c + 1) * 128)
            P.op("sp", lambda e, b=b, sl=sl: e.dma_start(
                out=hcT.t[0:512, :].rearrange("(h p) t -> p h t", p=128)[:, :, sl], in_=hm[b].t[:, :, :]),
                reads=tuple(V(hm[b].t[:, :, :], hm[b].res[k]) for k in range(4)), writes=(hcT[:, :],), dma=True)
        P.barrier()


def sec_pre_c(nc, P, T, xin, mix_norm, w_in, conv_w, S, N=512):
    with ExitStack() as es:
        C = Ctx(nc, P, es)
        setup_consts(P, C)
        g = load_gain(P, C, mix_norm, "gmix")
        w = load_w(P, C, w_in, D, C_IN, "win")
        cw = C.sb([128, 24, 4], F32, name="cw")
        for j in range(4):
            P.op("sp", lambda e, j=j: e.dma_start(out=cw.t[:, :, j], in_=conv_w[j, :].rearrange("(c p) -> p c", p=128),
                                                 allow_slow_non_contiguous=True), writes=(cw[:, :, :],), dma=True)
        ident = make_mask(P, C, [128, 128], [[-1, 128]], 0, 1, ALU.is_equal, dt=BF16, name="identb")
        halo = C.sb([128, 24, 3], BF16, name="halo")
        P.memset(halo[:, :, :], 0.0)
        dg = C.sb([128, 24, 4, 128], BF16, name="dg")
        for ch in range(24):
            for j in range(4):
                P.ts(dg[:, ch, j, :], ident[:, :], cw[:, ch, j:j + 1], None, ALU.mult, eng=("pool" if (ch + j) % 2 else "dve"))
        pss = Rot([C.ps([128, 512]) for _ in range(6)])
        psT = Rot([C.ps([128, 1024], BF16) for _ in range(2)])
        xs = [C.sb([128, 8, N], F32, name="x") for _ in range(1)]
        sq = C.sb([128, 8, N], BF16, name="sq")
        h = C.sb([128, 8, N], BF16, name="h")
        rstd = C.sb([128, N], F32, name="rstd")
        lnt = C.sb([128, N], F32, name="lnt")
        cv = Rot([C.sb([128, N + 3], BF16, name="cv") for _ in range(4)])
        y = C.sb([128, 16, N], F32, name="y", nres=16)
        vb = C.sb([128, 8, N], BF16, name="vb", nres=8)
        kb = C.sb([128, 8, N], BF16, name="kb", nres=8)
        qb = C.sb([128, 8, N], BF16, name="qb")
        s2 = Rot([C.sb([128, N], BF16, name="s2") for _ in range(2)])
        l2 = Rot([C.sb([128, N], F32, name="l2") for _ in range(2)])
        ba = [C.sb([8, N], F32, name="ba") for _ in range(2)]
        tsg = Rot([C.sb([128, 512], BF16, name="tsg") for _ in range(2)])
        ttm = Rot([C.sb([128, 1024], BF16, name="ttm") for _ in range(2)])
        xin_v = xin.t.rearrange("(c p) t -> p c t", p=128)
        for it in range(T // N):
            t0 = it * N
            x = xs[0]
            P.dma(x[:, :, :], V(xin_v[:, :, t0:t0 + N], xin.res[0]))
            rmsnorm_tile(P, C, x, g, h, N, C.ones, pss, sq, rstd, lnt)
            def proj(ch):
                ps = pss.next()
                for c in range(8):
                    P.mm(ps[:, 0:N], w[:, c, ch * 128:(ch + 1) * 128], h[:, c, :], start=(c == 0), stop=(c == 7))
                cvt = cv.next()
                P.act(cvt[:, 3:3 + N], ps[:, 0:N], AF.Copy)
                P.copy(cvt[:, 0:3], halo[:, ch, :])
                P.copy(halo[:, ch, :], cvt[:, N:N + 3])
                return cvt

            def conv(ch, cvt):
                ps2 = pss.next()
                for j in range(4):
                    P.mm(ps2[:, 0:N], dg[:, ch, j, :], cvt[:, j:j + N], start=(j == 0), stop=(j == 3))
                if ch < 16:
                    P.act(y.v((slice(None), ch, slice(None)), ch), ps2[:, 0:N], AF.Silu)
                else:
                    P.act(vb.v((slice(None), ch - 16, slice(None)), ch - 16), ps2[:, 0:N], AF.Silu)

            pend = [proj(0), proj(1)]
            for ch in range(24):
                if ch + 2 < 24:
                    pend.append(proj(ch + 2))
                conv(ch, pend.pop(0))
            for tb in range(N // 128):
                r0 = t0 + tb * 128
                for half in range(2):
                    ps = pss.next()
                    for c in range(8):
                        P.mm(ps[:, :], h[:, c, tb * 128:(tb + 1) * 128], w[:, c, 3072 + half * 512:3072 + (half + 1) * 512],
                             start=(c == 0), stop=(c == 7))
                    tg = tsg.next()
                    P.act(tg[:, :], ps[:, :], AF.Silu)
                    P.dma(V(S["sg_tm"].t[r0:r0 + 128, half * 512:(half + 1) * 512], S["sg_tm"].res[0]), tg[:, :])
            for i, nm in enumerate(("bT", "aT")):
                ps = pss.next()
                for c in range(8):
                    P.mm(ps[0:8, 0:N], w[:, c, 4096 + 8 * i:4104 + 8 * i], h[:, c, :], start=(c == 0), stop=(c == 7))
                P.act(ba[i][:, :], ps[0:8, 0:N], AF.Copy)
                P.dma(V(S[nm].t[:, t0:t0 + N], S[nm].res[0]), ba[i][:, :])
            for ch in range(16):
                yv = y.v((slice(None), ch, slice(None)), ch)
                s_ = s2.next()
                P.act(s_[:, :], yv, AF.Square)
                ps = pss.next()
                P.mm(ps[:, 0:N], C.ones[:, :], s_[:, :])
                l_ = l2.next()
                P.act(l_[:, :], ps[:, 0:N], AF.Ln, bias=C.eps[:, 0:1])
                P.act(l_[:, :], l_[:, :], AF.Exp, scale=-0.5)
                if ch < 8:
                    P.stt(qb[:, ch, :], yv, 128 ** -0.5, l_[:, :], ALU.mult, ALU.mult)
                else:
                    P.tt(kb.v((slice(None), ch - 8, slice(None)), ch - 8), yv, l_[:, :], ALU.mult)
            P.dma(V(S["qT"].t.rearrange("h d t -> d h t")[:, :, t0:t0 + N], S["qT"].res[0]), qb[:, :, :])
            P.dma(V(S["kT"].t.rearrange("h d t -> d h t")[:, :, t0:t0 + N], S["kT"].res[0]),
                  V(kb.t[:, :, :], kb.res[7]))
            for src, nm in ((kb, "k_tm"), (vb, "v_tm")):
                for tb in range(N // 128):
                    r0 = t0 + tb * 128
                    pt = psT.next()
                    for ch in range(8):
                        P.tr(pt[:, ch * 128:(ch + 1) * 128], src.v((slice(None), ch, slice(tb * 128, (tb + 1) * 128)), ch),
                             ident[:, :])
                    tt_ = ttm.next()
                    P.copy(tt_[:, :], pt[:, :])
                    P.dma(V(S[nm].t[r0:r0 + 128, :], S[nm].res[0]), tt_[:, :])
        P.barrier()


def sec_gdn(nc, P, T, S, a_log, dt_bias, head_gain, hcT, SCR):
    NSC = T // 128
    NCH = T // 64
    SEG = min(T, 2048)
    with ExitStack() as es:
        C = Ctx(nc, P, es)
        setup_consts(P, C)
        one8 = C.sb([8, 1], F32, name="one8")
        P.memset(one8[:, :], 1.0)
        identf = make_mask(P, C, [128, 128], [[-1, 128]], 0, 1, ALU.is_equal, dt=F32, name="identf")
        identb = C.sb([128, 128], BF16, name="identb")
        P.copy(identb[:, :], identf[:, :])
        mS = make_mask(P, C, [128, 128], [[-1, 128]], 0, 1, ALU.is_gt, dt=F32, name="mS")
        mI = make_mask(P, C, [128, 128], [[1, 128]], 0, -1, ALU.is_ge, dt=F32, name="mI")
        P.memset(mS[64:128, 0:64], 0.0, eng="pool")
        P.memset(mI[0:64, 64:128], 0.0, eng="pool")
        BIG = 30000.0
        pmS = C.sb([128, 128], F32, name="pmS")
        P.ts(pmS[:, :], mS[:, :], -BIG, BIG, ALU.mult, ALU.add)
        nmI = C.sb([128, 128], F32, name="nmI")
        P.ts(nmI[:, :], mI[:, :], BIG, -BIG, ALU.mult, ALU.add)
        gain_bc = C.sb([128, 128], F32, name="gainbc")
        P.op("sp", lambda e: e.dma_start(out=gain_bc.t[:, :], in_=head_gain.partition_broadcast(128)),
             writes=(gain_bc[:, :],), dma=True)
        tokc = C.sb([128, NSC, 5, 8], F32, name="tokc")
        egl = C.sb([128, 8, NCH], F32, name="egl")
        with ExitStack() as es2:
            C2 = Ctx(nc, P, es2)
            bt = C2.sb([8, T], F32, name="bt")
            at = C2.sb([8, T], F32, name="at")
            P.dma(bt[:, :], V(S["bT"].t[:, :], S["bT"].res[0]))
            P.dma(at[:, :], V(S["aT"].t[:, :], S["aT"].res[0]))
            al = C2.sb([8, 1], F32, name="al")
            db = C2.sb([8, 1], F32, name="db")
            P.op("sp", lambda e: e.dma_start(out=al.t[:, :], in_=a_log.rearrange("(h o) -> h o", o=1)),
                 writes=(al[:, :],), dma=True)
            P.op("sp", lambda e: e.dma_start(out=db.t[:, :], in_=dt_bias.rearrange("(h o) -> h o", o=1)),
                 writes=(db[:, :],), dma=True)
            P.act(al[:, :], al[:, :], AF.Exp)
            P.ts(al[:, :], al[:, :], -1.0, None, ALU.mult)
            P.act(bt[:, :], bt[:, :], AF.Exp, scale=-1.0)
            P.ts(bt[:, :], bt[:, :], 1.0, None, ALU.add)
            P.recip(bt[:, :], bt[:, :])
            P.act(at[:, :], at[:, :], AF.Exp, bias=db[:, 0:1])
            P.act(at[:, :], at[:, :], AF.Ln, bias=one8[:, 0:1])
            P.ts(at[:, :], at[:, :], al[:, 0:1], None, ALU.mult)
            nf = C2.sb([8, T], F32, name="nf")
            P.memset(nf[:, :], 1.0)
            P.memset(V(nf.t[:, :].rearrange("p (c l) -> p c l", l=64)[:, :, 0:1], nf.res[0]), 0.0)
            gr = C2.sb([8, T], F32, name="gr")
            P.scan(gr[:, :], nf[:, :], at[:, :], 0.0, ALU.mult, ALU.add)
            P.dma(SCR["gD"][:, :], gr[:, :])
            eg = C2.sb([8, T], F32, name="eg")
            P.act(eg[:, :], gr[:, :], AF.Exp)
            beg = nf
            P.tt(beg[:, :], bt[:, :], eg[:, :], ALU.mult)
            egg = at
            for c in range(NCH):
                sl = slice(c * 64, (c + 1) * 64)
                P.ts(egg[:, sl], gr[:, sl], -1.0, gr[:, c * 64 + 63:c * 64 + 64], ALU.mult, ALU.add)
            P.act(egg[:, :], egg[:, :], AF.Exp)
            eG = C2.sb([8, NCH], F32, name="eG")
            P.copy(eG[:, :], V(eg.t[:, :].rearrange("p (c l) -> p c l", l=64)[:, :, 63], eg.res[0]))
            P.dma(SCR["eGD"][:, :], eG[:, :])
            P.op("sp", lambda e: e.dma_start(out=egl.t[:, :, :].rearrange("p h c -> p (h c)"),
                                             in_=SCR["eGD"].t.rearrange("h c -> (h c)").partition_broadcast(128)),
                 reads=(SCR["eGD"][:, :],), writes=(egl[:, :, :],), dma=True)
            pst = Rot([C2.ps([128, 512]) for _ in range(2)])
            for sc in range(NSC):
                sl = slice(sc * 128, (sc + 1) * 128)
                ps = pst.next()
                for i, rw in enumerate((gr, bt, beg, egg, eg)):
                    P.tr(ps[:, 8 * i:8 * i + 8], rw[:, sl], identf[0:8, 0:8])
                P.copy(V(tokc.t[:, sc, :, :].rearrange("p a b -> p (a b)"), tokc.res[0]), ps[:, 0:40])
            P.barrier()
        GT = 256
        NG = T // GT
        SPG = GT // 128
        qTg = [C.sb([128, 8, GT], BF16, name="qTg") for _ in range(2)]
        kTg = [C.sb([128, 8, GT], BF16, name="kTg") for _ in range(2)]
        ktg = [C.sb([128, SPG, D], BF16, name="ktg") for _ in range(2)]
        vtg = [C.sb([128, SPG, D], BF16, name="vtg") for _ in range(2)]
        sgg = [C.sb([128, SPG, D], BF16, name="sgg") for _ in range(2)]
        Gbg = [C.sb([128, 8, GT], F32, name="Gbg") for _ in range(2)]
        banks = [C.ps([128, 512]) for _ in range(8)]

        def load_group(gi):
            b = gi % 2
            t0 = gi * GT
            P.dma(qTg[b][:, :, :], V(S["qT"].t.rearrange("h d t -> d h t")[:, :, t0:t0 + GT], S["qT"].res[0]))
            P.dma(kTg[b][:, :, :], V(S["kT"].t.rearrange("h d t -> d h t")[:, :, t0:t0 + GT], S["kT"].res[0]))
            for tl, nm in ((ktg, "k_tm"), (vtg, "v_tm"), (sgg, "sg_tm")):
                P.dma(tl[b][:, :, :], V(S[nm].t[t0:t0 + GT, :].rearrange("(s p) c -> p s c", p=128), S[nm].res[0]))
            for hd in range(8):
                P.op("sp", lambda e, b=b, t0=t0, hd=hd: e.dma_start(
                    out=Gbg[b].t[:, hd, :], in_=SCR["gD"].t[hd, t0:t0 + GT].partition_broadcast(128)),
                    reads=(SCR["gD"][:, :],), writes=(Gbg[b][:, :, :],), dma=True)

        def head_chain(hd):
            bank = banks[hd]

            def t32(name):
                return C.sb([128, 128], F32, name=f"{name}{hd}")

            def t16(name):
                return C.sb([128, 128], BF16, name=f"{name}{hd}")

            dtmp, Da, Dt = t32("dtmp"), t32("Da"), t32("Dt")
            XYs = Rot([C.sb([128, 256], F32, name=f"XY{hd}") for _ in range(2)])
            Qs = Rot([t32("Q"), t32("Q")])
            af, u, t3, o_, o_1, o_2 = t32("af"), t32("u"), t32("t3"), t32("o"), t32("o1"), t32("o2")
            ab_, tb_, v_b, k_b, k_d, wT_, vn, jk, hb = (t16("ab"), t16("tb"), t16("vb"), t16("kb"), t16("kd"),
                                                       t16("wT"), t16("vn"), t16("jk"), t16("hb"))
            Sst = t32("Sst")
            Sbf = Rot([t16("Sbf"), t16("Sbf")])
            s_ = C.sb([128, 4], F32, name=f"scl{hd}")
            P.memset(Sst[:, :], 0.0)
            sbf = Sbf.next()
            P.memset(sbf[:, :], 0.0)
            s0, s1, s2, s3 = (slice(0, 128), slice(128, 256), slice(256, 384), slice(384, 512))
            yield
            for sc in range(NSC):
                gi = (sc * 128) // GT
                b = gi % 2
                si = sc % SPG
                lsl = slice(si * 128, (si + 1) * 128)
                hsl = slice(hd * 128, (hd + 1) * 128)
                kTs = kTg[b][:, hd, lsl]
                qTs = qTg[b][:, hd, lsl]
                gsl = Gbg[b][:, hd, lsl]
                gcol = tokc[:, sc, 0, hd:hd + 1]
                bcol = tokc[:, sc, 1, hd:hd + 1]
                begcol = tokc[:, sc, 2, hd:hd + 1]
                eggcol = tokc[:, sc, 3, hd:hd + 1]
                P.mm(bank[:, s0], kTs, kTs)
                P.mm(bank[:, s1], kTs, qTs)
                P.stt(dtmp[:, :], gsl, gcol, pmS[:, :], ALU.subtract, ALU.max)
                P.act(v_b[:, :], vtg[b][:, si, hsl], AF.Copy, scale=bcol)
                yield
                P.act(Da[:, :], dtmp[:, :], AF.Exp, scale=-1.0)
                yield
                XY = XYs.next()
                X, Y = XY[:, 0:128], XY[:, 128:256]
                P.stt(X, bank[:, s0], bcol, Da[:, :], ALU.mult, ALU.mult)
                P.act(k_b[:, :], ktg[b][:, si, hsl], AF.Copy, scale=begcol)
                yield
                P.tr(bank[:, s2], X, identf[:, :])
                P.stt(dtmp[:, :], gsl, gcol, nmI[:, :], ALU.subtract, ALU.min)
                yield
                P.act(Y, bank[:, s2], AF.Copy)
                yield
                P.act(Dt[:, :], dtmp[:, :], AF.Exp)
                Q = Qs.next()
                P.tt(Q[:, :], identf[:, :], Y, ALU.subtract)
                yield
                P.tt(ab_[:, :], bank[:, s1], Dt[:, :], ALU.mult)
                P.act(k_d[:, :], ktg[b][:, si, hsl], AF.Copy, scale=eggcol)
                yield
                for lvl in range(1, 6):
                    P.mm(bank[:, s0], Y, X)
                    if lvl < 5:
                        P.mm(bank[:, s1], X, Y)
                    yield
                    XYn = XYs.next()
                    if lvl < 5:
                        P.act(XYn[:, 0:256], bank[:, 0:256], AF.Copy)
                    else:
                        P.act(XYn[:, 0:128], bank[:, s0], AF.Copy)
                    Xn, Yn = XYn[:, 0:128], XYn[:, 128:256]
                    yield
                    P.mm(bank[:, s2], Xn, Q[:, :])
                    yield
                    Qn = Qs.next()
                    P.tt(Qn[:, :], bank[:, s2], Q[:, :], ALU.add)
                    yield
                    X, Y, Q = Xn, Yn, Qn
                P.act(tb_[:, :], Q[:, :], AF.Copy)
                yield
                P.mm(bank[:, s0], tb_[:, :], v_b[:, :])
                P.mm(bank[:, s1], k_b[:, :], tb_[:, :])
                yield
                P.act(u[:, :], bank[:, s0], AF.Copy)
                yield
                P.act(wT_[:, :], bank[:, s1], AF.Copy)
                yield
                for cc in range(2):
                    pr = slice(cc * 64, (cc + 1) * 64)
                    isl = slice(si * 128 + cc * 64, si * 128 + (cc + 1) * 64)
                    ch = sc * 2 + cc
                    P.mm(bank[pr, s2], wT_[:, pr], sbf[:, :])
                    P.mm(bank[pr, s3], qTg[b][:, hd, isl], sbf[:, :])
                    yield
                    P.tt(vn[pr, :], u[pr, :], bank[pr, s2], ALU.subtract)
                    yield
                    P.mm(bank[pr, s0], ab_[pr, pr], vn[pr, :])
                    P.mm(bank[:, s1], k_d[pr, :], vn[pr, :])
                    yield
                    P.stt(Sst[:, :], Sst[:, :], egl[:, hd, ch:ch + 1], bank[:, s1], ALU.mult, ALU.add)
                    yield
                    sbf = Sbf.next()
                    P.act(sbf[:, :], Sst[:, :], AF.Copy)
                    P.act(t3[pr, :], bank[pr, s0], AF.Copy)
                    yield
                    P.stt(o_[pr, :], bank[pr, s3], V(tokc.t[pr, sc, 4, hd:hd + 1], tokc.res[0]), t3[pr, :],
                          ALU.mult, ALU.add)
                    yield
                P.act(jk[:, :], o_[:, :], AF.Square, accum=s_[:, 0:1])
                yield
                P.act(s_[:, 1:2], s_[:, 0:1], AF.Ln, scale=1.0 / 128.0, bias=C.eps[:, 0:1])
                yield
                P.act(s_[:, 2:3], s_[:, 1:2], AF.Exp, scale=-0.5)
                yield
                P.stt(o_1[:, :], o_[:, :], s_[:, 2:3], gain_bc[:, :], ALU.mult, ALU.mult)
                yield
                P.tt(o_2[:, :], o_1[:, :], sgg[b][:, si, hsl], ALU.mult, eng="pool")
                yield
                P.tr(bank[:, s2], o_2[:, :], identf[:, :])
                yield
                P.act(hb[:, :], bank[:, s2], AF.Copy)
                yield
                P.dma(V(hcT.t[hd * 128:(hd + 1) * 128, sc * 128:(sc + 1) * 128], hcT.res[0]), hb[:, :])
                yield "sc_done"

        chains = [head_chain(hd) for hd in range(8)]
        for ch_ in chains:
            next(ch_)
        load_group(0)
        for sc in range(NSC):
            if (sc * 128) % GT == 0:
                gi = (sc * 128) // GT
                if gi + 1 < NG:
                    load_group(gi + 1)
            active = list(chains)
            while active:
                for ch_ in list(active):
                    if next(ch_) == "sc_done":
                        active.remove(ch_)
        P.barrier()


W_SHAPES = dict(
    mix_norm=[4, D], ab_w_in=[2, D, AB_IN], ab_b_i=[2, 4], ab_b_f=[2, 4], ab_head_gain=[2, 4, 128],
    ab_w_out=[2, D, D], c_w_in=[2, D, C_IN], c_conv_w=[2, 4, 3072], c_a_log=[2, 8], c_dt_bias=[2, 8],
    c_head_gain=[2, 128], c_w_out=[2, D, D], xa_norm=[4, D], mem_norm=[D], xa_wq=[4, D, D], xa_wk=[4, D, D],
    xa_wv=[4, D, D], xa_wo=[4, D, D], mlp_norm=[4, D], mlp_w1=[4, D, DFF], mlp_w2=[4, DFF, D], final_norm=[D])


def build_program(T, depth=DEPTH):
    nc = bass.Bass("TRN2", target_bir_lowering=False)
    P = Prog(nc)
    xT = dram(nc, "xT", [D, T], F32, kind="ExternalInput")
    memT = dram(nc, "memT", [D, MEM], F32, kind="ExternalInput")
    W = {n: nc.dram_tensor(n, shp, F32, kind="ExternalInput").ap() for n, shp in W_SHAPES.items()}
    outT = dram(nc, "outT", [D, T], F32, kind="ExternalOutput")
    xA = dram(nc, "xA", [D, T], F32)
    xB = dram(nc, "xB", [D, T], F32)
    hcT = dram(nc, "hcT", [D, T], BF16)
    memnT = dram(nc, "memnT", [D, MEM], BF16)
    SA = dict(mqT=dram(nc, "mqT", [4, 64, T], BF16), mkT=dram(nc, "mkT", [4, 64, T], BF16), gT=dram(nc, "gT", [8, T], F32),
              sqT=dram(nc, "sqT", [4, 128, T], BF16), skT=dram(nc, "skT", [4, 128, T], BF16),
              mk_tm=dram(nc, "mk_tm", [T, 256], BF16), mv_tm=dram(nc, "mv_tm", [T, 512], BF16),
              sg_tm=dram(nc, "sg_tm", [T, 512], BF16), sv_tm=dram(nc, "sv_tm", [T, 512], BF16))
    SC = dict(qT=dram(nc, "cqT", [8, 128, T], BF16), kT=dram(nc, "ckT", [8, 128, T], BF16),
              k_tm=dram(nc, "ck_tm", [T, D], BF16), v_tm=dram(nc, "cv_tm", [T, D], BF16),
              sg_tm=dram(nc, "csg_tm", [T, D], BF16), bT=dram(nc, "cbT", [8, T], F32), aT=dram(nc, "caT", [8, T], F32))
    SCR = dict(gD=dram(nc, "gD", [8, T], F32), eGD=dram(nc, "eGD", [8, T // 64], F32))
    sec_memn(nc, P, memT, W["mem_norm"], memnT)
    cur = xT
    for l in range(depth):
        j = l // 2
        if l % 2 == 0:
            sec_pre_ab(nc, P, T, cur, W["mix_norm"][l], W["ab_w_in"][j], SA)
            sec_sb(nc, P, T, SA, hcT)
            sec_mlstm(nc, P, T, SA, W["ab_b_i"][j], W["ab_b_f"][j], W["ab_head_gain"][j], hcT)
            w_out = W["ab_w_out"][j]
        else:
            sec_pre_c(nc, P, T, cur, W["mix_norm"][l], W["c_w_in"][j], W["c_conv_w"][j], SC)
            sec_gdn(nc, P, T, SC, W["c_a_log"][j], W["c_dt_bias"][j], W["c_head_gain"][j], hcT, SCR)
            w_out = W["c_w_out"][j]
        sec_post_a(nc, P, T, cur, hcT, xB, w_out, W["xa_norm"][l], W["xa_wq"][l], W["xa_wk"][l], W["xa_wv"][l],
                   W["xa_wo"][l], memnT)
        last = (l == depth - 1)
        sec_post_b(nc, P, T, xB, outT if last else xA, W["mlp_norm"][l], W["mlp_w1"][l], W["mlp_w2"][l],
                   final_norm=W["final_norm"] if last else None)
        cur = xA
    P.finalize()
    return nc, P


def kernel(**inputs):
    x = np.asarray(inputs["x"], dtype=np.float32)
    mem = np.asarray(inputs["mem"], dtype=np.float32)
    B, T, _ = x.shape
    nc, _ = build_program(T)
    wts = {n: np.ascontiguousarray(np.asarray(inputs[n], dtype=np.float32)) for n in W_SHAPES}
    in_maps = []
    for b in range(B):
        m = dict(wts)
        m["xT"] = np.ascontiguousarray(x[b].T)
        m["memT"] = np.ascontiguousarray(mem[b].T)
        in_maps.append(m)
    res = run_bass_kernel_spmd(nc, in_maps, core_ids=list(range(B)))
    out = np.stack([np.ascontiguousarray(r["outT"].T) for r in res.results], axis=0)
    return out.astype(np.float32)
```

```python
import numpy as np
from contextlib import ExitStack
import concourse.bass as bass
import concourse.mybir as mybir
from concourse.bass_utils import run_bass_kernel_spmd

F32 = mybir.dt.float32
BF16 = mybir.dt.bfloat16
AF = mybir.ActivationFunctionType
ALU = mybir.AluOpType

D = 1024
DEPTH = 4
MEM = 256
EPS = 1e-6
AB_IN = 3080
C_IN = 4112
DFF = 4096
LIM = 30000
DLIM = 1800
SAME_ENGINE_SYNC = True


class Res:
    __slots__ = ("w", "r", "x")

    def __init__(self, x=False):
        self.w = {}
        self.r = {}
        self.x = x


class V:
    __slots__ = ("ap", "res")

    def __init__(self, ap, res):
        self.ap = ap
        self.res = res


class Tl:
    def __init__(self, t, nres=1, x=False):
        self.t = t
        self.res = [Res(x) for _ in range(nres)]

    def __getitem__(self, idx):
        return V(self.t[idx], self.res[0])

    def v(self, idx, k=0):
        return V(self.t[idx], self.res[k])


class Prog:
    ENGS = ("pe", "act", "dve", "pool", "sp")

    def __init__(self, nc):
        self.nc = nc
        self.lists = {e: [] for e in self.ENGS}
        self.cnt = {}
        self.seen = {e: {} for e in self.ENGS}

    DMA_SLOTS = {"sp": 24, "pool": 12, "act": 8}

    def op(self, eng, fn, reads=(), writes=(), dma=False):
        if dma:
            tot = self.cnt.get(("dq", eng), 0)
            self.cnt[("dq", eng)] = tot + 1
            key = ("d", eng, tot % self.DMA_SLOTS[eng])
        else:
            key = ("c", eng)
        n = self.cnt.get(key, 0) + 1
        self.cnt[key] = n
        waits = {}
        seen = self.seen[eng]
        if dma and n > 1 and n - 1 > seen.get(key, 0):
            waits[key] = n - 1

        def need(k, v):
            if k[0] == "c" and k[1] == eng and (eng == "pe" or not SAME_ENGINE_SYNC):
                return
            if v > seen.get(k, 0) and v > waits.get(k, 0):
                waits[k] = v

        xr = [r for r in reads if r.res.x]
        if xr:
            writes = tuple(writes) + tuple(xr)
        for r in reads:
            for k, v in r.res.w.items():
                need(k, v)
        for w in writes:
            for k, v in w.res.w.items():
                need(k, v)
            for k, v in w.res.r.items():
                need(k, v)
        for k, v in waits.items():
            seen[k] = v
        self.lists[eng].append((list(waits.items()), fn, key, n))
        for r in reads:
            if r.res.r.get(key, 0) < n:
                r.res.r[key] = n
        for w in writes:
            w.res.w = {key: n}
            w.res.r = {}

    def barrier(self):
        snap = {k: v for k, v in self.cnt.items() if k[0] != "dq"}
        for e in self.ENGS:
            waits = []
            for k, v in snap.items():
                if k[0] == "c" and k[1] == e:
                    continue
                if v > self.seen[e].get(k, 0):
                    waits.append((k, v))
                    self.seen[e][k] = v
            if waits:
                self.lists[e].append((waits, None, None, 0))

    def finalize(self):
        nc = self.nc
        self.barrier()
        with ExitStack() as es:
            sems = {}
            for key, n in self.cnt.items():
                if key[0] == "dq":
                    continue
                lim = DLIM if key[0] == "d" else LIM
                for g in range((n - 1) // lim + 1):
                    sems[(key, g)] = es.enter_context(nc.semaphore("s" + "_".join(str(z) for z in key) + f"_{g}"))
            block = es.enter_context(nc.Block())

            def run(name, eng):
                for waits, fn, key, n in self.lists[name]:
                    for (k, v) in waits:
                        lim = DLIM if k[0] == "d" else LIM
                        g = (v - 1) // lim
                        val = v - g * lim
                        if k[0] == "d":
                            if g > 0:
                                eng.wait_ge(sems[(k, g - 1)], lim * 16)
                            eng.wait_ge(sems[(k, g)], val * 16)
                        else:
                            eng.wait_ge(sems[(k, g)], val)
                    if fn is not None:
                        inst = fn(eng)
                        lim = DLIM if key[0] == "d" else LIM
                        g = (n - 1) // lim
                        inst.then_inc(sems[(key, g)], 16 if key[0] == "d" else 1)

            @block.tensor
            def _(e):
                run("pe", e)

            @block.scalar
            def _(e):
                run("act", e)

            @block.vector
            def _(e):
                run("dve", e)

            @block.gpsimd
            def _(e):
                run("pool", e)

            @block.sync
            def _(e):
                run("sp", e)

    def mm(self, out, lhsT, rhs, start=True, stop=True):
        self.op("pe", lambda e: e.matmul(out.ap, lhsT.ap, rhs.ap, start=start, stop=stop),
                reads=(lhsT, rhs) if start else (lhsT, rhs, out), writes=(out,))

    def tr(self, out, in_, ident):
        self.op("pe", lambda e: e.transpose(out.ap, in_.ap, ident.ap), reads=(in_, ident), writes=(out,))

    def act(self, out, in_, func, scale=1.0, bias=0.0, accum=None, eng="act"):
        rd = [in_]
        if isinstance(scale, V):
            rd.append(scale)
        if isinstance(bias, V):
            rd.append(bias)
        sc = scale.ap if isinstance(scale, V) else scale
        bi = bias.ap if isinstance(bias, V) else bias
        wr = [out]
        if accum is not None:
            wr.append(accum)
        acc = accum.ap if accum is not None else None
        self.op("act", lambda e: e.activation(out.ap, in_.ap, func, bias=bi, scale=sc, accum_out=acc),
                reads=rd, writes=wr)

    def tt(self, out, a, b, op, eng="dve"):
        self.op(eng, lambda e: e.tensor_tensor(out.ap, a.ap, b.ap, op), reads=(a, b), writes=(out,))

    def ts(self, out, a, s1, s2, op0, op1=None, eng="dve", accum=None):
        rd = [a]
        if isinstance(s1, V):
            rd.append(s1)
        if isinstance(s2, V):
            rd.append(s2)
        v1 = s1.ap if isinstance(s1, V) else s1
        v2 = s2.ap if isinstance(s2, V) else s2
        wr = [out]
        if accum is not None:
            wr.append(accum)
        acc = accum.ap if accum is not None else None
        if op1 is None:
            self.op(eng, lambda e: e.tensor_scalar(out.ap, a.ap, v1, v2, op0, accum_out=acc) if acc is not None
                    else e.tensor_scalar(out.ap, a.ap, v1, v2, op0), reads=rd, writes=wr)
        else:
            self.op(eng, lambda e: e.tensor_scalar(out.ap, a.ap, v1, v2, op0, op1, accum_out=acc) if acc is not None
                    else e.tensor_scalar(out.ap, a.ap, v1, v2, op0, op1), reads=rd, writes=wr)

    def stt(self, out, a, s, b, op0, op1):
        rd = [a, b]
        if isinstance(s, V):
            rd.append(s)
        sv = s.ap if isinstance(s, V) else s
        self.op("dve", lambda e: e.scalar_tensor_tensor(out.ap, a.ap, sv, b.ap, op0, op1), reads=rd, writes=(out,))

    def scan(self, out, d0, d1, initial, op0, op1):
        rd = [d0, d1]
        if isinstance(initial, V):
            rd.append(initial)
        iv = initial.ap if isinstance(initial, V) else initial
        self.op("dve", lambda e: e.tensor_tensor_scan(out.ap, d0.ap, d1.ap, iv, op0, op1), reads=rd, writes=(out,))

    def copy(self, out, in_, eng="dve"):
        self.op(eng, lambda e: e.tensor_copy(out.ap, in_.ap), reads=(in_,), writes=(out,))

    def recip(self, out, in_):
        self.op("dve", lambda e: e.reciprocal(out.ap, in_.ap), reads=(in_,), writes=(out,))

    def memset(self, out, val, eng="dve"):
        self.op(eng, lambda e: e.memset(out.ap, val), writes=(out,))

    def dma(self, out, in_, q="sp", **kw):
        self.op(q, lambda e: e.dma_start(out=out.ap, in_=in_.ap, **kw), reads=(in_,), writes=(out,), dma=True)


class Ctx:
    _uid = [0]

    def __init__(self, nc, P, es):
        self.nc, self.P, self.es = nc, P, es
        self.n = 0
        Ctx._uid[0] += 1
        self.uid = Ctx._uid[0]

    def sb(self, shape, dt, nres=1, name=None):
        self.n += 1
        t = self.es.enter_context(self.nc.sbuf_tensor(f"{name or 't'}_{self.uid}_{self.n}", list(shape), dt))
        return Tl(t, nres)

    def ps(self, shape, dt=F32, name=None):
        self.n += 1
        assert shape[0] == 128 and shape[1] * (4 if dt == F32 else 2) == 2048, "PSUM tiles are whole banks"
        t = self.es.enter_context(self.nc.psum_tensor(f"{name or 'p'}_{self.uid}_{self.n}", list(shape), dt))
        return Tl(t, x=True)


class Rot:
    def __init__(self, items):
        self.items = items
        self.i = 0

    def next(self):
        t = self.items[self.i % len(self.items)]
        self.i += 1
        return t


def dram(nc, name, shape, dt, kind=None):
    if kind is None:
        t = nc.dram_tensor(name, list(shape), dt)
    else:
        t = nc.dram_tensor(name, list(shape), dt, kind=kind)
    return Tl(t.ap())


def load_w(P, C, w_dram_ap, K, N, name, q="pool", res=None, step=2048, colmajor=False, nres=1):
    kc = K // 128
    w = C.sb([128, kc, N], BF16, name=name, nres=nres) if not isinstance(C, Tl) else C
    src = w_dram_ap.rearrange("(c p) n -> p c n", p=128)
    blocks = [(n0, min(N, n0 + step)) for n0 in range(0, N, step)]
    order = [(c, blk) for blk in blocks for c in range(kc)] if colmajor else [(c, blk) for c in range(kc) for blk in blocks]
    for c, (n0, n1) in order:
        k = (n0 // step) % nres if nres > 1 else 0
        P.dma(V(w.t[:, c, n0:n1], w.res[k]), V(src[:, c, n0:n1], res or Res()), q=q)
    return w


def rmsnorm_tile(P, C, x, g, h, N, ones, ps_rot, sq, rstd, lnt):
    if isinstance(sq, Tl):
        P.act(sq[:, :, :], x[:, :, :], AF.Square)
        sq = [sq[:, c, :] for c in range(8)]
    else:
        for c in range(8):
            P.act(sq[c], x[:, c, :], AF.Square)
    ps = ps_rot.next()
    for c in range(8):
        P.mm(ps[:, 0:N], ones[:, :], sq[c], start=(c == 0), stop=(c == 7))
    P.act(lnt[:, :], ps[:, 0:N], AF.Ln, scale=1.0 / D, bias=C.eps[:, 0:1])
    P.act(rstd[:, :], lnt[:, :], AF.Exp, scale=-0.5)
    for c in range(8):
        P.stt(h[:, c, :], x[:, c, :], g[:, c:c + 1], rstd[:, :], ALU.mult, ALU.mult)


def setup_consts(P, C):
    C.ones = C.sb([128, 128], BF16, name="ones")
    P.memset(C.ones[:, :], 1.0)
    C.eps = C.sb([128, 1], F32, name="eps")
    P.memset(C.eps[:, :], EPS)


def load_gain(P, C, g_dram_row_ap, name):
    g = C.sb([128, 8], F32, name=name)
    P.op("sp", lambda e: e.dma_start(out=g.t[:, :], in_=g_dram_row_ap.rearrange("(c p) -> p c", p=128),
                                     allow_slow_non_contiguous=True), writes=(g[:, :],), dma=True)
    return g


def sec_memn(nc, P, memT, mem_norm, memnT_out):
    with ExitStack() as es:
        C = Ctx(nc, P, es)
        setup_consts(P, C)
        g = load_gain(P, C, mem_norm, "gmem")
        x = C.sb([128, 8, MEM], F32)
        P.dma(x[:, :, :], V(memT.t.rearrange("(c p) t -> p c t", p=128), memT.res[0]))
        sq = C.sb([128, 8, MEM], BF16)
        rstd = C.sb([128, MEM], F32)
        lnt = C.sb([128, MEM], F32)
        h = C.sb([128, 8, MEM], BF16)
        ps = C.ps([128, 512])
        rmsnorm_tile(P, C, x, g, h, MEM, C.ones, Rot([ps]), sq, rstd, lnt)
        P.dma(V(memnT_out.t.rearrange("(c p) t -> p c t", p=128), memnT_out.res[0]), h[:, :, :])
        P.barrier()


def sec_post_a(nc, P, T, xin, hc, xout, w_out, xa_norm, wq, wk, wv, wo, memnT, N=512):
    with ExitStack() as es:
        C = Ctx(nc, P, es)
        setup_consts(P, C)
        g = load_gain(P, C, xa_norm, "gxa")
        kT = C.sb([128, 8, MEM], BF16, name="kT")
        v_sb = C.sb([128, 2, D], BF16, name="vsb")
        pss = Rot([C.ps([128, 512]) for _ in range(8)])
        wout_sb = C.sb([128, 8, D], BF16, name="wout")
        wq_sb = C.sb([128, 8, D], BF16, name="wq")
        wo_sb = C.sb([128, 8, D], BF16, name="wo")
        with ExitStack() as es2:
            C2 = Ctx(nc, P, es2)
            wk_sb = load_w(P, C2, wk, D, D, "wk")
            wv_sb = load_w(P, C2, wv, D, D, "wv")
            load_w(P, wout_sb, w_out, D, D, "wout")
            load_w(P, wq_sb, wq, D, D, "wq")
            load_w(P, wo_sb, wo, D, D, "wo")
            mn = C2.sb([128, 8, MEM], BF16, name="mn")
            P.dma(mn[:, :, :], V(memnT.t.rearrange("(c p) t -> p c t", p=128), memnT.res[0]))
            for o in range(8):
                ps = pss.next()
                for c in range(8):
                    P.mm(ps[:, 0:MEM], wk_sb[:, c, o * 128:(o + 1) * 128], mn[:, c, :], start=(c == 0), stop=(c == 7))
                P.act(kT[:, o, :], ps[:, 0:MEM], AF.Copy)
            for mb in range(2):
                for half in range(2):
                    ps = pss.next()
                    for c in range(8):
                        P.mm(ps[:, :], mn[:, c, mb * 128:(mb + 1) * 128], wv_sb[:, c, half * 512:(half + 1) * 512],
                             start=(c == 0), stop=(c == 7))
                    P.copy(v_sb[:, mb, half * 512:(half + 1) * 512], ps[:, :])
            P.barrier()
        xin_v = xin.t.rearrange("(c p) t -> p c t", p=128)
        hc_v = hc.t.rearrange("(c p) t -> p c t", p=128)
        xout_v = xout.t.rearrange("(c p) t -> p c t", p=128)
        NCHAIN = 2

        def tile_chain(k):
            x = C.sb([128, 8, N], F32, name=f"x{k}")
            hcx = C.sb([128, 8, N], BF16, name=f"hc{k}")
            sq = C.sb([128, 8, N], BF16, name=f"sq{k}")
            h = C.sb([128, 8, N], BF16, name=f"h{k}")
            qs = C.sb([128, 8, N], BF16, name=f"q{k}", nres=8)
            os_ = C.sb([128, 8, N], BF16, name=f"o{k}", nres=8)
            rstd = C.sb([128, N], F32, name=f"rstd{k}")
            lnt = C.sb([128, N], F32, name=f"lnt{k}")
            pT = [C.sb([128, N], BF16, name=f"pT{k}") for _ in range(4)]
            rden = [C.sb([128, N], F32, name=f"rden{k}") for _ in range(2)]
            yield
            for it in range(k, T // N, NCHAIN):
                t0 = it * N
                P.dma(x[:, :, :], V(xin_v[:, :, t0:t0 + N], xin.res[0]))
                P.dma(hcx[:, :, :], V(hc_v[:, :, t0:t0 + N], hc.res[0]))
                for o in range(8):
                    ps = pss.next()
                    for c in range(8):
                        P.mm(ps[:, 0:N], wout_sb[:, c, o * 128:(o + 1) * 128], hcx[:, c, :], start=(c == 0), stop=(c == 7))
                    P.tt(x[:, o, :], x[:, o, :], ps[:, 0:N], ALU.add)
                    yield
                P.act(sq[:, :, :], x[:, :, :], AF.Square)
                yield
                ps = pss.next()
                for c in range(8):
                    P.mm(ps[:, 0:N], C.ones[:, :], sq[:, c, :], start=(c == 0), stop=(c == 7))
                P.act(lnt[:, :], ps[:, 0:N], AF.Ln, scale=1.0 / D, bias=C.eps[:, 0:1])
                yield
                P.act(rstd[:, :], lnt[:, :], AF.Exp, scale=-0.5)
                yield
                for c in range(8):
                    P.stt(h[:, c, :], x[:, c, :], g[:, c:c + 1], rstd[:, :], ALU.mult, ALU.mult)
                    if c % 2 == 1:
                        yield
                for o in range(8):
                    ps = pss.next()
                    for c in range(8):
                        P.mm(ps[:, 0:N], wq_sb[:, c, o * 128:(o + 1) * 128], h[:, c, :], start=(c == 0), stop=(c == 7))
                    P.act(qs.v((slice(None), o, slice(None)), o), ps[:, 0:N], AF.Copy, scale=1.0 / 16.0)
                    yield
                for hd in range(4):
                    pts = []
                    for mb in range(2):
                        ps = pss.next()
                        for j in range(2):
                            P.mm(ps[:, 0:N], kT[:, 2 * hd + j, mb * 128:(mb + 1) * 128],
                                 qs.v((slice(None), 2 * hd + j, slice(None)), 2 * hd + j), start=(j == 0), stop=(j == 1))
                        pt = pT[(2 * hd + mb) % 4]
                        P.act(pt[:, :], ps[:, 0:N], AF.Exp)
                        pts.append(pt)
                    yield
                    ps = pss.next()
                    for mb in range(2):
                        P.mm(ps[:, 0:N], C.ones[:, :], pts[mb][:, :], start=(mb == 0), stop=(mb == 1))
                    rd = rden[hd % 2]
                    P.recip(rd[:, :], ps[:, 0:N])
                    yield
                    for j in range(2):
                        ps = pss.next()
                        for mb in range(2):
                            P.mm(ps[:, 0:N], v_sb[:, mb, (2 * hd + j) * 128:(2 * hd + j + 1) * 128], pts[mb][:, :],
                                 start=(mb == 0), stop=(mb == 1))
                        P.tt(os_.v((slice(None), 2 * hd + j, slice(None)), 2 * hd + j), ps[:, 0:N], rd[:, :], ALU.mult)
                    yield
                for o in range(8):
                    ps = pss.next()
                    for c in range(8):
                        P.mm(ps[:, 0:N], wo_sb[:, c, o * 128:(o + 1) * 128], os_.v((slice(None), c, slice(None)), c),
                             start=(c == 0), stop=(c == 7))
                    P.tt(x[:, o, :], x[:, o, :], ps[:, 0:N], ALU.add)
                    yield
                P.dma(V(xout_v[:, :, t0:t0 + N], xout.res[0]), x[:, :, :], q="sp")
                yield

        chains = [tile_chain(k) for k in range(NCHAIN)]
        for ch_ in chains:
            next(ch_)
        active = list(chains)
        stagger = 20
        steps = 0
        while active:
            for idx, ch_ in enumerate(list(active)):
                if ch_ is chains[1] and steps < stagger:
                    continue
                try:
                    next(ch_)
                except StopIteration:
                    active.remove(ch_)
            steps += 1
        P.barrier()


def sec_post_b(nc, P, T, xin, xout, mlp_norm, w1, w2, final_norm=None, N=512):
    with ExitStack() as es:
        C = Ctx(nc, P, es)
        setup_consts(P, C)
        g = load_gain(P, C, mlp_norm, "gmlp")
        gf = load_gain(P, C, final_norm, "gfin") if final_norm is not None else None
        w1_sb = load_w(P, C, w1, D, DFF, "w1", step=512, colmajor=True, nres=8)
        w2_sb = load_w(P, C, w2, DFF, D, "w2")
        pss = Rot([C.ps([128, 512]) for _ in range(8)])
        xs = [C.sb([128, 8, N], F32, name="x") for _ in range(1)]
        h = C.sb([128, 8, N], BF16, name="h")
        gg = C.sb([128, 32, N], BF16, name="gg", nres=32)
        sq = [gg.v((slice(None), 24 + c, slice(None)), 24 + c) for c in range(8)]
        rr = [C.sb([128, N], F32, name="rr") for _ in range(3)]
        rstd = C.sb([128, N], F32, name="rstd")
        lnt = C.sb([128, N], F32, name="lnt")
        xin_v = xin.t.rearrange("(c p) t -> p c t", p=128)
        xout_v = xout.t.rearrange("(c p) t -> p c t", p=128)
        for it in range(T // N):
            t0 = it * N
            x = xs[0]
            P.dma(x[:, :, :], V(xin_v[:, :, t0:t0 + N], xin.res[0]))
            rmsnorm_tile(P, C, x, g, h, N, C.ones, pss, sq, rstd, lnt)
            for f in range(32):
                ps = pss.next()
                for c in range(8):
                    P.mm(ps[:, 0:N], w1_sb.v((slice(None), c, slice(f * 128, (f + 1) * 128)), f // 4), h[:, c, :],
                         start=(c == 0), stop=(c == 7))
                r = rr[f % 3]
                P.act(r[:, :], ps[:, 0:N], AF.Relu)
                P.tt(gg.v((slice(None), f, slice(None)), f), r[:, :], r[:, :], ALU.mult, eng="pool")
            for o in range(8):
                ps = pss.next()
                for f in range(32):
                    P.mm(ps[:, 0:N], w2_sb[:, f, o * 128:(o + 1) * 128], gg.v((slice(None), f, slice(None)), f),
                         start=(f == 0), stop=(f == 31))
                P.tt(x[:, o, :], x[:, o, :], ps[:, 0:N], ALU.add)
            if gf is not None:
                for c in range(8):
                    P.act(sq[c], x[:, c, :], AF.Square)
                ps = pss.next()
                for c in range(8):
                    P.mm(ps[:, 0:N], C.ones[:, :], sq[c], start=(c == 0), stop=(c == 7))
                P.act(lnt[:, :], ps[:, 0:N], AF.Ln, scale=1.0 / D, bias=C.eps[:, 0:1])
                P.act(rstd[:, :], lnt[:, :], AF.Exp, scale=-0.5)
                for c in range(8):
                    P.stt(x[:, c, :], x[:, c, :], gf[:, c:c + 1], rstd[:, :], ALU.mult, ALU.mult)
            P.dma(V(xout_v[:, :, t0:t0 + N], xout.res[0]), x[:, :, :], q="sp")
        P.barrier()


def sec_pre_ab(nc, P, T, xin, mix_norm, w_in, S, N=512):
    with ExitStack() as es:
        C = Ctx(nc, P, es)
        setup_consts(P, C)
        g = load_gain(P, C, mix_norm, "gmix")
        w = load_w(P, C, w_in, D, AB_IN, "win")
        pss = Rot([C.ps([128, 512]) for _ in range(8)])
        xs = [C.sb([128, 8, N], F32, name="x") for _ in range(2)]
        sq = C.sb([128, 8, N], BF16, name="sq")
        h = C.sb([128, 8, N], BF16, name="h")
        rstd = C.sb([128, N], F32, name="rstd")
        lnt = C.sb([128, N], F32, name="lnt")
        mq_sb = [C.sb([64, 4, N], BF16, name="mq") for _ in range(2)]
        mk_sb = [C.sb([64, 4, N], BF16, name="mk") for _ in range(2)]
        g_sb = [C.sb([8, N], F32, name="gs") for _ in range(2)]
        sq_sb = [C.sb([128, 4, N], BF16, name="sqs") for _ in range(2)]
        sk_sb = [C.sb([128, 4, N], BF16, name="sks") for _ in range(2)]
        tmk = [C.sb([128, 256], BF16, name="tmk") for _ in range(2)]
        tmv = [C.sb([128, 512], BF16, name="tmv") for _ in range(2)]
        tsv = [C.sb([128, 512], BF16, name="tsv") for _ in range(2)]
        tsg = [C.sb([128, 512], BF16, name="tsg") for _ in range(2)]
        ex = [C.sb([128, 512], F32, name="ex") for _ in range(2)]
        xin_v = xin.t.rearrange("(c p) t -> p c t", p=128)
        for it in range(T // N):
            t0 = it * N
            b = it % 2
            x = xs[b]
            P.dma(x[:, :, :], V(xin_v[:, :, t0:t0 + N], xin.res[0]))
            rmsnorm_tile(P, C, x, g, h, N, C.ones, pss, sq, rstd, lnt)

            def fm(col0, M, dst, scale=1.0):
                ps = pss.next()
                for c in range(8):
                    P.mm(ps[0:M, 0:N], w[:, c, col0:col0 + M], h[:, c, :], start=(c == 0), stop=(c == 7))
                P.act(dst, ps[0:M, 0:N], AF.Copy, scale=scale)

            for hd in range(4):
                fm(64 * hd, 64, mq_sb[b][:, hd, :])
                fm(256 + 64 * hd, 64, mk_sb[b][:, hd, :], 0.125)
                fm(1544 + 128 * hd, 128, sq_sb[b][:, hd, :], 128 ** -0.5)
                fm(2056 + 128 * hd, 128, sk_sb[b][:, hd, :])
            fm(1536, 8, g_sb[b][:, :])
            P.dma(V(S["mqT"].t.rearrange("h d t -> d h t")[:, :, t0:t0 + N], S["mqT"].res[0]), mq_sb[b][:, :, :])
            P.dma(V(S["mkT"].t.rearrange("h d t -> d h t")[:, :, t0:t0 + N], S["mkT"].res[0]), mk_sb[b][:, :, :])
            P.dma(V(S["sqT"].t.rearrange("h d t -> d h t")[:, :, t0:t0 + N], S["sqT"].res[0]), sq_sb[b][:, :, :])
            P.dma(V(S["skT"].t.rearrange("h d t -> d h t")[:, :, t0:t0 + N], S["skT"].res[0]), sk_sb[b][:, :, :])
            P.dma(V(S["gT"].t[:, t0:t0 + N], S["gT"].res[0]), g_sb[b][:, :])
            for tb in range(N // 128):
                r0 = t0 + tb * 128
                bb = tb % 2

                def tm(col0, W):
                    ps = pss.next()
                    for c in range(8):
                        P.mm(ps[:, 0:W], h[:, c, tb * 128:(tb + 1) * 128], w[:, c, col0:col0 + W],
                             start=(c == 0), stop=(c == 7))
                    return ps

                ps = tm(256, 256)
                P.act(tmk[bb][:, :], ps[:, 0:256], AF.Copy, scale=0.125)
                P.dma(V(S["mk_tm"].t[r0:r0 + 128, :], S["mk_tm"].res[0]), tmk[bb][:, :])
                ps = tm(512, 512)
                P.copy(tmv[bb][:, :], ps[:, :])
                P.dma(V(S["mv_tm"].t[r0:r0 + 128, :], S["mv_tm"].res[0]), tmv[bb][:, :])
                ps = tm(2568, 512)
                P.copy(tsv[bb][:, :], ps[:, :])
                P.dma(V(S["sv_tm"].t[r0:r0 + 128, :], S["sv_tm"].res[0]), tsv[bb][:, :])
                ps = tm(1024, 512)
                P.act(ex[bb][:, :], ps[:, :], AF.Exp, scale=-1.0)
                P.ts(ex[bb][:, :], ex[bb][:, :], 1.0, None, ALU.add)
                P.recip(ex[bb][:, :], ex[bb][:, :])
                P.copy(tsg[bb][:, :], ex[bb][:, :], eng="pool")
                P.dma(V(S["sg_tm"].t[r0:r0 + 128, :], S["sg_tm"].res[0]), tsg[bb][:, :])
        P.barrier()


def make_mask(P, C, shape, pattern, base, cm, op, val=1.0, dt=BF16, name="mask"):
    src = C.sb(shape, F32, name=name + "s")
    P.memset(src[:, :], val, eng="pool")
    m = C.sb(shape, dt, name=name)
    P.op("pool", lambda e: e.affine_select(m.t[:, :], src.t[:, :], pattern, op, 0.0, base=base, channel_multiplier=cm),
         reads=(src[:, :],), writes=(m[:, :],))
    return m


def sec_sb(nc, P, T, S, hcT, N=512):
    NB = T // 128
    QB = N // 128
    with ExitStack() as es:
        C = Ctx(nc, P, es)
        setup_consts(P, C)
        one_col = C.sb([128, 1], F32, name="onec")
        P.memset(one_col[:, :], 1.0)
        negtri = make_mask(P, C, [128, 128], [[-1, 128]], 0, 1, ALU.is_ge, val=-1.0, name="ntri")
        negones = C.sb([128, 128], BF16, name="nones")
        P.memset(negones[:, :], -1.0)
        masks = [make_mask(P, C, [128, N], [[1, N]], -r * 128, -1, ALU.is_gt, name=f"m{r}") for r in range(QB)]
        KT = [C.sb([128, T], BF16, name="KT") for _ in range(2)]
        QT = [C.sb([128, T], BF16, name="QT") for _ in range(2)]
        Vv = [C.sb([128, NB, 128], BF16, name="Vv") for _ in range(2)]
        NSLOT = 4
        bankAB = [C.ps([128, 512]) for _ in range(NSLOT)]
        bankO = [C.ps([128, 512]) for _ in range(NSLOT)]
        ezs = [C.sb([128, N], F32, name="ez") for _ in range(NSLOT)]
        sps = [Rot([C.sb([128, N], BF16, name="sp") for _ in range(2)]) for _ in range(NSLOT)]
        wts = [Rot([C.sb([128, N], BF16, name="wt") for _ in range(2)]) for _ in range(NSLOT)]
        Sfs = [C.sb([128, N], F32, name="Sf") for _ in range(NSLOT)]
        Sbs = [Rot([C.sb([128, N], BF16, name="Sb") for _ in range(2)]) for _ in range(NSLOT)]
        obs = [C.sb([128, N], BF16, name="ob") for _ in range(NSLOT)]
        loaded = set()

        def load_head(hd):
            b = hd % 2
            P.dma(KT[b][:, :], V(S["skT"].t[hd, :, :], S["skT"].res[0]))
            P.dma(QT[b][:, :], V(S["sqT"].t[hd, :, :], S["sqT"].res[0]))
            P.dma(Vv[b][:, :, :], V(S["sv_tm"].t.rearrange("(j p) c -> p j c", p=128)[:, :, hd * 128:(hd + 1) * 128],
                                   S["sv_tm"].res[0]))

        def chain(slot, hd, gq):
            b = hd % 2
            q = QT[b][:, gq * N:(gq + 1) * N]
            jmax = gq * QB + QB - 1
            pab, po = bankAB[slot], bankO[slot]
            e, Sf, o = ezs[slot], Sfs[slot], obs[slot]
            sb_prev = None
            for j in range(jmax, -1, -1):
                k = KT[b][:, j * 128:(j + 1) * 128]
                P.mm(pab[:, 0:N], k, q)
                yield
                P.act(e[:, :], pab[:, 0:N], AF.Exp)
                yield
                s_ = sps[slot].next()
                P.act(s_[:, :], e[:, :], AF.Ln, bias=1.0)
                yield
                r = j - gq * QB
                if r >= 0:
                    P.tt(s_[:, :], s_[:, :], masks[r][:, :], ALU.mult, eng="pool")
                    yield
                P.mm(pab[:, 0:N], negtri[:, :], s_[:, :], start=True, stop=False)
                if sb_prev is not None:
                    P.mm(pab[:, 0:N], negones[:, :], sb_prev[:, :], start=False, stop=False)
                P.mm(pab[:, 0:N], k, q, start=False, stop=True)
                yield
                w = wts[slot].next()
                P.act(w[:, :], pab[:, 0:N], AF.Exp)
                yield
                if r >= 0:
                    P.tt(w[:, :], w[:, :], masks[r][:, :], ALU.mult, eng="pool")
                    yield
                P.mm(po[:, 0:N], Vv[b][:, j, :], w[:, :], start=(j == jmax), stop=(j == 0))
                if j > 0:
                    if j == jmax:
                        P.copy(Sf[:, :], s_[:, :])
                    else:
                        P.tt(Sf[:, :], Sf[:, :], s_[:, :], ALU.add)
                    yield
                    sb_prev = Sbs[slot].next()
                    P.copy(sb_prev[:, :], Sf[:, :])
                yield
            P.copy(o[:, :], po[:, 0:N])
            yield
            P.dma(V(hcT.t[512 + hd * 128:512 + (hd + 1) * 128, gq * N:(gq + 1) * N], hcT.res[0]), o[:, :])

        work = [(hd, gq) for hd in range(4) for gq in range(T // N - 1, -1, -1)]
        slots = [None] * NSLOT
        slot_head = [None] * NSLOT
        wi = 0
        while True:
            busy = False
            for sl_ in range(NSLOT):
                if slots[sl_] is None and wi < len(work):
                    hd, gq = work[wi]
                    if not any(slots[z] is not None and slot_head[z] == hd - 2 for z in range(NSLOT)):
                        wi += 1
                        if hd not in loaded:
                            load_head(hd)
                            loaded.add(hd)
                        slots[sl_] = chain(sl_, hd, gq)
                        slot_head[sl_] = hd
                if slots[sl_] is not None:
                    busy = True
                    try:
                        next(slots[sl_])
                    except StopIteration:
                        slots[sl_] = None
            if not busy and wi >= len(work):
                break
        P.barrier()


def sec_mlstm(nc, P, T, S, b_i, b_f, head_gain, hcT):
    NCH = T // 128
    with ExitStack() as es:
        C = Ctx(nc, P, es)
        setup_consts(P, C)
        one4 = C.sb([4, 1], F32, name="one4")
        P.memset(one4[:, :], 1.0)
        ident = make_mask(P, C, [128, 128], [[-1, 128]], 0, 1, ALU.is_equal, dt=F32, name="identf")
        identb = C.sb([128, 128], BF16, name="identb")
        P.copy(identb[:, :], ident[:, :])
        maskLT = make_mask(P, C, [128, 128], [[1, 128]], 0, -1, ALU.is_ge, name="mlt")
        gain_bc = C.sb([128, 512], F32, name="gainbc")
        P.op("sp", lambda e: e.dma_start(out=gain_bc.t[:, :],
                                         in_=head_gain.rearrange("h v -> (h v)").partition_broadcast(128)),
             writes=(gain_bc[:, :],), dma=True)
        tok = C.sb([128, NCH, 12], F32, name="tok")
        with ExitStack() as es2:
            C2 = Ctx(nc, P, es2)
            mi = C2.sb([4, T], F32, name="mi")
            mf = C2.sb([4, T], F32, name="mf")
            P.dma(mi[:, :], V(S["gT"].t[0:4, :], S["gT"].res[0]))
            P.dma(mf[:, :], V(S["gT"].t[4:8, :], S["gT"].res[0]))
            bi = C2.sb([4, 1], F32, name="bi")
            bf = C2.sb([4, 1], F32, name="bf")
            P.op("sp", lambda e: e.dma_start(out=bi.t[:, :], in_=b_i.rearrange("(h o) -> h o", o=1)),
                 writes=(bi[:, :],), dma=True)
            P.op("sp", lambda e: e.dma_start(out=bf.t[:, :], in_=b_f.rearrange("(h o) -> h o", o=1)),
                 writes=(bf[:, :],), dma=True)
            P.ts(bi[:, :], bi[:, :], 1.0 / 15.0, None, ALU.mult)
            P.ts(bf[:, :], bf[:, :], -1.0, None, ALU.mult)
            t1 = C2.sb([4, T], F32, name="t1")
            P.act(t1[:, :], mi[:, :], AF.Tanh, scale=1.0 / 15.0, bias=bi[:, 0:1])
            e1 = C2.sb([4, T], F32, name="e1")
            P.act(e1[:, :], mf[:, :], AF.Exp, scale=-1.0, bias=bf[:, 0:1])
            P.act(e1[:, :], e1[:, :], AF.Ln, bias=one4[:, 0:1])
            P.ts(e1[:, :], e1[:, :], -1.0, None, ALU.mult)
            ones_r = mf
            P.memset(ones_r[:, :], 1.0)
            Fc = C2.sb([4, T], F32, name="Fc")
            P.scan(Fc[:, :], ones_r[:, :], e1[:, :], 0.0, ALU.mult, ALU.add)
            a = mi
            P.stt(a[:, :], t1[:, :], 15.0, Fc[:, :], ALU.mult, ALU.subtract)
            M = t1
            P.scan(M[:, :], a[:, :], a[:, :], 0.0, ALU.max, ALU.max)
            Er = C2.sb([4, T], F32, name="Er")
            Dr = e1
            Gr = ones_r
            gd = C2.sb([4, NCH], F32, name="gd")
            for c in range(NCH):
                sl = slice(c * 128, (c + 1) * 128)
                me = M[:, c * 128 + 127:c * 128 + 128]
                P.ts(Er[:, sl], a[:, sl], me, None, ALU.subtract)
                P.ts(Dr[:, sl], Fc[:, sl], -1.0, me, ALU.mult, ALU.subtract)
                if c == 0:
                    P.ts(gd[:, 0:1], me, -1.0, None, ALU.mult)
                else:
                    P.tt(gd[:, c:c + 1], M[:, c * 128 - 1:c * 128], me, ALU.subtract)
                P.ts(Gr[:, sl], Fc[:, sl], 0.0, gd[:, c:c + 1], ALU.mult, ALU.add)
            P.act(Er[:, :], Er[:, :], AF.Exp)
            P.act(Dr[:, :], Dr[:, :], AF.Exp, scale=2.0)
            P.act(Gr[:, :], Gr[:, :], AF.Exp)
            pst = Rot([C2.ps([128, 512]) for _ in range(2)])
            for c in range(NCH):
                sl = slice(c * 128, (c + 1) * 128)
                ps = pst.next()
                for i, rw in enumerate((Er, Dr, Gr)):
                    P.tr(ps[:, 4 * i:4 * i + 4], rw[:, sl], ident[0:4, 0:4])
                P.copy(tok[:, c, :], ps[:, 0:12])
            P.barrier()
        qT = [C.sb([64, 4, 128], BF16, name="qT") for _ in range(2)]
        kT = [C.sb([64, 4, 128], BF16, name="kT") for _ in range(2)]
        ktm = [C.sb([128, 256], BF16, name="ktm") for _ in range(2)]
        vtm = [C.sb([128, 512], BF16, name="vtm") for _ in range(2)]
        sgt = [C.sb([128, 512], BF16, name="sgt") for _ in range(2)]
        hm = [C.sb([128, 4, 128], BF16, name="hm", nres=4) for _ in range(2)]
        bankA = [C.ps([128, 512]) for _ in range(4)]
        bankB = [C.ps([128, 512]) for _ in range(4)]

        def load_chunk(c):
            b = c % 2
            sl = slice(c * 128, (c + 1) * 128)
            P.dma(qT[b][:, :, :], V(S["mqT"].t.rearrange("h d t -> d h t")[:, :, sl], S["mqT"].res[0]))
            P.dma(kT[b][:, :, :], V(S["mkT"].t.rearrange("h d t -> d h t")[:, :, sl], S["mkT"].res[0]))
            P.dma(ktm[b][:, :], V(S["mk_tm"].t[sl, :], S["mk_tm"].res[0]))
            P.dma(vtm[b][:, :], V(S["mv_tm"].t[sl, :], S["mv_tm"].res[0]))
            P.dma(sgt[b][:, :], V(S["sg_tm"].t[sl, :], S["sg_tm"].res[0]))

        def head_chain(hd):
            pa, pb = bankA[hd], bankB[hd]
            Cst = C.sb([64, 129], F32, name=f"Cst{hd}")
            Cbf = C.sb([64, 129], BF16, name=f"Cbf{hd}")
            s_m = C.sb([128, 128], BF16, name=f"sm{hd}")
            v_e = C.sb([128, 129], BF16, name=f"vt{hd}")
            jk = C.sb([128, 128], BF16, name=f"jk{hd}")
            s_ = C.sb([128, 8], F32, name=f"sc{hd}")
            o_1 = C.sb([128, 128], F32, name=f"o1{hd}")
            o_2 = C.sb([128, 128], F32, name=f"o2{hd}")
            P.memset(Cst[:, :], 0.0)
            yield
            for c in range(NCH):
                b = c % 2
                ecol = tok[:, c, hd:hd + 1]
                dcol = tok[:, c, 4 + hd:5 + hd]
                gcol = tok[0:64, c, 8 + hd:9 + hd]
                P.mm(pa[:, 0:128], kT[b][:, hd, :], qT[b][:, hd, :])
                P.ts(v_e[:, 0:128], vtm[b][:, hd * 128:(hd + 1) * 128], ecol, None, ALU.mult, eng="pool")
                yield
                P.tt(s_m[:, :], pa[:, 0:128], maskLT[:, :], ALU.mult)
                P.copy(v_e[:, 128:129], ecol, eng="pool")
                yield
                P.ts(Cbf[:, :], Cst[:, :], gcol, None, ALU.mult)
                yield
                P.mm(pb[:, 0:129], s_m[:, :], v_e[:, :], start=True, stop=False)
                P.mm(pb[:, 0:129], qT[b][:, hd, :], Cbf[:, :], start=False, stop=True)
                P.mm(pa[0:64, 128:257], ktm[b][:, hd * 64:(hd + 1) * 64], v_e[:, :])
                yield
                P.stt(Cst[:, :], Cst[:, :], gcol, pa[0:64, 128:257], ALU.mult, ALU.add)
                P.act(jk[:, :], pb[:, 0:128], AF.Square, accum=s_[:, 0:1])
                yield
                P.copy(s_[:, 1:2], pb[:, 128:129])
                yield
                P.ts(s_[:, 2:3], s_[:, 1:2], s_[:, 1:2], None, ALU.mult)
                yield
                P.ts(s_[:, 2:3], s_[:, 2:3], dcol, EPS * 128.0, ALU.max, ALU.mult)
                yield
                P.tt(s_[:, 3:4], s_[:, 2:3], s_[:, 0:1], ALU.add)
                yield
                P.act(s_[:, 4:5], s_[:, 3:4], AF.Ln, scale=1.0 / 128.0)
                yield
                P.act(s_[:, 5:6], s_[:, 4:5], AF.Exp, scale=-0.5)
                yield
                P.stt(o_1[:, :], pb[:, 0:128], s_[:, 5:6], gain_bc[:, hd * 128:(hd + 1) * 128], ALU.mult, ALU.mult)
                yield
                P.tt(o_2[:, :], o_1[:, :], sgt[b][:, hd * 128:(hd + 1) * 128], ALU.mult, eng="pool")
                yield
                P.tr(pa[:, 384:512], o_2[:, :], ident[:, :])
                yield
                P.copy(hm[b].v((slice(None), hd, slice(None)), hd), pa[:, 384:512])
                yield "chunk_done"

        chains = [head_chain(hd) for hd in range(4)]
        for ch_ in chains:
            next(ch_)
        load_chunk(0)
        for c in range(NCH):
            if c + 1 < NCH:
                load_chunk(c + 1)
            active = list(chains)
            while active:
                for ch_ in list(active):
                    if next(ch_) == "chunk_done":
                        active.remove(ch_)
            b = c % 2
            sl = slice(c * 128, (c + 1) * 128)
            P.op("sp", lambda e, b=b, sl=sl: e.dma_start(
                out=hcT.t[0:512, :].rearrange("(h p) t -> p h t", p=128)[:, :, sl], in_=hm[b].t[:, :, :]),
                reads=tuple(V(hm[b].t[:, :, :], hm[b].res[k]) for k in range(4)), writes=(hcT[:, :],), dma=True)
        P.barrier()


def sec_pre_c(nc, P, T, xin, mix_norm, w_in, conv_w, S, N=512):
    with ExitStack() as es:
        C = Ctx(nc, P, es)
        setup_consts(P, C)
        g = load_gain(P, C, mix_norm, "gmix")
        w = load_w(P, C, w_in, D, C_IN, "win")
        cw = C.sb([128, 24, 4], F32, name="cw")
        for j in range(4):
            P.op("sp", lambda e, j=j: e.dma_start(out=cw.t[:, :, j], in_=conv_w[j, :].rearrange("(c p) -> p c", p=128),
                                                 allow_slow_non_contiguous=True), writes=(cw[:, :, :],), dma=True)
        ident = make_mask(P, C, [128, 128], [[-1, 128]], 0, 1, ALU.is_equal, dt=BF16, name="identb")
        halo = C.sb([128, 24, 3], BF16, name="halo")
        P.memset(halo[:, :, :], 0.0)
        dg = C.sb([128, 24, 4, 128], BF16, name="dg")
        for ch in range(24):
            for j in range(4):
                P.ts(dg[:, ch, j, :], ident[:, :], cw[:, ch, j:j + 1], None, ALU.mult, eng=("pool" if (ch + j) % 2 else "dve"))
        pss = Rot([C.ps([128, 512]) for _ in range(6)])
        psT = Rot([C.ps([128, 1024], BF16) for _ in range(2)])
        xs = [C.sb([128, 8, N], F32, name="x") for _ in range(1)]
        sq = C.sb([128, 8, N], BF16, name="sq")
        h = C.sb([128, 8, N], BF16, name="h")
        rstd = C.sb([128, N], F32, name="rstd")
        lnt = C.sb([128, N], F32, name="lnt")
        cv = Rot([C.sb([128, N + 3], BF16, name="cv") for _ in range(3)])
        y = C.sb([128, 16, N], F32, name="y", nres=16)
        vb = C.sb([128, 8, N], BF16, name="vb", nres=8)
        kb = C.sb([128, 8, N], BF16, name="kb", nres=8)
        qb = C.sb([128, 8, N], BF16, name="qb")
        s2 = Rot([C.sb([128, N], BF16, name="s2") for _ in range(4)])
        l2 = Rot([C.sb([128, N], F32, name="l2") for _ in range(4)])
        ba = [C.sb([8, N], F32, name="ba") for _ in range(2)]
        tsg = Rot([C.sb([128, 512], BF16, name="tsg") for _ in range(2)])
        ttm = Rot([C.sb([128, 1024], BF16, name="ttm") for _ in range(2)])
        xin_v = xin.t.rearrange("(c p) t -> p c t", p=128)
        for it in range(T // N):
            t0 = it * N
            x = xs[0]
            P.dma(x[:, :, :], V(xin_v[:, :, t0:t0 + N], xin.res[0]))
            rmsnorm_tile(P, C, x, g, h, N, C.ones, pss, sq, rstd, lnt)
            def proj(ch):
                ps = pss.next()
                for c in range(8):
                    P.mm(ps[:, 0:N], w[:, c, ch * 128:(ch + 1) * 128], h[:, c, :], start=(c == 0), stop=(c == 7))
                cvt = cv.next()
                P.act(cvt[:, 3:3 + N], ps[:, 0:N], AF.Copy)
                P.copy(cvt[:, 0:3], halo[:, ch, :])
                P.copy(halo[:, ch, :], cvt[:, N:N + 3])
                return cvt

            def conv(ch, cvt):
                ps2 = pss.next()
                for j in range(4):
                    P.mm(ps2[:, 0:N], dg[:, ch, j, :], cvt[:, j:j + N], start=(j == 0), stop=(j == 3))
                if ch < 16:
                    P.act(y.v((slice(None), ch, slice(None)), ch), ps2[:, 0:N], AF.Silu)
                else:
                    P.act(vb.v((slice(None), ch - 16, slice(None)), ch - 16), ps2[:, 0:N], AF.Silu)

            pend = [proj(0), proj(1)]
            for ch in range(24):
                if ch + 2 < 24:
                    pend.append(proj(ch + 2))
                conv(ch, pend.pop(0))
            for tb in range(N // 128):
                r0 = t0 + tb * 128
                for half in range(2):
                    ps = pss.next()
                    for c in range(8):
                        P.mm(ps[:, :], h[:, c, tb * 128:(tb + 1) * 128], w[:, c, 3072 + half * 512:3072 + (half + 1) * 512],
                             start=(c == 0), stop=(c == 7))
                    tg = tsg.next()
                    P.act(tg[:, :], ps[:, :], AF.Silu)
                    P.dma(V(S["sg_tm"].t[r0:r0 + 128, half * 512:(half + 1) * 512], S["sg_tm"].res[0]), tg[:, :])
            for i, nm in enumerate(("bT", "aT")):
                ps = pss.next()
                for c in range(8):
                    P.mm(ps[0:8, 0:N], w[:, c, 4096 + 8 * i:4104 + 8 * i], h[:, c, :], start=(c == 0), stop=(c == 7))
                P.act(ba[i][:, :], ps[0:8, 0:N], AF.Copy)
                P.dma(V(S[nm].t[:, t0:t0 + N], S[nm].res[0]), ba[i][:, :])
            for c0 in range(0, 16, 4):
                chs = list(range(c0, c0 + 4))
                yv = {ch: y.v((slice(None), ch, slice(None)), ch) for ch in chs}
                sv, pv, lv = {}, {}, {}
                for ch in chs:
                    sv[ch] = s2.next()
                    P.act(sv[ch][:, :], yv[ch], AF.Square)
                for ch in chs:
                    pv[ch] = pss.next()
                    P.mm(pv[ch][:, 0:N], C.ones[:, :], sv[ch][:, :])
                for ch in chs:
                    lv[ch] = l2.next()
                    P.act(lv[ch][:, :], pv[ch][:, 0:N], AF.Ln, bias=C.eps[:, 0:1])
                for ch in chs:
                    P.act(lv[ch][:, :], lv[ch][:, :], AF.Exp, scale=-0.5)
                for ch in chs:
                    if ch < 8:
                        P.stt(qb[:, ch, :], yv[ch], 128 ** -0.5, lv[ch][:, :], ALU.mult, ALU.mult)
                    else:
                        P.tt(kb.v((slice(None), ch - 8, slice(None)), ch - 8), yv[ch], lv[ch][:, :], ALU.mult)
            P.dma(V(S["qT"].t.rearrange("h d t -> d h t")[:, :, t0:t0 + N], S["qT"].res[0]), qb[:, :, :])
            P.dma(V(S["kT"].t.rearrange("h d t -> d h t")[:, :, t0:t0 + N], S["kT"].res[0]),
                  V(kb.t[:, :, :], kb.res[7]))
            for src, nm in ((kb, "k_tm"), (vb, "v_tm")):
                for tb in range(N // 128):
                    r0 = t0 + tb * 128
                    pt = psT.next()
                    for ch in range(8):
                        P.tr(pt[:, ch * 128:(ch + 1) * 128], src.v((slice(None), ch, slice(tb * 128, (tb + 1) * 128)), ch),
                             ident[:, :])
                    tt_ = ttm.next()
                    P.copy(tt_[:, :], pt[:, :])
                    P.dma(V(S[nm].t[r0:r0 + 128, :], S[nm].res[0]), tt_[:, :])
        P.barrier()


def sec_gdn(nc, P, T, S, a_log, dt_bias, head_gain, hcT, SCR):
    NSC = T // 128
    NCH = T // 64
    SEG = min(T, 2048)
    with ExitStack() as es:
        C = Ctx(nc, P, es)
        setup_consts(P, C)
        one8 = C.sb([8, 1], F32, name="one8")
        P.memset(one8[:, :], 1.0)
        identf = make_mask(P, C, [128, 128], [[-1, 128]], 0, 1, ALU.is_equal, dt=F32, name="identf")
        identb = C.sb([128, 128], BF16, name="identb")
        P.copy(identb[:, :], identf[:, :])
        mS = make_mask(P, C, [128, 128], [[-1, 128]], 0, 1, ALU.is_gt, dt=F32, name="mS")
        mI = make_mask(P, C, [128, 128], [[1, 128]], 0, -1, ALU.is_ge, dt=F32, name="mI")
        P.memset(mS[64:128, 0:64], 0.0, eng="pool")
        P.memset(mI[0:64, 64:128], 0.0, eng="pool")
        BIG = 30000.0
        pmS = C.sb([128, 128], F32, name="pmS")
        P.ts(pmS[:, :], mS[:, :], -BIG, BIG, ALU.mult, ALU.add)
        nmI = C.sb([128, 128], F32, name="nmI")
        P.ts(nmI[:, :], mI[:, :], BIG, -BIG, ALU.mult, ALU.add)
        gain_bc = C.sb([128, 128], F32, name="gainbc")
        P.op("sp", lambda e: e.dma_start(out=gain_bc.t[:, :], in_=head_gain.partition_broadcast(128)),
             writes=(gain_bc[:, :],), dma=True)
        tokc = C.sb([128, NSC, 5, 8], F32, name="tokc")
        egl = C.sb([128, 8, NCH], F32, name="egl")
        with ExitStack() as es2:
            C2 = Ctx(nc, P, es2)
            bt = C2.sb([8, T], F32, name="bt")
            at = C2.sb([8, T], F32, name="at")
            P.dma(bt[:, :], V(S["bT"].t[:, :], S["bT"].res[0]))
            P.dma(at[:, :], V(S["aT"].t[:, :], S["aT"].res[0]))
            al = C2.sb([8, 1], F32, name="al")
            db = C2.sb([8, 1], F32, name="db")
            P.op("sp", lambda e: e.dma_start(out=al.t[:, :], in_=a_log.rearrange("(h o) -> h o", o=1)),
                 writes=(al[:, :],), dma=True)
            P.op("sp", lambda e: e.dma_start(out=db.t[:, :], in_=dt_bias.rearrange("(h o) -> h o", o=1)),
                 writes=(db[:, :],), dma=True)
            P.act(al[:, :], al[:, :], AF.Exp)
            P.ts(al[:, :], al[:, :], -1.0, None, ALU.mult)
            P.act(bt[:, :], bt[:, :], AF.Exp, scale=-1.0)
            P.ts(bt[:, :], bt[:, :], 1.0, None, ALU.add)
            P.recip(bt[:, :], bt[:, :])
            P.act(at[:, :], at[:, :], AF.Exp, bias=db[:, 0:1])
            P.act(at[:, :], at[:, :], AF.Ln, bias=one8[:, 0:1])
            P.ts(at[:, :], at[:, :], al[:, 0:1], None, ALU.mult)
            nf = C2.sb([8, T], F32, name="nf")
            P.memset(nf[:, :], 1.0)
            P.memset(V(nf.t[:, :].rearrange("p (c l) -> p c l", l=64)[:, :, 0:1], nf.res[0]), 0.0)
            gr = C2.sb([8, T], F32, name="gr")
            P.scan(gr[:, :], nf[:, :], at[:, :], 0.0, ALU.mult, ALU.add)
            P.dma(SCR["gD"][:, :], gr[:, :])
            eg = C2.sb([8, T], F32, name="eg")
            P.act(eg[:, :], gr[:, :], AF.Exp)
            beg = nf
            P.tt(beg[:, :], bt[:, :], eg[:, :], ALU.mult)
            egg = at
            for c in range(NCH):
                sl = slice(c * 64, (c + 1) * 64)
                P.ts(egg[:, sl], gr[:, sl], -1.0, gr[:, c * 64 + 63:c * 64 + 64], ALU.mult, ALU.add)
            P.act(egg[:, :], egg[:, :], AF.Exp)
            eG = C2.sb([8, NCH], F32, name="eG")
            P.copy(eG[:, :], V(eg.t[:, :].rearrange("p (c l) -> p c l", l=64)[:, :, 63], eg.res[0]))
            P.dma(SCR["eGD"][:, :], eG[:, :])
            P.op("sp", lambda e: e.dma_start(out=egl.t[:, :, :].rearrange("p h c -> p (h c)"),
                                             in_=SCR["eGD"].t.rearrange("h c -> (h c)").partition_broadcast(128)),
                 reads=(SCR["eGD"][:, :],), writes=(egl[:, :, :],), dma=True)
            pst = Rot([C2.ps([128, 512]) for _ in range(2)])
            for sc in range(NSC):
                sl = slice(sc * 128, (sc + 1) * 128)
                ps = pst.next()
                for i, rw in enumerate((gr, bt, beg, egg, eg)):
                    P.tr(ps[:, 8 * i:8 * i + 8], rw[:, sl], identf[0:8, 0:8])
                P.copy(V(tokc.t[:, sc, :, :].rearrange("p a b -> p (a b)"), tokc.res[0]), ps[:, 0:40])
            P.barrier()
        GT = 256
        NG = T // GT
        SPG = GT // 128
        qTg = [C.sb([128, 8, GT], BF16, name="qTg") for _ in range(2)]
        kTg = [C.sb([128, 8, GT], BF16, name="kTg") for _ in range(2)]
        ktg = [C.sb([128, SPG, D], BF16, name="ktg") for _ in range(2)]
        vtg = [C.sb([128, SPG, D], BF16, name="vtg") for _ in range(2)]
        sgg = [C.sb([128, SPG, D], BF16, name="sgg") for _ in range(2)]
        Gbg = [C.sb([128, 8, GT], F32, name="Gbg") for _ in range(2)]
        banks = [C.ps([128, 512]) for _ in range(8)]

        def load_group(gi):
            b = gi % 2
            t0 = gi * GT
            P.dma(qTg[b][:, :, :], V(S["qT"].t.rearrange("h d t -> d h t")[:, :, t0:t0 + GT], S["qT"].res[0]))
            P.dma(kTg[b][:, :, :], V(S["kT"].t.rearrange("h d t -> d h t")[:, :, t0:t0 + GT], S["kT"].res[0]))
            for tl, nm in ((ktg, "k_tm"), (vtg, "v_tm"), (sgg, "sg_tm")):
                P.dma(tl[b][:, :, :], V(S[nm].t[t0:t0 + GT, :].rearrange("(s p) c -> p s c", p=128), S[nm].res[0]))
            for hd in range(8):
                P.op("sp", lambda e, b=b, t0=t0, hd=hd: e.dma_start(
                    out=Gbg[b].t[:, hd, :], in_=SCR["gD"].t[hd, t0:t0 + GT].partition_broadcast(128)),
                    reads=(SCR["gD"][:, :],), writes=(Gbg[b][:, :, :],), dma=True)

        def head_chain(hd):
            bank = banks[hd]

            def t32(name):
                return C.sb([128, 128], F32, name=f"{name}{hd}")

            def t16(name):
                return C.sb([128, 128], BF16, name=f"{name}{hd}")

            dtmp, Da, Dt = t32("dtmp"), t32("Da"), t32("Dt")
            XYs = Rot([C.sb([128, 256], F32, name=f"XY{hd}") for _ in range(2)])
            Qs = Rot([t32("Q"), t32("Q")])
            af, u, t3, o_, o_1, o_2 = t32("af"), t32("u"), t32("t3"), t32("o"), t32("o1"), t32("o2")
            ab_, tb_, v_b, k_b, k_d, wT_, vn, jk, hb = (t16("ab"), t16("tb"), t16("vb"), t16("kb"), t16("kd"),
                                                       t16("wT"), t16("vn"), t16("jk"), t16("hb"))
            Sst = t32("Sst")
            Sbf = Rot([t16("Sbf"), t16("Sbf")])
            s_ = C.sb([128, 4], F32, name=f"scl{hd}")
            P.memset(Sst[:, :], 0.0)
            sbf = Sbf.next()
            P.memset(sbf[:, :], 0.0)
            s0, s1, s2, s3 = (slice(0, 128), slice(128, 256), slice(256, 384), slice(384, 512))
            yield
            for sc in range(NSC):
                gi = (sc * 128) // GT
                b = gi % 2
                si = sc % SPG
                lsl = slice(si * 128, (si + 1) * 128)
                hsl = slice(hd * 128, (hd + 1) * 128)
                kTs = kTg[b][:, hd, lsl]
                qTs = qTg[b][:, hd, lsl]
                gsl = Gbg[b][:, hd, lsl]
                gcol = tokc[:, sc, 0, hd:hd + 1]
                bcol = tokc[:, sc, 1, hd:hd + 1]
                begcol = tokc[:, sc, 2, hd:hd + 1]
                eggcol = tokc[:, sc, 3, hd:hd + 1]
                P.mm(bank[:, s0], kTs, kTs)
                P.mm(bank[:, s1], kTs, qTs)
                P.stt(dtmp[:, :], gsl, gcol, pmS[:, :], ALU.subtract, ALU.max)
                P.ts(v_b[:, :], vtg[b][:, si, hsl], bcol, None, ALU.mult, eng="pool")
                yield
                P.act(Da[:, :], dtmp[:, :], AF.Exp, scale=-1.0)
                yield
                XY = XYs.next()
                X, Y = XY[:, 0:128], XY[:, 128:256]
                P.stt(X, bank[:, s0], bcol, Da[:, :], ALU.mult, ALU.mult)
                P.ts(k_b[:, :], ktg[b][:, si, hsl], begcol, None, ALU.mult, eng="pool")
                yield
                P.tr(bank[:, s2], X, identf[:, :])
                P.stt(dtmp[:, :], gsl, gcol, nmI[:, :], ALU.subtract, ALU.min)
                yield
                P.act(Y, bank[:, s2], AF.Copy)
                yield
                P.act(Dt[:, :], dtmp[:, :], AF.Exp)
                Q = Qs.next()
                P.tt(Q[:, :], identf[:, :], Y, ALU.subtract)
                yield
                P.tt(ab_[:, :], bank[:, s1], Dt[:, :], ALU.mult)
                P.ts(k_d[:, :], ktg[b][:, si, hsl], eggcol, None, ALU.mult, eng="pool")
                yield
                for lvl in range(1, 6):
                    P.mm(bank[:, s0], Y, X)
                    if lvl < 5:
                        P.mm(bank[:, s1], X, Y)
                    yield
                    XYn = XYs.next()
                    if lvl < 5:
                        P.act(XYn[:, 0:256], bank[:, 0:256], AF.Copy)
                    else:
                        P.act(XYn[:, 0:128], bank[:, s0], AF.Copy)
                    Xn, Yn = XYn[:, 0:128], XYn[:, 128:256]
                    yield
                    P.mm(bank[:, s2], Xn, Q[:, :])
                    yield
                    Qn = Qs.next()
                    P.tt(Qn[:, :], bank[:, s2], Q[:, :], ALU.add)
                    yield
                    X, Y, Q = Xn, Yn, Qn
                P.act(tb_[:, :], Q[:, :], AF.Copy)
                yield
                P.mm(bank[:, s0], tb_[:, :], v_b[:, :])
                P.mm(bank[:, s1], k_b[:, :], tb_[:, :])
                yield
                P.act(u[:, :], bank[:, s0], AF.Copy)
                yield
                P.act(wT_[:, :], bank[:, s1], AF.Copy)
                yield
                for cc in range(2):
                    pr = slice(cc * 64, (cc + 1) * 64)
                    isl = slice(si * 128 + cc * 64, si * 128 + (cc + 1) * 64)
                    ch = sc * 2 + cc
                    P.mm(bank[pr, s2], wT_[:, pr], sbf[:, :])
                    P.mm(bank[pr, s3], qTg[b][:, hd, isl], sbf[:, :])
                    yield
                    P.tt(vn[pr, :], u[pr, :], bank[pr, s2], ALU.subtract)
                    yield
                    P.mm(bank[pr, s0], ab_[pr, pr], vn[pr, :])
                    P.mm(bank[:, s1], k_d[pr, :], vn[pr, :])
                    yield
                    P.stt(Sst[:, :], Sst[:, :], egl[:, hd, ch:ch + 1], bank[:, s1], ALU.mult, ALU.add)
                    yield
                    sbf = Sbf.next()
                    P.act(sbf[:, :], Sst[:, :], AF.Copy)
                    P.act(t3[pr, :], bank[pr, s0], AF.Copy)
                    yield
                    P.stt(o_[pr, :], bank[pr, s3], V(tokc.t[pr, sc, 4, hd:hd + 1], tokc.res[0]), t3[pr, :],
                          ALU.mult, ALU.add)
                    yield
                P.act(jk[:, :], o_[:, :], AF.Square, accum=s_[:, 0:1])
                yield
                P.act(s_[:, 1:2], s_[:, 0:1], AF.Ln, scale=1.0 / 128.0, bias=C.eps[:, 0:1])
                yield
                P.act(s_[:, 2:3], s_[:, 1:2], AF.Exp, scale=-0.5)
                yield
                P.stt(o_1[:, :], o_[:, :], s_[:, 2:3], gain_bc[:, :], ALU.mult, ALU.mult)
                yield
                P.tt(o_2[:, :], o_1[:, :], sgg[b][:, si, hsl], ALU.mult, eng="pool")
                yield
                P.tr(bank[:, s2], o_2[:, :], identf[:, :])
                yield
                P.act(hb[:, :], bank[:, s2], AF.Copy)
                yield
                P.dma(V(hcT.t[hd * 128:(hd + 1) * 128, sc * 128:(sc + 1) * 128], hcT.res[0]), hb[:, :])
                yield "sc_done"

        chains = [head_chain(hd) for hd in range(8)]
        for ch_ in chains:
            next(ch_)
        load_group(0)
        for sc in range(NSC):
            if (sc * 128) % GT == 0:
                gi = (sc * 128) // GT
                if gi + 1 < NG:
                    load_group(gi + 1)
            active = list(chains)
            while active:
                for ch_ in list(active):
                    if next(ch_) == "sc_done":
                        active.remove(ch_)
        P.barrier()


W_SHAPES = dict(
    mix_norm=[4, D], ab_w_in=[2, D, AB_IN], ab_b_i=[2, 4], ab_b_f=[2, 4], ab_head_gain=[2, 4, 128],
    ab_w_out=[2, D, D], c_w_in=[2, D, C_IN], c_conv_w=[2, 4, 3072], c_a_log=[2, 8], c_dt_bias=[2, 8],
    c_head_gain=[2, 128], c_w_out=[2, D, D], xa_norm=[4, D], mem_norm=[D], xa_wq=[4, D, D], xa_wk=[4, D, D],
    xa_wv=[4, D, D], xa_wo=[4, D, D], mlp_norm=[4, D], mlp_w1=[4, D, DFF], mlp_w2=[4, DFF, D], final_norm=[D])


def build_program(T, depth=DEPTH):
    nc = bass.Bass("TRN2", target_bir_lowering=False)
    P = Prog(nc)
    xT = dram(nc, "xT", [D, T], F32, kind="ExternalInput")
    memT = dram(nc, "memT", [D, MEM], F32, kind="ExternalInput")
    W = {n: nc.dram_tensor(n, shp, F32, kind="ExternalInput").ap() for n, shp in W_SHAPES.items()}
    outT = dram(nc, "outT", [D, T], F32, kind="ExternalOutput")
    xA = dram(nc, "xA", [D, T], F32)
    xB = dram(nc, "xB", [D, T], F32)
    hcT = dram(nc, "hcT", [D, T], BF16)
    memnT = dram(nc, "memnT", [D, MEM], BF16)
    SA = dict(mqT=dram(nc, "mqT", [4, 64, T], BF16), mkT=dram(nc, "mkT", [4, 64, T], BF16), gT=dram(nc, "gT", [8, T], F32),
              sqT=dram(nc, "sqT", [4, 128, T], BF16), skT=dram(nc, "skT", [4, 128, T], BF16),
              mk_tm=dram(nc, "mk_tm", [T, 256], BF16), mv_tm=dram(nc, "mv_tm", [T, 512], BF16),
              sg_tm=dram(nc, "sg_tm", [T, 512], BF16), sv_tm=dram(nc, "sv_tm", [T, 512], BF16))
    SC = dict(qT=dram(nc, "cqT", [8, 128, T], BF16), kT=dram(nc, "ckT", [8, 128, T], BF16),
              k_tm=dram(nc, "ck_tm", [T, D], BF16), v_tm=dram(nc, "cv_tm", [T, D], BF16),
              sg_tm=dram(nc, "csg_tm", [T, D], BF16), bT=dram(nc, "cbT", [8, T], F32), aT=dram(nc, "caT", [8, T], F32))
    SCR = dict(gD=dram(nc, "gD", [8, T], F32), eGD=dram(nc, "eGD", [8, T // 64], F32))
    sec_memn(nc, P, memT, W["mem_norm"], memnT)
    cur = xT
    for l in range(depth):
        j = l // 2
        if l % 2 == 0:
            sec_pre_ab(nc, P, T, cur, W["mix_norm"][l], W["ab_w_in"][j], SA)
            sec_sb(nc, P, T, SA, hcT)
            sec_mlstm(nc, P, T, SA, W["ab_b_i"][j], W["ab_b_f"][j], W["ab_head_gain"][j], hcT)
            w_out = W["ab_w_out"][j]
        else:
            sec_pre_c(nc, P, T, cur, W["mix_norm"][l], W["c_w_in"][j], W["c_conv_w"][j], SC)
            sec_gdn(nc, P, T, SC, W["c_a_log"][j], W["c_dt_bias"][j], W["c_head_gain"][j], hcT, SCR)
            w_out = W["c_w_out"][j]
        sec_post_a(nc, P, T, cur, hcT, xB, w_out, W["xa_norm"][l], W["xa_wq"][l], W["xa_wk"][l], W["xa_wv"][l],
                   W["xa_wo"][l], memnT)
        last = (l == depth - 1)
        sec_post_b(nc, P, T, xB, outT if last else xA, W["mlp_norm"][l], W["mlp_w1"][l], W["mlp_w2"][l],
                   final_norm=W["final_norm"] if last else None)
        cur = xA
    P.finalize()
    return nc, P


def kernel(**inputs):
    x = np.asarray(inputs["x"], dtype=np.float32)
    mem = np.asarray(inputs["mem"], dtype=np.float32)
    B, T, _ = x.shape
    nc, _ = build_program(T)
    wts = {n: np.ascontiguousarray(np.asarray(inputs[n], dtype=np.float32)) for n in W_SHAPES}
    in_maps = []
    for b in range(B):
        m = dict(wts)
        m["xT"] = np.ascontiguousarray(x[b].T)
        m["memT"] = np.ascontiguousarray(mem[b].T)
        in_maps.append(m)
    res = run_bass_kernel_spmd(nc, in_maps, core_ids=list(range(B)))
    out = np.stack([np.ascontiguousarray(r["outT"].T) for r in res.results], axis=0)
    return out.astype(np.float32)
```
